# Optimizing a Trainium2 kernel written in Bass

```python
import math
import jax, jax.numpy as jnp
from jax import lax
import numpy as np

D_MODEL = 1024
BATCH = 2
SEQ = 8192
DEPTH = 2

A_GROUPS = 4
A_GROUP_DIM = 128
A_WIDTH = A_GROUPS * A_GROUP_DIM
A_CHUNK = 128
B_HEADS = 4
B_KDIM = 128
B_VDIM = 128
B_QK_WIDTH = B_HEADS * B_KDIM
B_WIDTH = B_HEADS * B_VDIM
B_CHUNK = 64
C_HEADS = 16
C_HEAD_DIM = D_MODEL // C_HEADS
C_WIDTH = C_HEADS * C_HEAD_DIM
DILATED_PATTERNS = ((128, 1), (512, 4), (2048, 16))
REL_BUCKETS = 32
REL_MAX_DISTANCE = 1024
N_EXPERTS = 16
EXPERT_D_FF = 1024
CAPACITY_FACTOR = 2
EPS = 1e-6
NEG_INF = -1e30

kernel_name = 'hybrid_gmlp_hgrn2_dilated_ec_moe'


def rmsnorm(x, g):
    xf = x.astype(jnp.float32)
    y = xf * lax.rsqrt(jnp.mean(xf * xf, axis=-1, keepdims=True) + EPS)
    return (y * g.astype(jnp.float32)).astype(x.dtype)


def layernorm(x, g, b):
    xf = x.astype(jnp.float32)
    mu = jnp.mean(xf, axis=-1, keepdims=True)
    xc = xf - mu
    y = xc * lax.rsqrt(jnp.mean(xc * xc, axis=-1, keepdims=True) + EPS)
    return (y * g.astype(jnp.float32) + b.astype(jnp.float32)).astype(x.dtype)


def chunked_spatial_gating(u, v, w_s, b_s, ln_g, ln_b):
    B, S, _ = v.shape
    v = layernorm(v, ln_g, ln_b)
    vc = v.reshape(B, S // A_CHUNK, A_CHUNK, A_GROUPS, A_GROUP_DIM)
    mixed = jnp.einsum('gts,bnsgc->bntgc', w_s, vc) + b_s.T[:, :, None]
    return u * mixed.reshape(B, S, A_WIDTH)


def gla_chunk_scan(q, k, log_f, v):
    B, S, H, K = q.shape
    V = v.shape[-1]
    n = S // B_CHUNK

    def chunks(t):
        return t.reshape(B, n, B_CHUNK, H, t.shape[-1]).transpose(1, 0, 3, 2, 4)

    lower = jnp.tril(jnp.ones((B_CHUNK, B_CHUNK), dtype=bool))[:, :, None]

    def step(state, inp):
        qc, kc, fc, vc = inp
        b = jnp.cumsum(fc, axis=2)
        diff = b[:, :, :, None, :] - b[:, :, None, :, :]
        decay = jnp.exp(jnp.where(lower, diff, -jnp.inf))
        scores = jnp.einsum('bhtsk,bhsk->bhts', qc[:, :, :, None, :] * decay, kc)
        o = (jnp.einsum('bhts,bhsv->bhtv', scores, vc)
             + jnp.einsum('bhtk,bhkv->bhtv', qc * jnp.exp(b), state))
        b_end = b[:, :, -1]
        state = (state * jnp.exp(b_end)[..., None]
                 + jnp.einsum('bhsk,bhsv->bhkv', kc * jnp.exp(b_end[:, :, None] - b), vc))
        return state, o

    state0 = jnp.zeros((B, H, K, V), jnp.float32)
    _, o = lax.scan(step, state0, (chunks(q), chunks(k), chunks(log_f), chunks(v)))
    return o.transpose(1, 0, 3, 2, 4).reshape(B, S, H, V)


def hgrn2_bidirectional(q_raw, f_fwd_raw, f_bwd_raw, i_raw, gate, lb_fwd, lb_bwd, norm_g):
    B, S, _ = q_raw.shape

    def heads(t, d):
        return t.reshape(B, S, B_HEADS, d).astype(jnp.float32)

    q = heads(jax.nn.silu(q_raw), B_KDIM)
    v = heads(i_raw, B_VDIM)

    def direction(f_raw, lb, rev):
        f = lb + (1.0 - lb) * jax.nn.sigmoid(f_raw.astype(jnp.float32))
        args = (q, heads(1.0 - f, B_KDIM), heads(jnp.log(f), B_KDIM), v)
        if rev:
            return gla_chunk_scan(*[a[:, ::-1] for a in args])[:, ::-1]
        return gla_chunk_scan(*args)

    o = direction(f_fwd_raw, lb_fwd, False) + direction(f_bwd_raw, lb_bwd, True)
    o = o * lax.rsqrt(jnp.mean(o * o, axis=-1, keepdims=True) + EPS)
    o = o.reshape(B, S, B_WIDTH) * norm_g.astype(jnp.float32)
    o = o * jax.nn.sigmoid(gate.astype(jnp.float32))
    return o.astype(q_raw.dtype)


def t5_bucket(rel):
    half_buckets = REL_BUCKETS // 2
    max_exact = half_buckets // 2
    n = jnp.abs(rel)
    scaled = (jnp.log(jnp.maximum(n, 1).astype(jnp.float32) / max_exact)
              / math.log(REL_MAX_DISTANCE / max_exact))
    large = jnp.minimum(max_exact + (scaled * (half_buckets - max_exact)).astype(jnp.int32),
                        half_buckets - 1)
    return jnp.where(rel > 0, half_buckets, 0) + jnp.where(n < max_exact, n, large)


def dilated_branch(q, k, v, rel_table, dilation, half):
    B, S, H, Dh = q.shape
    n = S // dilation
    nb = -(-n // half)
    pad = nb * half - n

    def strided(t, front, back):
        t = t.reshape(B, n, dilation, H, Dh).transpose(0, 2, 1, 3, 4)
        return jnp.pad(t, ((0, 0), (0, 0), (front, back), (0, 0), (0, 0)))

    qb = strided(q, 0, pad).reshape(B, dilation, nb, half, H, Dh)

    def band(t):
        tb = strided(t, half, half + pad).reshape(B, dilation, nb + 2, half, H, Dh)
        return jnp.concatenate([tb[:, :, :-2], tb[:, :, 1:-1], tb[:, :, 2:]], axis=3)

    kb, vb = band(k), band(v)
    qi = jnp.arange(half)
    kc = jnp.arange(3 * half)
    rel = kc[None, :] - half - qi[:, None]
    bias = rel_table[t5_bucket(rel * dilation)].transpose(2, 0, 1).astype(jnp.float32)
    k_sub = (jnp.arange(nb)[:, None] - 1) * half + kc[None, :]
    valid = (jnp.abs(rel) <= half)[None] & ((k_sub >= 0) & (k_sub < n))[:, None, :]
    logits = jnp.einsum('brnqhc,brnkhc->brnhqk', qb, kb).astype(jnp.float32) + bias
    logits = jnp.where(valid[None, None, :, None], logits, NEG_INF)
    m = jnp.max(logits, axis=-1, keepdims=True)
    p = jnp.exp(logits - m)
    den = jnp.sum(p, axis=-1, keepdims=True)
    o = jnp.einsum('brnhqk,brnkhc->brnqhc', (p / den).astype(v.dtype), vb).astype(jnp.float32)
    lse = (m + jnp.log(den))[..., 0]

    def unstride(t):
        t = t[:, :, :n]
        return jnp.swapaxes(t, 1, 2).reshape((B, S) + t.shape[3:])

    o = unstride(o.reshape(B, dilation, nb * half, H, Dh))
    lse = unstride(lse.transpose(0, 1, 2, 4, 3).reshape(B, dilation, nb * half, H))
    return o, lse


def dilated_mixture(q, k, v, rel_table):
    outs, lses = [], []
    for window, dilation in DILATED_PATTERNS:
        o, lse = dilated_branch(q, k, v, rel_table, dilation, window // (2 * dilation))
        outs.append(o)
        lses.append(lse)
    w = jax.nn.softmax(jnp.stack(lses), axis=0)
    return jnp.einsum('pbsh,pbshc->bshc', w, jnp.stack(outs)).astype(q.dtype)


def expert_choice_moe(x, w_router, w_gate, w_up, w_down):
    B, S, D = x.shape
    cap = max(1, CAPACITY_FACTOR * S // N_EXPERTS)
    logits = jnp.einsum('bsd,de->bse', x, w_router).astype(jnp.float32)
    affinity = jax.nn.softmax(logits, axis=-1)
    g, idx = lax.top_k(jnp.swapaxes(affinity, 1, 2), cap)
    bidx = jnp.arange(B)[:, None, None]
    xe = x[bidx, idx]
    h = (jax.nn.silu(jnp.einsum('becd,edf->becf', xe, w_gate))
         * jnp.einsum('becd,edf->becf', xe, w_up))
    y = jnp.einsum('becf,efd->becd', h, w_down) * g[..., None].astype(x.dtype)
    return jnp.zeros_like(x).at[bidx, idx].add(y)


def setup_inputs(seed: int = 0) -> dict:
    key = jax.random.key(seed)
    ks = jax.random.split(key, 19)
    n_even, n_odd = (DEPTH + 1) // 2, DEPTH // 2
    even_in = 2 * A_WIDTH + 3 * B_QK_WIDTH + 2 * B_WIDTH
    even_out = A_WIDTH + B_WIDTH

    def nrm(k, shape, scale):
        return jax.random.normal(k, shape, jnp.float32) * scale

    return {
        'x': nrm(ks[0], (BATCH, SEQ, D_MODEL), 1.0),
        'mix_norm': 1.0 + nrm(ks[1], (DEPTH, D_MODEL), 0.05),
        'ffn_norm': 1.0 + nrm(ks[2], (DEPTH, D_MODEL), 0.05),
        'final_norm': 1.0 + nrm(ks[3], (D_MODEL,), 0.05),
        'w_in_even': nrm(ks[4], (n_even, D_MODEL, even_in), D_MODEL ** -0.5),
        'w_out_even': nrm(ks[5], (n_even, even_out, D_MODEL), even_out ** -0.5),
        'a_ln_g': 1.0 + nrm(ks[6], (n_even, A_WIDTH), 0.05),
        'a_ln_b': nrm(ks[7], (n_even, A_WIDTH), 0.02),
        'a_w_s': nrm(ks[8], (n_even, A_GROUPS, A_CHUNK, A_CHUNK), A_CHUNK ** -0.5),
        'a_b_s': 1.0 + nrm(ks[9], (n_even, A_GROUPS, A_CHUNK), 0.1),
        'b_lb_table': nrm(ks[10], (2, DEPTH + 1, B_QK_WIDTH), 0.1),
        'b_norm_g': 1.0 + nrm(ks[11], (n_even, B_WIDTH), 0.05),
        'w_qkv_odd': nrm(ks[12], (n_odd, D_MODEL, 3 * C_WIDTH), D_MODEL ** -0.5),
        'w_o_odd': nrm(ks[13], (n_odd, C_WIDTH, D_MODEL), C_WIDTH ** -0.5),
        'rel_bias': nrm(ks[14], (REL_BUCKETS, C_HEADS), 0.5),
        'w_router': nrm(ks[15], (DEPTH, D_MODEL, N_EXPERTS), D_MODEL ** -0.5),
        'w_gate': nrm(ks[16], (DEPTH, N_EXPERTS, D_MODEL, EXPERT_D_FF), D_MODEL ** -0.5),
        'w_up': nrm(ks[17], (DEPTH, N_EXPERTS, D_MODEL, EXPERT_D_FF), D_MODEL ** -0.5),
        'w_down': nrm(ks[18], (DEPTH, N_EXPERTS, EXPERT_D_FF, D_MODEL), EXPERT_D_FF ** -0.5),
    }


def reference(x, mix_norm, ffn_norm, final_norm, w_in_even, w_out_even, a_ln_g, a_ln_b,
              a_w_s, a_b_s, b_lb_table, b_norm_g, w_qkv_odd, w_o_odd, rel_bias,
              w_router, w_gate, w_up, w_down):
    B, S, _ = x.shape
    sizes = (A_WIDTH, A_WIDTH, B_QK_WIDTH, B_QK_WIDTH, B_QK_WIDTH, B_WIDTH, B_WIDTH)
    cuts = [sum(sizes[:j]) for j in range(1, len(sizes))]
    lower_bounds = jnp.cumsum(jax.nn.softmax(b_lb_table.astype(jnp.float32), axis=1), axis=1)
    for layer in range(DEPTH):
        h = rmsnorm(x, mix_norm[layer])
        if layer % 2 == 0:
            e = layer // 2
            proj = jnp.einsum('bsd,dn->bsn', h, w_in_even[e])
            u, v, q_b, f_fwd, f_bwd, i_b, g_b = jnp.split(proj, cuts, axis=-1)
            a_out = chunked_spatial_gating(jax.nn.gelu(u), jax.nn.gelu(v), a_w_s[e], a_b_s[e],
                                           a_ln_g[e], a_ln_b[e])
            b_out = hgrn2_bidirectional(q_b, f_fwd, f_bwd, i_b, g_b, lower_bounds[0, layer],
                                        lower_bounds[1, layer], b_norm_g[e])
            mixed = jnp.einsum('bsn,nd->bsd', jnp.concatenate([a_out, b_out], axis=-1), w_out_even[e])
        else:
            o = layer // 2
            qkv = jnp.einsum('bsd,dn->bsn', h, w_qkv_odd[o]).reshape(B, S, 3, C_HEADS, C_HEAD_DIM)
            q = qkv[:, :, 0] * (C_HEAD_DIM ** -0.5)
            attn = dilated_mixture(q, qkv[:, :, 1], qkv[:, :, 2], rel_bias)
            mixed = jnp.einsum('bsn,nd->bsd', attn.reshape(B, S, C_WIDTH), w_o_odd[o])
        x = x + mixed
        x = x + expert_choice_moe(rmsnorm(x, ffn_norm[layer]), w_router[layer], w_gate[layer],
                                  w_up[layer], w_down[layer])
    return rmsnorm(x, final_norm)
```

```python
import numpy as np
from contextlib import ExitStack
import concourse.bass as bass
import concourse.mybir as mybir
from concourse.bass_utils import run_bass_kernel_spmd

F32 = mybir.dt.float32
BF16 = mybir.dt.bfloat16
I32 = mybir.dt.int32
AF = mybir.ActivationFunctionType
ALU = mybir.AluOpType
AX = mybir.AxisListType

S = 8192
D = 1024
T = 1024
NB = S // T
NT = T // 128
NCH = T // 64
EPS = 1e-6
NE = 16
CAP = 1024


class KB:
    def __init__(self):
        self.nc = bass.Bass("TRN2", target_bir_lowering=False)
        nc = self.nc
        self.es = ExitStack()
        self.E = dict(pe=nc.tensor, act=nc.scalar, dve=nc.vector, pool=nc.gpsimd, sp=nc.sync)
        self.sem = {k: self.es.enter_context(nc.semaphore("sem_" + k)) for k in self.E}
        self.cnt = dict.fromkeys(self.E, 0)
        self.waited = {}
        self.lastw = {}
        self.readers = {}
        self.dq = {}
        for q in ("sp", "pool", "act"):
            sems = [self.es.enter_context(nc.semaphore(f"dq_{q}_{i}")) for i in range(12)]
            self.dq[q] = dict(sems=sems, vals=[0] * 12, i=0)
        self.nps = 0

    def sb(self, st, name, shape, dt):
        self.nps += 1
        return st.enter_context(self.nc.sbuf_tensor(f"sb{self.nps}_{name}", list(shape), dt))

    def ps(self, st, name, shape, dt=F32):
        self.nps += 1
        return st.enter_context(self.nc.psum_tensor(f"ps{self.nps}_{name}", list(shape), dt))

    def _wait(self, eng, h):
        sem, name, val, heng = h
        k = (eng, name)
        if self.waited.get(k, 0) >= val:
            return
        self.E[eng].wait_ge(sem, val)
        self.waited[k] = val

    def _deps(self, eng, r, w, is_dma=False):
        out = []
        for k in r:
            h = self.lastw.get(k)
            if h is not None:
                if is_dma or h[3] != eng or eng != "pe":
                    out.append(h)
        for k in w:
            h = self.lastw.get(k)
            if h is not None and (is_dma or h[3] != eng):
                out.append(h)
            for h in self.readers.get(k, ()):
                if is_dma or h[3] != eng:
                    out.append(h)
        return out

    def _record(self, h, r, w):
        for k in r:
            lst = self.readers.setdefault(k, [])
            if h[3] is not None:
                lst[:] = [x for x in lst if x[3] != h[3]]
            lst.append(h)
            if len(lst) > 24:
                del lst[0]
        for k in w:
            self.lastw[k] = h
            self.readers[k] = []

    def op(self, eng, fn, r=(), w=()):
        for h in self._deps(eng, r, w):
            self._wait(eng, h)
        ins = fn(self.E[eng])
        self.cnt[eng] += 1
        ins.then_inc(self.sem[eng], 1)
        h = (self.sem[eng], "sem_" + eng, self.cnt[eng], eng)
        self._record(h, r, w)
        return h

    def dma(self, q, out, in_, r=(), w=(), **kw):
        for h in self._deps(q, r, w, is_dma=True):
            self._wait(q, h)
        d = self.dq[q]
        i = d["i"]
        d["i"] = (i + 1) % len(d["sems"])
        sem = d["sems"][i]
        name = f"dq_{q}_{i}"
        if d["vals"][i] > 0:
            self._wait(q, (sem, name, d["vals"][i], None))
        ins = self.E[q].dma_start(out=out, in_=in_, **kw)
        ins.then_inc(sem, 16)
        d["vals"][i] += 16
        h = (sem, name, d["vals"][i], None)
        self._record(h, r, w)
        return h

    def idma(self, out, in_, out_offset=None, in_offset=None, r=(), w=(), **kw):
        q = "pool"
        for h in self._deps(q, r, w, is_dma=True):
            self._wait(q, h)
        d = self.dq[q]
        i = d["i"]
        d["i"] = (i + 1) % len(d["sems"])
        sem = d["sems"][i]
        name = f"dq_{q}_{i}"
        if d["vals"][i] > 0:
            self._wait(q, (sem, name, d["vals"][i], None))
        ins = self.nc.gpsimd.indirect_dma_start(out=out, out_offset=out_offset, in_=in_, in_offset=in_offset, **kw)
        ins.then_inc(sem, 16)
        d["vals"][i] += 16
        h = (sem, name, d["vals"][i], None)
        self._record(h, r, w)
        return h

    def barrier(self):
        hs = [(self.sem[e], "sem_" + e, self.cnt[e], e) for e in self.E if self.cnt[e] > 0]
        for q, d in self.dq.items():
            for i, s in enumerate(d["sems"]):
                if d["vals"][i] > 0:
                    hs.append((s, f"dq_{q}_{i}", d["vals"][i], None))
        for eng in self.E:
            for h in hs:
                if h[3] != eng:
                    self._wait(eng, h)
        self.lastw.clear()
        self.readers.clear()


def build_program(stage=99):
    kb = KB()
    nc = kb.nc
    op, dma = kb.op, kb.dma

    def dram_in(name, shape, dt=F32):
        return nc.dram_tensor(name, list(shape), dt, kind="ExternalInput").ap()

    x_in = dram_in("x", [S, D])
    gains = dram_in("gains", [5, 128, D])
    w_in = dram_in("w_in", [D, 3584])
    w_out = dram_in("w_out", [D, D])
    a_ln = dram_in("a_ln", [2, 128, 512])
    a_wsT = dram_in("a_wsT", [128, 4, 128])
    a_bs = dram_in("a_bs", [128, 512])
    lbt = dram_in("lbt", [128, 4, 2, 3])
    b_ng = dram_in("b_ng", [64, 512])
    ident_f = dram_in("ident_f", [128, 128])
    rmask_in = dram_in("rmask", [128, T])
    tri_in = dram_in("tri", [64, 2, 64])
    ones_in = dram_in("ones_f", [128, 128])
    lstrict_in = dram_in("lstrict", [128, 128])
    iota_in = dram_in("iota_s", [128, CAP])
    rmask2_in = dram_in("rmask2", [128, NE * 64])
    tokdig_in = dram_in("tokdig", [128, 64, 2])
    w_router = dram_in("w_router", [2, D, NE])
    if stage in (104, 141, 142, 143, 144, 145, 40, 41):
        w_gate = w_up = w_down = None
    else:
        w_gate = dram_in("w_gate", [2, NE, D, D])
        w_up = dram_in("w_up", [2, NE, D, D])
        w_down = dram_in("w_down", [2, NE, D, D])
    rel_bias = dram_in("rel_bias", [32, 16])
    gpat_in = dram_in("gpat", [33, 3, 383])
    w_qkv = dram_in("w_qkv", [D, 3072])
    w_o = dram_in("w_o", [D, D])
    out_d = nc.dram_tensor("out", [S, D], F32, kind="ExternalOutput").ap()
    vecbuf = nc.dram_tensor("vecbuf", [3, 16, 383], F32, kind="Internal").ap()
    dk = "ExternalOutput" if stage in (143, 144, 145) else "Internal"
    if stage == 143:
        dbg_s = nc.dram_tensor("dbg_s", [128, 256], F32, kind="ExternalOutput").ap()
        dbg_p = nc.dram_tensor("dbg_p", [128, 256], BF16, kind="ExternalOutput").ap()
        dbg_v = nc.dram_tensor("dbg_v", [128, 2, 65], BF16, kind="ExternalOutput").ap()
        dbg_k = nc.dram_tensor("dbg_k", [64, 2, 128], BF16, kind="ExternalOutput").ap()
        dbg_q = nc.dram_tensor("dbg_q", [64, 128], BF16, kind="ExternalOutput").ap()
        dbg_o = nc.dram_tensor("dbg_o", [128, 8, 65], F32, kind="ExternalOutput").ap()
        dbg_a = nc.dram_tensor("dbg_a", [128, 8, 65], F32, kind="ExternalOutput").ap()
    qTbuf = nc.dram_tensor("qTbuf", [8, 128, S], BF16, kind=dk).ap()
    kTbuf = nc.dram_tensor("kTbuf", [8, 128, S + 2048], BF16, kind=dk).ap()
    vbuf = nc.dram_tensor("vbuf", [S + 2 * 1088, 16, 65], BF16, kind=dk).ap()
    accbuf = nc.dram_tensor("accbuf", [3, S, 16, 65], F32, kind="Internal").ap()
    h2buf = nc.dram_tensor("h2buf", [S, D], BF16, kind="Internal").ap()
    xbuf = nc.dram_tensor("xbuf", [S, D], F32, kind="Internal").ap()
    hTbuf = nc.dram_tensor("hTbuf", [NB, 128, 8, T], BF16, kind="Internal").ap()

    G = ExitStack()
    identb = kb.sb(G, "identb", [128, 128], BF16)
    identf = kb.sb(G, "identf", [128, 128], F32)
    epsc = kb.sb(G, "epsc", [128, 1], F32)
    gain_sb = kb.sb(G, "gain_sb", [128, D], F32)
    pdb = [kb.ps(G, f"pdb{i}", [128, 1024], F32) for i in range(3)]
    pst = [pdb[i // 2][:, (i % 2) * 512:(i % 2 + 1) * 512] for i in range(6)]
    psb = [kb.ps(G, f"psb{i}", [128, 1024], BF16) for i in range(2)]
    psi = [0]
    pbi = [0]

    def next_ps():
        i = psi[0]
        psi[0] = (i + 1) % len(pst)
        return pst[i], ("pst", i)

    def next_pb():
        i = pbi[0]
        pbi[0] = (i + 1) % len(psb)
        return psb[i], ("psb", i)

    dma("sp", identf[:], ident_f[:, :], w=["identf"])
    op("dve", lambda e: e.tensor_copy(out=identb[:], in_=identf[:]), r=["identf"], w=["identb"])
    op("dve", lambda e: e.memset(epsc[:], EPS), w=["epsc"])

    def load_gain(i):
        dma("sp", gain_sb[:], gains[i, :, :], w=["gain"])

    def rms_tile(st_tag, xt, hb, junk, ss, rs, keys_x):
        op("act", lambda e: e.activation(out=junk[:], in_=xt, func=AF.Square, accum_out=ss[:]),
           r=keys_x, w=[st_tag + "junk", st_tag + "ss"])
        op("act", lambda e: e.activation(out=rs[:], in_=ss[:], func=AF.Sqrt, bias=epsc[:], scale=1.0 / D),
           r=[st_tag + "ss", "epsc"], w=[st_tag + "rs"])
        op("dve", lambda e: e.reciprocal(out=rs[:], in_=rs[:]), r=[st_tag + "rs"], w=[st_tag + "rs"])
        op("dve", lambda e: e.scalar_tensor_tensor(out=hb[:], in0=xt, scalar=rs[:, 0:1], in1=gain_sb[:],
                                                   op0=ALU.mult, op1=ALU.mult),
           r=keys_x + [st_tag + "rs", "gain"], w=[st_tag + "hb"])

    def phase_m0():
        P = ExitStack()
        lb_sb = kb.sb(P, "lb_sb", [128, 4, 2, 3], F32)
        lb_e = kb.sb(P, "lb_e", [128, 4, 2, 3], F32)
        lb_s = kb.sb(P, "lb_s", [128, 4, 2], F32)
        lbv = kb.sb(P, "lbv", [128, 4, 2], F32)
        oml = kb.sb(P, "oml", [128, 4, 2], F32)
        rmask = kb.sb(P, "rmask", [128, T], F32)
        tri = kb.sb(P, "tri", [64, 2, 64], F32)
        wsT = kb.sb(P, "wsT", [128, 4, 128], BF16)
        lng = kb.sb(P, "lng", [128, 512], F32)
        lnb = kb.sb(P, "lnb", [128, 512], F32)
        bsf = kb.sb(P, "bsf", [128, 512], F32)
        bng = kb.sb(P, "bng", [64, 512], F32)
        Sin_b = kb.sb(P, "Sin_b", [128, NB, 4, 128], F32)
        Bb = kb.sb(P, "Bb", [128, NB, 4, 128], F32)
        Ab = kb.sb(P, "Ab", [128, NB, 4], F32)
        Sf = kb.sb(P, "Sf", [128, 4, 128], BF16)
        hT = kb.sb(P, "hT", [128, 8, T], BF16)
        load_gain(0)
        dma("sp", lb_sb[:], lbt[:, :, :, :], w=["lb_sb"])
        dma("sp", rmask[:], rmask_in[:, :], w=["rmask"])
        dma("sp", tri[:], tri_in[:, :, :], w=["tri"])
        dma("pool", wsT[:], a_wsT[:, :, :], w=["wsT"])
        dma("sp", lng[:], a_ln[0, :, :], w=["lng"])
        dma("sp", lnb[:], a_ln[1, :, :], w=["lnb"])
        dma("sp", bsf[:], a_bs[:, :], w=["bsf"])
        dma("sp", bng[:], b_ng[:, :], w=["bng"])
        op("act", lambda e: e.activation(out=lb_e[:], in_=lb_sb[:], func=AF.Exp), r=["lb_sb"], w=["lb_e"])
        op("dve", lambda e: e.tensor_reduce(out=lb_s[:], in_=lb_e[:], axis=AX.X, op=ALU.add), r=["lb_e"], w=["lb_s"])
        op("dve", lambda e: e.reciprocal(out=lb_s[:], in_=lb_s[:]), r=["lb_s"], w=["lb_s"])
        op("dve", lambda e: e.tensor_tensor(out=lbv[:], in0=lb_e[:, :, :, 0], in1=lb_s[:], op=ALU.mult),
           r=["lb_e", "lb_s"], w=["lbv"])
        op("dve", lambda e: e.tensor_scalar(out=oml[:], in0=lbv[:], scalar1=-1.0, scalar2=1.0, op0=ALU.mult, op1=ALU.add),
           r=["lbv"], w=["oml"])

        def make_hT(blk, src):
            with ExitStack() as L:
                xts = [kb.sb(L, f"m0x{i}", [128, D], F32) for i in range(2)]
                hbs = [kb.sb(L, f"m0h{i}", [128, D], BF16) for i in range(2)]
                junk = kb.sb(L, "m0junk", [128, D], BF16)
                sss = [kb.sb(L, f"m0ss{i}", [128, 1], F32) for i in range(2)]
                rss = [kb.sb(L, f"m0rs{i}", [128, 1], F32) for i in range(2)]
                for t in range(NT):
                    i = t % 2
                    tag = f"mh{i}"
                    r0 = blk * T + t * 128
                    dma("sp", xts[i][:], src[r0:r0 + 128, :], w=[tag + "x"])
                    rms_tile(tag, xts[i][:], hbs[i], junk, sss[i], rss[i], [tag + "x"])
                    pb, pk = next_pb()
                    for c in range(8):
                        op("pe", lambda e, c=c: e.transpose(out=pb[:, c * 128:(c + 1) * 128],
                                                            in_=hbs[i][:, c * 128:(c + 1) * 128], identity=identb[:]),
                           r=[tag + "hb", "identb"], w=[pk])
                    op("act", lambda e: e.copy(out=hT[:, :, t * 128:(t + 1) * 128],
                                               in_=pb[:, :].rearrange("p (c t) -> p c t", c=8)),
                       r=[pk], w=[("hT", t)])
                kb.barrier()

        def load_w(wt, col0, ncol, key):
            dma("pool", wt, w_in.rearrange("(c p) n -> p c n", p=128)[:, :, col0:col0 + ncol], w=[key])

        def proj_fm(wt, wkey, j, dst_fn):
            for tb in range(T // 512):
                ps, pk = next_ps()
                for c in range(8):
                    op("pe", lambda e, c=c: e.matmul(ps[:, :], lhsT=wt[:, c, j, :], rhs=hT[:, c, tb * 512:(tb + 1) * 512],
                                                      start=(c == 0), stop=(c == 7)),
                       r=[wkey] + [("hT", t) for t in range(tb * 4, tb * 4 + 4)], w=[pk])
                dst_fn(tb, ps, pk)

        def hgrn_prep(L, wt, wkey, h, d, qf, need_q, bufs):
            fb, lb_, eb = bufs["fb"], bufs["lb"], bufs["eb"]
            Qt, Kt, eend = bufs["Qt"][d], bufs["Kt"][d], bufs["eend"][d]
            tag = f"hp{d}"

            def evac_sig(tb, ps, pk):
                op("act", lambda e: e.activation(out=fb[:, tb * 512:(tb + 1) * 512], in_=ps[:, :], func=AF.Sigmoid),
                   r=[pk], w=[("fb", tb)])
            proj_fm(wt, wkey, 1 + d, evac_sig)
            fbk = [("fb", tb) for tb in range(4)]
            op("dve", lambda e: e.tensor_scalar(out=fb[:], in0=fb[:], scalar1=oml[:, h, d:d + 1], scalar2=lbv[:, h, d:d + 1],
                                                op0=ALU.mult, op1=ALU.add), r=fbk + ["oml", "lbv"], w=fbk)
            op("act", lambda e: e.activation(out=lb_[:], in_=fb[:], func=AF.Ln), r=fbk, w=["lb"])
            op("dve", lambda e: e.tensor_scalar(out=fb[:], in0=fb[:], scalar1=-1.0, scalar2=1.0, op0=ALU.mult, op1=ALU.add),
               r=fbk, w=fbk)
            op("dve", lambda e: e.tensor_tensor_scan(out=eb[:], data0=rmask[:], data1=lb_[:], initial=0.0,
                                                     op0=ALU.mult, op1=ALU.add), r=["rmask", "lb"], w=["eb"])
            eb3 = eb[:, :].rearrange("p (c j) -> p c j", j=64)
            if d == 1:
                op("dve", lambda e: e.tensor_tensor(out=lb_[:], in0=lb_[:], in1=eb[:], op=ALU.subtract), r=["lb", "eb"], w=["lb"])
                op("dve", lambda e: e.tensor_copy(out=eend[:], in_=eb3[:, :, 63]), r=["eb"], w=[tag + "eend"])
                op("dve", lambda e: e.tensor_tensor(out=eb3, in0=lb_[:, :].rearrange("p (c j) -> p c j", j=64),
                                                    in1=eend[:, :].unsqueeze(2).to_broadcast([128, NCH, 64]), op=ALU.add),
                   r=["lb", tag + "eend"], w=["eb"])
            col = 63 if d == 0 else 0
            op("dve", lambda e: e.tensor_reduce(out=bufs["ltot"][:], in_=eb3[:, :, col], axis=AX.X, op=ALU.add),
               r=["eb"], w=["ltot"])
            op("act", lambda e: e.activation(out=lb_[:], in_=eb[:], func=AF.Exp, scale=-1.0), r=["eb"], w=["lb"])
            op("dve", lambda e: e.tensor_tensor(out=Kt[:], in0=fb[:], in1=lb_[:], op=ALU.mult), r=fbk + ["lb"], w=[tag + "Kt"])
            op("act", lambda e: e.activation(out=lb_[:], in_=eb[:], func=AF.Exp), r=["eb"], w=["lb"])
            lb3 = lb_[:, :].rearrange("p (c j) -> p c j", j=64)
            op("dve", lambda e: e.tensor_copy(out=eend[:], in_=lb3[:, :, col]), r=["lb"], w=[tag + "eend"])
            if need_q:
                op("dve", lambda e: e.tensor_tensor(out=Qt[:], in0=qf[:], in1=lb_[:], op=ALU.mult), r=["qf", "lb"], w=[tag + "Qt"])

        def proj_tok(wt, wkey, j, dst, dkey, func):
            for g4 in range(NCH // 4):
                ps, pk = next_ps()
                for cc in range(4):
                    ch = g4 * 4 + cc
                    for c in range(8):
                        op("pe", lambda e, c=c, cc=cc, ch=ch: e.matmul(ps[0:64, cc * 128:(cc + 1) * 128],
                                                                       lhsT=hT[:, c, ch * 64:(ch + 1) * 64], rhs=wt[:, c, j, :],
                                                                       start=(c == 0), stop=(c == 7)),
                           r=[wkey, ("hT", ch // 2)], w=[pk])
                op("act", lambda e: e.activation(out=dst[:, g4 * 4:(g4 + 1) * 4, :],
                                                 in_=ps[0:64, :].rearrange("p (c v) -> p c v", c=4), func=func),
                   r=[pk], w=[(dkey, g4)])

        def hgrn_scan(d, Qt, Kt, eend, vt, St, o_acc, first, emit_o, tag):
            order = range(NCH) if d == 0 else range(NCH - 1, -1, -1)
            for ch in order:
                cs = slice(ch * 64, (ch + 1) * 64)
                pb, pbk = next_pb()
                op("pe", lambda e: e.transpose(out=pb[0:64, 0:128], in_=Kt[:, cs], identity=identb[:]),
                   r=[tag + "Kt", "identb"], w=[pbk])
                ktk = kt_tok[ch % 2]
                op("act", lambda e: e.copy(out=ktk[:], in_=pb[0:64, 0:128]), r=[pbk], w=[("ktk", ch % 2)])
                if emit_o:
                    ps, pk = next_ps()
                    op("pe", lambda e: e.matmul(ps[0:64, 0:64], lhsT=Kt[:, cs], rhs=Qt[:, cs], start=True, stop=True),
                       r=[tag + "Kt", tag + "Qt"], w=[pk])
                    sT = sc_sb[ch % 2]
                    op("dve", lambda e: e.tensor_tensor(out=sT[:], in0=ps[0:64, 0:64], in1=tri[:, d, :], op=ALU.mult),
                       r=[pk, "tri"], w=[("scT", ch % 2)])
                    ps2, pk2 = next_ps()
                    op("pe", lambda e: e.matmul(ps2[0:64, 0:128], lhsT=sT[:], rhs=vt[:, ch, :], start=True, stop=False),
                       r=[("scT", ch % 2), ("vt", ch // 4)], w=[pk2])
                    op("pe", lambda e: e.matmul(ps2[0:64, 0:128], lhsT=Qt[:, cs], rhs=St[:], start=False, stop=True),
                       r=[tag + "Qt", tag + "St"], w=[pk2])
                    if first:
                        op("act", lambda e: e.copy(out=o_acc[:, ch, :], in_=ps2[0:64, 0:128]), r=[pk2], w=[("oacc", ch)])
                    else:
                        op("dve", lambda e: e.tensor_tensor(out=o_acc[:, ch, :], in0=ps2[0:64, 0:128], in1=o_acc[:, ch, :],
                                                            op=ALU.add), r=[pk2, ("oacc", ch)], w=[("oacc", ch)])
                ps3, pk3 = next_ps()
                op("pe", lambda e: e.matmul(ps3[:, 0:128], lhsT=identb[:], rhs=St[:], start=True, stop=False),
                   r=["identb", tag + "St"], w=[pk3])
                op("pe", lambda e: e.matmul(ps3[:, 0:128], lhsT=ktk[:], rhs=vt[:, ch, :], start=False, stop=True),
                   r=[("ktk", ch % 2), ("vt", ch // 4)], w=[pk3])
                op("act", lambda e: e.activation(out=St[:], in_=ps3[:, 0:128], func=AF.Copy, scale=eend[:, ch:ch + 1]),
                   r=[pk3, tag + "eend"], w=[tag + "St"])

        if stage >= 1:
            for blk in range(NB):
                make_hT(blk, x_in)
                dma("pool", hTbuf[blk], hT[:], r=[("hT", t) for t in range(NT)])
                with ExitStack() as L:
                    bufs = dict(fb=kb.sb(L, "fb", [128, T], F32), lb=kb.sb(L, "lbuf", [128, T], F32),
                                eb=kb.sb(L, "eb", [128, T], F32),
                                Qt=[None, kb.sb(L, "Qtb", [128, T], BF16)], Kt=[None, kb.sb(L, "Ktb", [128, T], BF16)],
                                eend=[None, kb.sb(L, "eendb", [128, NCH], F32)], ltot=kb.sb(L, "ltotA", [128, 1], F32))
                    wts = [kb.sb(L, f"whA{i}", [128, 8, 5, 128], BF16) for i in range(2)]
                    vt = kb.sb(L, "vtA", [64, NCH, 128], BF16)
                    kt_tok = [kb.sb(L, f"ktkA{i}", [64, 128], BF16) for i in range(2)]
                    sc_sb = None
                    StA = kb.sb(L, "StA", [128, 128], BF16)
                    for h in range(4):
                        wt = wts[h % 2]
                        wkey = ("whA", h % 2)
                        for j, c0 in ((2, 2048), (3, 2560)):
                            dma("pool", wt[:, :, j, :], w_in.rearrange("(c p) n -> p c n", p=128)[:, :, c0 + h * 128:c0 + (h + 1) * 128],
                                w=[wkey])
                        proj_tok(wt, wkey, 3, vt, "vt", AF.Copy)
                        hgrn_prep(L, wt, wkey, h, 1, None, False, bufs)
                        op("dve", lambda e: e.memset(StA[:], 0.0), w=["hp1St"])
                        hgrn_scan(1, None, bufs["Kt"][1], bufs["eend"][1], vt, StA, None, False, False, "hp1")
                        op("act", lambda e: e.copy(out=Bb[:, blk, h, :], in_=StA[:]), r=["hp1St"], w=[("Bb", blk, h)])
                        op("act", lambda e: e.activation(out=Ab[:, blk, h:h + 1], in_=bufs["ltot"][:], func=AF.Exp),
                           r=["ltot"], w=[("Ab", blk, h)])
                    kb.barrier()
            op("dve", lambda e: e.memset(Sin_b[:, NB - 1, :, :], 0.0), w=[("Sin", NB - 1)])
            for blk in range(NB - 2, -1, -1):
                for h in range(4):
                    op("dve", lambda e, blk=blk, h=h: e.scalar_tensor_tensor(
                        out=Sin_b[:, blk, h, :], in0=Sin_b[:, blk + 1, h, :], scalar=Ab[:, blk + 1, h:h + 1],
                        in1=Bb[:, blk + 1, h, :], op0=ALU.mult, op1=ALU.add),
                        r=[("Sin", blk + 1), ("Ab", blk + 1, h), ("Bb", blk + 1, h)], w=[("Sin", blk)])
            kb.barrier()

        if stage >= 2:
            op("dve", lambda e: e.memset(Sf[:], 0.0), w=["Sf"])
            for blk in range(NB):
                dma("sp", hT[:], hTbuf[blk], w=[("hT", t) for t in range(NT)])
                with ExitStack() as LB:
                    catT = kb.sb(LB, "catT", [128, 8, T], BF16)
                    with ExitStack() as L:
                        wuv = kb.sb(L, "wuv", [128, 8, 1024], BF16)
                        load_w(wuv[:, :, 0:512], 0, 512, "wu")
                        load_w(wuv[:, :, 512:1024], 512, 512, "wv")
                        gus = [kb.sb(L, f"gu{i}", [128, 512], F32) for i in range(2)]
                        gvs = [kb.sb(L, f"gv{i}", [128, 512], F32) for i in range(2)]
                        vnb = [kb.sb(L, f"vnb{i}", [128, 512], BF16) for i in range(2)]
                        aob = [kb.sb(L, f"aob{i}", [128, 512], BF16) for i in range(2)]
                        st6 = [kb.sb(L, f"st6{i}", [128, 6], F32) for i in range(2)]
                        mv = [kb.sb(L, f"mv{i}", [128, 2], F32) for i in range(2)]
                        for t in range(NT):
                            i = t % 2
                            ts = slice(t * 128, (t + 1) * 128)
                            psu, pku = next_ps()
                            psv, pkv = next_ps()
                            for c in range(8):
                                op("pe", lambda e, c=c: e.matmul(psu[:, :], lhsT=hT[:, c, ts], rhs=wuv[:, c, 0:512],
                                                                  start=(c == 0), stop=(c == 7)), r=[("hT", t), "wu"], w=[pku])
                            for c in range(8):
                                op("pe", lambda e, c=c: e.matmul(psv[:, :], lhsT=hT[:, c, ts], rhs=wuv[:, c, 512:1024],
                                                                  start=(c == 0), stop=(c == 7)), r=[("hT", t), "wv"], w=[pkv])
                            op("act", lambda e: e.activation(out=gus[i][:], in_=psu[:, :], func=AF.Gelu_apprx_tanh), r=[pku], w=[("gu", i)])
                            op("act", lambda e: e.activation(out=gvs[i][:], in_=psv[:, :], func=AF.Gelu_apprx_tanh), r=[pkv], w=[("gv", i)])
                            op("dve", lambda e: e.bn_stats(out=st6[i][:], in_=gvs[i][:]), r=[("gv", i)], w=[("st6", i)])
                            op("dve", lambda e: e.bn_aggr(out=mv[i][:], in_=st6[i][:]), r=[("st6", i)], w=[("mv", i)])
                            op("act", lambda e: e.activation(out=mv[i][:, 1:2], in_=mv[i][:, 1:2], func=AF.Sqrt, bias=epsc[:], scale=1.0),
                               r=[("mv", i), "epsc"], w=[("mv", i)])
                            op("dve", lambda e: e.reciprocal(out=mv[i][:, 1:2], in_=mv[i][:, 1:2]), r=[("mv", i)], w=[("mv", i)])
                            op("dve", lambda e: e.tensor_scalar(out=gvs[i][:], in0=gvs[i][:], scalar1=mv[i][:, 0:1], scalar2=mv[i][:, 1:2],
                                                                op0=ALU.subtract, op1=ALU.mult), r=[("gv", i), ("mv", i)], w=[("gv", i)])
                            op("dve", lambda e: e.tensor_tensor(out=gvs[i][:], in0=gvs[i][:], in1=lng[:], op=ALU.mult),
                               r=[("gv", i), "lng"], w=[("gv", i)])
                            op("dve", lambda e: e.tensor_tensor(out=vnb[i][:], in0=gvs[i][:], in1=lnb[:], op=ALU.add),
                               r=[("gv", i), "lnb"], w=[("vnb", i)])
                            psm, pkm = next_ps()
                            for g in range(4):
                                op("pe", lambda e, g=g: e.matmul(psm[:, g * 128:(g + 1) * 128], lhsT=wsT[:, g, :],
                                                                  rhs=vnb[i][:, g * 128:(g + 1) * 128], start=True, stop=True),
                                   r=["wsT", ("vnb", i)], w=[pkm])
                            op("dve", lambda e: e.tensor_tensor(out=gvs[i][:], in0=psm[:, :], in1=bsf[:], op=ALU.add),
                               r=[pkm, "bsf"], w=[("gv", i)])
                            op("dve", lambda e: e.tensor_tensor(out=aob[i][:], in0=gvs[i][:], in1=gus[i][:], op=ALU.mult),
                               r=[("gv", i), ("gu", i)], w=[("aob", i)])
                            pb, pbk = next_pb()
                            for g in range(4):
                                op("pe", lambda e, g=g: e.transpose(out=pb[:, g * 128:(g + 1) * 128], in_=aob[i][:, g * 128:(g + 1) * 128],
                                                                    identity=identb[:]), r=[("aob", i), "identb"], w=[pbk])
                            op("act", lambda e: e.copy(out=catT[:, 0:4, ts], in_=pb[:, 0:512].rearrange("p (c t) -> p c t", c=4)),
                               r=[pbk], w=[("catA", t)])
                        kb.barrier()
                    with ExitStack() as L:
                        bufs = dict(fb=kb.sb(L, "fbB", [128, T], F32), lb=kb.sb(L, "lbufB", [128, T], F32),
                                    eb=kb.sb(L, "ebB", [128, T], F32),
                                    Qt=[kb.sb(L, f"QtB{d}", [128, T], BF16) for d in range(2)],
                                    Kt=[kb.sb(L, f"KtB{d}", [128, T], BF16) for d in range(2)],
                                    eend=[kb.sb(L, f"eendB{d}", [128, NCH], F32) for d in range(2)], ltot=kb.sb(L, "ltotB", [128, 1], F32))
                        wts = [kb.sb(L, f"whB{i}", [128, 8, 5, 128], BF16) for i in range(2)]
                        qf = kb.sb(L, "qf", [128, T], F32)
                        vt = kb.sb(L, "vtB", [64, NCH, 128], BF16)
                        sg = kb.sb(L, "sgB", [64, NCH, 128], BF16)
                        kt_tok = [kb.sb(L, f"ktkB{i}", [64, 128], BF16) for i in range(2)]
                        sc_sb = [kb.sb(L, f"scB{i}", [64, 64], BF16) for i in range(2)]
                        o_acc = kb.sb(L, "o_acc", [64, NCH, 128], F32)
                        osq = kb.sb(L, "osq", [64, NCH, 128], F32)
                        ssq = kb.sb(L, "ssq", [64, NCH], F32)
                        obf = kb.sb(L, "obf", [64, NCH, 128], BF16)
                        Sb = kb.sb(L, "SbB", [128, 128], BF16)
                        Sb1 = kb.sb(L, "SbB1", [128, 128], BF16)
                        for h in range(4):
                            wt = wts[h % 2]
                            wkey = ("whB", h % 2)
                            for j in range(5):
                                c0 = 1024 + j * 512
                                dma("pool", wt[:, :, j, :], w_in.rearrange("(c p) n -> p c n", p=128)[:, :, c0 + h * 128:c0 + (h + 1) * 128],
                                    w=[wkey])

                            def evac_q(tb, ps, pk):
                                op("act", lambda e: e.activation(out=qf[:, tb * 512:(tb + 1) * 512], in_=ps[:, :], func=AF.Silu),
                                   r=[pk], w=["qf"])
                            proj_fm(wt, wkey, 0, evac_q)
                            proj_tok(wt, wkey, 3, vt, "vt", AF.Copy)
                            proj_tok(wt, wkey, 4, sg, "sg", AF.Sigmoid)
                            for d in range(2):
                                hgrn_prep(L, wt, wkey, h, d, qf, True, bufs)
                            op("act", lambda e: e.copy(out=Sb[:], in_=Sf[:, h, :]), r=["Sf"], w=["hp0St"])
                            hgrn_scan(0, bufs["Qt"][0], bufs["Kt"][0], bufs["eend"][0], vt, Sb, o_acc, True, True, "hp0")
                            op("act", lambda e: e.copy(out=Sf[:, h, :], in_=Sb[:]), r=["hp0St"], w=["Sf"])
                            op("act", lambda e: e.copy(out=Sb1[:], in_=Sin_b[:, blk, h, :]), r=[("Sin", blk)], w=["hp1St"])
                            hgrn_scan(1, bufs["Qt"][1], bufs["Kt"][1], bufs["eend"][1], vt, Sb1, o_acc, False, True, "hp1")
                            oak = [("oacc", ch) for ch in range(NCH)]
                            op("dve", lambda e: e.tensor_tensor(out=osq[:], in0=o_acc[:], in1=o_acc[:], op=ALU.mult), r=oak + ["hp1St"], w=["osq"])
                            op("dve", lambda e: e.tensor_reduce(out=ssq[:], in_=osq[:], axis=AX.X, op=ALU.add), r=["osq"], w=["ssq"])
                            op("act", lambda e: e.activation(out=ssq[:], in_=ssq[:], func=AF.Sqrt, bias=epsc[0:64, :], scale=1.0 / 128),
                               r=["ssq", "epsc"], w=["ssq"])
                            op("dve", lambda e: e.reciprocal(out=ssq[:], in_=ssq[:]), r=["ssq"], w=["ssq"])
                            op("dve", lambda e: e.tensor_tensor(out=osq[:], in0=o_acc[:], in1=ssq[:, :].unsqueeze(2).to_broadcast([64, NCH, 128]), op=ALU.mult),
                               r=oak + ["ssq"], w=["osq"])
                            op("dve", lambda e: e.tensor_tensor(out=osq[:], in0=osq[:],
                                                                in1=bng[:, h * 128:(h + 1) * 128].unsqueeze(1).to_broadcast([64, NCH, 128]),
                                                                op=ALU.mult), r=["osq", "bng"], w=["osq"])
                            op("dve", lambda e: e.tensor_tensor(out=obf[:], in0=osq[:], in1=sg[:], op=ALU.mult),
                               r=["osq"] + [("sg", g4) for g4 in range(NCH // 4)], w=["obf"])
                            for g8 in range(NCH // 8):
                                pb, pbk = next_pb()
                                for cc in range(8):
                                    ch = g8 * 8 + cc
                                    op("pe", lambda e, cc=cc, ch=ch: e.transpose(out=pb[:, cc * 64:(cc + 1) * 64], in_=obf[:, ch, :],
                                                                                  identity=identb[0:64, 0:64]), r=["obf", "identb"], w=[pbk])
                                op("act", lambda e: e.copy(out=catT[:, 4 + h, g8 * 512:(g8 + 1) * 512], in_=pb[:, 0:512]),
                                   r=[pbk], w=[("catB", h, g8)])
                        kb.barrier()
                    with ExitStack() as L:
                        wo = kb.sb(L, "wo", [128, 8, D], BF16)
                        dma("pool", wo[:], w_out.rearrange("(c p) n -> p c n", p=128), w=["wo"])
                        xts = [kb.sb(L, f"ox{i}", [128, D], F32) for i in range(2)]
                        for t in range(NT):
                            i = t % 2
                            ts = slice(t * 128, (t + 1) * 128)
                            r0 = blk * T + t * 128
                            dma("sp", xts[i][:], x_in[r0:r0 + 128, :], w=[("ox", i)])
                            for hf in range(2):
                                ps, pk = next_ps()
                                for c in range(8):
                                    op("pe", lambda e, c=c: e.matmul(ps[:, :], lhsT=catT[:, c, ts], rhs=wo[:, c, hf * 512:(hf + 1) * 512],
                                                                      start=(c == 0), stop=(c == 7)), r=["wo"], w=[pk])
                                op("dve", lambda e: e.tensor_tensor(out=xts[i][:, hf * 512:(hf + 1) * 512], in0=ps[:, :],
                                                                    in1=xts[i][:, hf * 512:(hf + 1) * 512], op=ALU.add),
                                   r=[pk, ("ox", i)], w=[("ox", i)])
                            dma("pool", xbuf[r0:r0 + 128, :], xts[i][:], r=[("ox", i)])
                        kb.barrier()
        P.close()
        kb.barrier()


    def phase_moe(layer):
        P = ExitStack()
        NTT = S // 128
        aff = kb.sb(P, "aff", [128, NE, NTT], F32)
        pos = kb.sb(P, "pos", [128, NE, NTT], F32)
        gsel = kb.sb(P, "gsel", [128, NE, NTT], F32)
        onesf = kb.sb(P, "onesf", [128, 128], F32)
        load_gain(1 + 2 * layer)
        dma("sp", onesf[:], ones_in[:, :], w=["onesf"])
        with ExitStack() as L:
            wr = kb.sb(L, "wr", [128, 8, NE], F32)
            dma("sp", wr[:], w_router[layer].rearrange("(c p) e -> p c e", p=128), w=["wr"])
            xts = [kb.sb(L, f"rx{i}", [128, D], F32) for i in range(2)]
            hfs = [kb.sb(L, f"rhf{i}", [128, D], F32) for i in range(2)]
            hbs = [kb.sb(L, f"rhb{i}", [128, D], BF16) for i in range(2)]
            hfT = [kb.sb(L, f"rhT{i}", [128, 8, 128], F32) for i in range(2)]
            junk = kb.sb(L, "rjunk", [128, D], BF16)
            sss = [kb.sb(L, f"rss{i}", [128, 1], F32) for i in range(2)]
            rss = [kb.sb(L, f"rrs{i}", [128, 1], F32) for i in range(2)]
            lg = [kb.sb(L, f"rlg{i}", [128, NE], F32) for i in range(2)]
            mx = [kb.sb(L, f"rmx{i}", [128, 1], F32) for i in range(2)]
            sm = [kb.sb(L, f"rsm{i}", [128, 1], F32) for i in range(2)]
            for t in range(NTT):
                i = t % 2
                tg = f"r{i}"
                dma("sp", xts[i][:], xbuf[t * 128:(t + 1) * 128, :], w=[tg + "x"])
                op("act", lambda e: e.activation(out=junk[:], in_=xts[i][:], func=AF.Square, accum_out=sss[i][:]),
                   r=[tg + "x"], w=[tg + "ss"])
                op("act", lambda e: e.activation(out=rss[i][:], in_=sss[i][:], func=AF.Sqrt, bias=epsc[:], scale=1.0 / D),
                   r=[tg + "ss", "epsc"], w=[tg + "rs"])
                op("dve", lambda e: e.reciprocal(out=rss[i][:], in_=rss[i][:]), r=[tg + "rs"], w=[tg + "rs"])
                op("dve", lambda e: e.scalar_tensor_tensor(out=hfs[i][:], in0=xts[i][:], scalar=rss[i][:, 0:1], in1=gain_sb[:],
                                                           op0=ALU.mult, op1=ALU.mult), r=[tg + "x", tg + "rs", "gain"], w=[tg + "hf"])
                op("act", lambda e: e.copy(out=hbs[i][:], in_=hfs[i][:]), r=[tg + "hf"], w=[tg + "hb"])
                dma("pool", h2buf[t * 128:(t + 1) * 128, :], hbs[i][:], r=[tg + "hb"])
                for hh in range(2):
                    ps, pk = next_ps()
                    for c4 in range(4):
                        c = hh * 4 + c4
                        op("pe", lambda e: e.transpose(out=ps[:, c4 * 128:(c4 + 1) * 128], in_=hfs[i][:, c * 128:(c + 1) * 128],
                                                       identity=identf[:]), r=[tg + "hf", "identf"], w=[pk])
                    op("act", lambda e: e.copy(out=hfT[i][:, hh * 4:(hh + 1) * 4, :], in_=ps[:, :].rearrange("p (c t) -> p c t", c=4)),
                       r=[pk], w=[tg + "hT"])
                ps, pk = next_ps()
                for c in range(8):
                    op("pe", lambda e: e.matmul(ps[:, 0:NE], lhsT=hfT[i][:, c, :], rhs=wr[:, c, :], start=(c == 0), stop=(c == 7)),
                       r=[tg + "hT", "wr"], w=[pk])
                op("dve", lambda e: e.tensor_reduce(out=mx[i][:], in_=ps[:, 0:NE], axis=AX.X, op=ALU.max), r=[pk], w=[tg + "mx"])
                op("dve", lambda e: e.tensor_scalar(out=mx[i][:], in0=mx[i][:], scalar1=-1.0, scalar2=None, op0=ALU.mult),
                   r=[tg + "mx"], w=[tg + "mx"])
                op("act", lambda e: e.activation(out=lg[i][:], in_=ps[:, 0:NE], func=AF.Exp, bias=mx[i][:], scale=1.0, accum_out=sm[i][:]),
                   r=[pk, tg + "mx"], w=[tg + "lg", tg + "sm"])
                op("dve", lambda e: e.reciprocal(out=sm[i][:], in_=sm[i][:]), r=[tg + "sm"], w=[tg + "sm"])
                op("dve", lambda e: e.tensor_scalar(out=aff[:, :, t], in0=lg[i][:], scalar1=sm[i][:, 0:1], scalar2=None, op0=ALU.mult),
                   r=[tg + "lg", tg + "sm"], w=["aff"])
            kb.barrier()
        if stage == 30:
            P.close()
            return
        with ExitStack() as L:
            lo = kb.sb(L, "bs_lo", [128, NE], F32)
            mid = kb.sb(L, "bs_mid", [128, NE], F32)
            msk = kb.sb(L, "bs_msk", [128, NE, NTT], F32)
            cnt = kb.sb(L, "bs_cnt", [128, NE], F32)
            cmp_ = kb.sb(L, "bs_cmp", [128, NE], F32)
            op("dve", lambda e: e.memset(lo[:], 0.0), w=["lo"])
            for it in range(30):
                wv = 2.0 ** (-(it + 1))
                op("dve", lambda e: e.tensor_scalar(out=mid[:], in0=lo[:], scalar1=wv, scalar2=None, op0=ALU.add), r=["lo"], w=["mid"])
                op("dve", lambda e: e.tensor_tensor(out=msk[:], in0=aff[:], in1=mid[:, :].unsqueeze(2).to_broadcast([128, NE, NTT]),
                                                    op=ALU.is_ge), r=["aff", "mid"], w=["msk"])
                op("dve", lambda e: e.tensor_reduce(out=cnt[:], in_=msk[:], axis=AX.X, op=ALU.add), r=["msk"], w=["cnt"])
                ps, pk = next_ps()
                op("pe", lambda e: e.matmul(ps[:, 0:NE], lhsT=onesf[:], rhs=cnt[:], start=True, stop=True), r=["onesf", "cnt"], w=[pk])
                op("dve", lambda e: e.tensor_scalar(out=cmp_[:], in0=ps[:, 0:NE], scalar1=CAP - 0.5, scalar2=None, op0=ALU.is_ge),
                   r=[pk], w=["cmp"])
                op("dve", lambda e: e.scalar_tensor_tensor(out=lo[:], in0=cmp_[:], scalar=wv, in1=lo[:], op0=ALU.mult, op1=ALU.add),
                   r=["cmp", "lo"], w=["lo"])
            lst = kb.sb(L, "lstrict", [128, 128], BF16)
            onesb = kb.sb(L, "onesb", [128, 128], BF16)
            rm2 = kb.sb(L, "rm2", [128, NE, NTT], F32)
            mskb = kb.sb(L, "mskb", [128, NE, NTT], BF16)
            wit = kb.sb(L, "wit", [128, NE, NTT], F32)
            tot = kb.sb(L, "tot", [128, NE, NTT], F32)
            dma("pool", lst[:], lstrict_in[:, :], w=["lst"])
            dma("sp", rm2[:], rmask2_in[:, :].rearrange("p (e t) -> p e t", e=NE), w=["rm2"])
            op("dve", lambda e: e.tensor_copy(out=onesb[:], in_=onesf[:]), r=["onesf"], w=["onesb"])
            op("dve", lambda e: e.tensor_tensor(out=msk[:], in0=aff[:], in1=lo[:, :].unsqueeze(2).to_broadcast([128, NE, NTT]),
                                                op=ALU.is_ge), r=["aff", "lo"], w=["msk"])
            op("dve", lambda e: e.tensor_tensor(out=gsel[:], in0=aff[:], in1=msk[:], op=ALU.mult), r=["aff", "msk"], w=["gsel"])
            op("dve", lambda e: e.tensor_copy(out=mskb[:], in_=msk[:]), r=["msk"], w=["mskb"])
            mflat = mskb[:, :, :].rearrange("p e t -> p (e t)")
            for hf2 in range(2):
                ps, pk = next_ps()
                op("pe", lambda e: e.matmul(ps[:, :], lhsT=lst[:], rhs=mflat[:, hf2 * 512:(hf2 + 1) * 512], start=True, stop=True),
                   r=["lst", "mskb"], w=[pk])
                op("act", lambda e: e.copy(out=wit[:, hf2 * 8:(hf2 + 1) * 8, :], in_=ps[:, :].rearrange("p (e t) -> p e t", e=8)),
                   r=[pk], w=["wit"])
                ps2, pk2 = next_ps()
                op("pe", lambda e: e.matmul(ps2[:, :], lhsT=onesb[:], rhs=mflat[:, hf2 * 512:(hf2 + 1) * 512], start=True, stop=True),
                   r=["onesb", "mskb"], w=[pk2])
                op("act", lambda e: e.copy(out=tot[:, hf2 * 8:(hf2 + 1) * 8, :], in_=ps2[:, :].rearrange("p (e t) -> p e t", e=8)),
                   r=[pk2], w=["tot"])
            op("dve", lambda e: e.tensor_tensor_scan(out=pos[:, :, :].rearrange("p e t -> p (e t)"),
                                                     data0=rm2[:, :, :].rearrange("p e t -> p (e t)"),
                                                     data1=tot[:, :, :].rearrange("p e t -> p (e t)"), initial=0.0,
                                                     op0=ALU.mult, op1=ALU.add), r=["rm2", "tot"], w=["pos"])
            op("dve", lambda e: e.tensor_tensor(out=pos[:], in0=pos[:], in1=tot[:], op=ALU.subtract), r=["pos", "tot"], w=["pos"])
            op("dve", lambda e: e.tensor_tensor(out=pos[:], in0=pos[:], in1=wit[:], op=ALU.add), r=["pos", "wit"], w=["pos"])
            op("dve", lambda e: e.scalar_tensor_tensor(out=pos[:], in0=pos[:], scalar=1.0, in1=msk[:], op0=ALU.add, op1=ALU.mult),
               r=["pos", "msk"], w=["pos"])
            op("dve", lambda e: e.tensor_scalar(out=pos[:], in0=pos[:], scalar1=-1.0, scalar2=None, op0=ALU.add), r=["pos"], w=["pos"])
            kb.barrier()
        with ExitStack() as L:
            iota = kb.sb(L, "iota", [128, CAP], F32)
            tokd = kb.sb(L, "tokd", [128, NTT, 2], F32)
            Eb = [kb.sb(L, f"Eb{i}", [128, CAP], BF16) for i in range(2)]
            rhs4 = [kb.sb(L, f"rhs4{i}", [128, NTT, 4], BF16) for i in range(2)]
            gtmp = kb.sb(L, "gtmp", [128, NTT], F32)
            sl4 = kb.sb(L, "sl4", [4, CAP], F32)
            slT = kb.sb(L, "slT", [128, 8, 4], F32)
            idxf = kb.sb(L, "idxf", [128, 8], F32)
            idxs = [kb.sb(L, f"idxi{i}", [128, 8], I32) for i in range(2)]
            gss = [kb.sb(L, f"gs{i}", [128, 8], F32) for i in range(2)]
            xe = kb.sb(L, "xe", [128, 8, D], BF16)
            xeT = kb.sb(L, "xeT", [128, 8, CAP], BF16)
            hTm = kb.sb(L, "hTm", [128, 8, CAP], BF16)
            sgt = [kb.sb(L, f"sgt{i}", [128, 512], F32) for i in range(2)]
            yts = [kb.sb(L, f"yt{i}", [128, D], F32) for i in range(2)]
            Wg = [kb.sb(L, f"Wg{i}", [128, 8, D], BF16) for i in range(2)]
            Wu = [kb.sb(L, f"Wu{i}", [128, 8, D], BF16) for i in range(2)]
            Wd = [kb.sb(L, f"Wd{i}", [128, 8, D], BF16) for i in range(2)]
            dma("sp", iota[:], iota_in[:, :], w=["iota"])
            dma("sp", tokd[:], tokdig_in[:, :, :], w=["tokd"])
            for i in range(2):
                op("dve", lambda e: e.tensor_copy(out=rhs4[i][:, :, 0:2], in_=tokd[:]), r=["tokd"], w=[("rhs4", i)])

            def load_expert(ex):
                i = ex % 2
                for nm, wt_, src in (("Wg", Wg[i], w_gate), ("Wu", Wu[i], w_up), ("Wd", Wd[i], w_down)):
                    for c2 in range(2):
                        dma("pool", wt_[:, c2 * 4:(c2 + 1) * 4, :],
                            src[layer, ex].rearrange("(c p) n -> p c n", p=128)[:, c2 * 4:(c2 + 1) * 4, :], w=[(nm, i)])

            load_expert(0)
            for ex in range(NE):
                i = ex % 2
                if ex + 1 < NE:
                    load_expert(ex + 1)
                op("dve", lambda e: e.tensor_copy(out=rhs4[i][:, :, 2], in_=gsel[:, ex, :]), r=["gsel"], w=[("rhs4", i)])
                op("dve", lambda e: e.tensor_tensor(out=gtmp[:], in0=gsel[:, ex, :], in1=rhs4[i][:, :, 2], op=ALU.subtract),
                   r=["gsel", ("rhs4", i)], w=["gtmp"])
                op("dve", lambda e: e.tensor_copy(out=rhs4[i][:, :, 3], in_=gtmp[:]), r=["gtmp"], w=[("rhs4", i)])
                psA, pkA = next_ps()
                psB, pkB = next_ps()
                for t in range(NTT):
                    j = t % 2
                    op("dve", lambda e: e.tensor_scalar(out=Eb[j][:], in0=iota[:], scalar1=pos[:, ex, t:t + 1], scalar2=None,
                                                        op0=ALU.is_equal), r=["iota", "pos"], w=[("Eb", j)])
                    op("pe", lambda e: e.matmul(psA[0:4, :], lhsT=rhs4[i][:, t, :], rhs=Eb[j][:, 0:512], start=(t == 0), stop=(t == NTT - 1)),
                       r=[("rhs4", i), ("Eb", j)], w=[pkA])
                    op("pe", lambda e: e.matmul(psB[0:4, :], lhsT=rhs4[i][:, t, :], rhs=Eb[j][:, 512:1024], start=(t == 0), stop=(t == NTT - 1)),
                       r=[("rhs4", i), ("Eb", j)], w=[pkB])
                op("act", lambda e: e.copy(out=sl4[:, 0:512], in_=psA[0:4, :]), r=[pkA], w=["sl4"])
                op("act", lambda e: e.copy(out=sl4[:, 512:1024], in_=psB[0:4, :]), r=[pkB], w=["sl4"])
                ps, pk = next_ps()
                for sbk in range(8):
                    op("pe", lambda e: e.transpose(out=ps[:, sbk * 4:(sbk + 1) * 4], in_=sl4[:, sbk * 128:(sbk + 1) * 128],
                                                   identity=identf[0:4, 0:4]), r=["sl4", "identf"], w=[pk])
                op("act", lambda e: e.copy(out=slT[:], in_=ps[:, 0:32].rearrange("p (s c) -> p s c", c=4)), r=[pk], w=["slT"])
                op("dve", lambda e: e.scalar_tensor_tensor(out=idxf[:], in0=slT[:, :, 0], scalar=128.0, in1=slT[:, :, 1],
                                                           op0=ALU.mult, op1=ALU.add), r=["slT"], w=["idxf"])
                op("dve", lambda e: e.tensor_copy(out=idxs[i][:], in_=idxf[:]), r=["idxf"], w=[("idx", i)])
                op("dve", lambda e: e.tensor_tensor(out=gss[i][:], in0=slT[:, :, 2], in1=slT[:, :, 3], op=ALU.add), r=["slT"], w=[("gs", i)])
                for sbk in range(8):
                    kb.idma(out=xe[:, sbk, :], in_=h2buf[:, :], in_offset=bass.IndirectOffsetOnAxis(ap=idxs[i][:, sbk:sbk + 1], axis=0),
                            r=[("idx", i)], w=[("xe", sbk)])
                for sbk in range(8):
                    pb, pbk = next_pb()
                    for c in range(8):
                        op("pe", lambda e: e.transpose(out=pb[:, c * 128:(c + 1) * 128], in_=xe[:, sbk, c * 128:(c + 1) * 128],
                                                       identity=identb[:]), r=[("xe", sbk), "identb"], w=[pbk])
                    op("act", lambda e: e.copy(out=xeT[:, :, sbk * 128:(sbk + 1) * 128], in_=pb[:, :].rearrange("p (c t) -> p c t", c=8)),
                       r=[pbk], w=[("xeT", sbk // 4)])
                for fb in range(8):
                    for sh in range(2):
                        psg, pkg = next_ps()
                        psu, pku = next_ps()
                        for c in range(8):
                            op("pe", lambda e: e.matmul(psg[:, :], lhsT=Wg[i][:, c, fb * 128:(fb + 1) * 128], rhs=xeT[:, c, sh * 512:(sh + 1) * 512],
                                                        start=(c == 0), stop=(c == 7)), r=[("Wg", i), ("xeT", sh)], w=[pkg])
                        for c in range(8):
                            op("pe", lambda e: e.matmul(psu[:, :], lhsT=Wu[i][:, c, fb * 128:(fb + 1) * 128], rhs=xeT[:, c, sh * 512:(sh + 1) * 512],
                                                        start=(c == 0), stop=(c == 7)), r=[("Wu", i), ("xeT", sh)], w=[pku])
                        k2 = (fb * 2 + sh) % 2
                        op("act", lambda e: e.activation(out=sgt[k2][:], in_=psg[:, :], func=AF.Silu), r=[pkg], w=[("sgt", k2)])
                        op("dve", lambda e: e.tensor_tensor(out=hTm[:, fb, sh * 512:(sh + 1) * 512], in0=psu[:, :], in1=sgt[k2][:], op=ALU.mult),
                           r=[pku, ("sgt", k2)], w=[("hTm", fb, sh)])
                for sbk in range(8):
                    k2 = sbk % 2
                    for dh in range(2):
                        ps, pk = next_ps()
                        for fb in range(8):
                            op("pe", lambda e: e.matmul(ps[:, :], lhsT=hTm[:, fb, sbk * 128:(sbk + 1) * 128], rhs=Wd[i][:, fb, dh * 512:(dh + 1) * 512],
                                                        start=(fb == 0), stop=(fb == 7)), r=[("Wd", i), ("hTm", fb, sbk // 4)], w=[pk])
                        op("act", lambda e: e.activation(out=yts[k2][:, dh * 512:(dh + 1) * 512], in_=ps[:, :], func=AF.Copy,
                                                         scale=gss[i][:, sbk:sbk + 1]), r=[pk, ("gs", i)], w=[("yt", k2)])
                    kb.idma(out=xbuf[:, :], out_offset=bass.IndirectOffsetOnAxis(ap=idxs[i][:, sbk:sbk + 1], axis=0), in_=yts[k2][:],
                            compute_op=ALU.add, r=[("yt", k2), ("idx", i)] + [("xs", ex - 1, jj) for jj in range(8)], w=[("xs", ex, sbk)])
            kb.barrier()
        P.close()
        kb.barrier()

    def phase_final():
        load_gain(4)
        with ExitStack() as L:
            xts = [kb.sb(L, f"fx{i}", [128, D], F32) for i in range(2)]
            ots = [kb.sb(L, f"fo{i}", [128, D], F32) for i in range(2)]
            junk = kb.sb(L, "fjunk", [128, D], BF16)
            sss = [kb.sb(L, f"fss{i}", [128, 1], F32) for i in range(2)]
            rss = [kb.sb(L, f"frs{i}", [128, 1], F32) for i in range(2)]
            for t in range(S // 128):
                i = t % 2
                tg = f"f{i}"
                dma("sp", xts[i][:], xbuf[t * 128:(t + 1) * 128, :], w=[tg + "x"])
                if do_final_norm:
                    op("act", lambda e: e.activation(out=junk[:], in_=xts[i][:], func=AF.Square, accum_out=sss[i][:]),
                       r=[tg + "x"], w=[tg + "ss"])
                    op("act", lambda e: e.activation(out=rss[i][:], in_=sss[i][:], func=AF.Sqrt, bias=epsc[:], scale=1.0 / D),
                       r=[tg + "ss", "epsc"], w=[tg + "rs"])
                    op("dve", lambda e: e.reciprocal(out=rss[i][:], in_=rss[i][:]), r=[tg + "rs"], w=[tg + "rs"])
                    op("dve", lambda e: e.scalar_tensor_tensor(out=ots[i][:], in0=xts[i][:], scalar=rss[i][:, 0:1], in1=gain_sb[:],
                                                               op0=ALU.mult, op1=ALU.mult), r=[tg + "x", tg + "rs", "gain"], w=[tg + "o"])
                    dma("pool", out_d[t * 128:(t + 1) * 128, :], ots[i][:], r=[tg + "o"], w=[("outd", t)])
                else:
                    dma("pool", out_d[t * 128:(t + 1) * 128, :], xts[i][:], r=[tg + "x"], w=[("outd", t)])
            kb.barrier()

    PV = 1088
    KP = 1024
    DILS = (1, 4, 16)

    def phase_attn():
        P = ExitStack()
        load_gain(2)
        with ExitStack() as L:
            rbl = kb.sb(L, "rbl", [33, 16], F32)
            gq = kb.sb(L, "gq", [33, 3, 383], F32)
            vecs = kb.sb(L, "vecs", [16, 3, 383], F32)
            zt = kb.sb(L, "zt", [128, 1040], BF16)
            op("dve", lambda e: e.memset(rbl[:], 1.0), w=["rbl"])
            dma("sp", rbl[0:32, :], rel_bias[:, :], w=["rbl"])
            dma("sp", gq[:], gpat_in[:, :, :], w=["gq"])
            for di in range(3):
                ps, pk = next_ps()
                op("pe", lambda e: e.matmul(ps[0:16, 0:383], lhsT=rbl[:], rhs=gq[:, di, :], start=True, stop=True), r=["rbl", "gq"], w=[pk])
                op("act", lambda e: e.copy(out=vecs[:, di, :], in_=ps[0:16, 0:383]), r=[pk], w=["vecs"])
            dma("sp", vecbuf.rearrange("d h u -> h d u"), vecs[:], r=["vecs"], w=["vecbuf"])
            op("dve", lambda e: e.memset(zt[:], 0.0), w=["zt"])
            for hp in range(8):
                dma("sp", kTbuf[hp, :, 0:KP], zt[:, 0:KP], r=["zt"])
                dma("sp", kTbuf[hp, :, KP + S:KP + S + KP], zt[:, 0:KP], r=["zt"])
            for base in (0, PV + S):
                for a in range(8):
                    dma("sp", vbuf[base + a * 128:base + (a + 1) * 128, :, :], zt[:, :].rearrange("p (h c) -> p h c", h=16), r=["zt"])
                dma("sp", vbuf[base + 1024:base + 1088, :, :], zt[0:64, :].rearrange("p (h c) -> p h c", h=16), r=["zt"])
            kb.barrier()
        with ExitStack() as L:
            wqkv = kb.sb(L, "wqkv", [128, 8, 3072], BF16)
            for j in range(6):
                dma("pool", wqkv[:, :, j * 512:(j + 1) * 512], w_qkv.rearrange("(c p) n -> p c n", p=128)[:, :, j * 512:(j + 1) * 512], w=["wqkv"])
            hT = kb.sb(L, "hT1", [128, 8, T], BF16)
            xts = [kb.sb(L, f"a1x{i}", [128, D], F32) for i in range(2)]
            hbs = [kb.sb(L, f"a1h{i}", [128, D], BF16) for i in range(2)]
            junk = kb.sb(L, "a1junk", [128, D], BF16)
            sss = [kb.sb(L, f"a1ss{i}", [128, 1], F32) for i in range(2)]
            rss = [kb.sb(L, f"a1rs{i}", [128, 1], F32) for i in range(2)]
            qst = [kb.sb(L, f"qst{i}", [128, 512], BF16) for i in range(2)]
            vau = [kb.sb(L, f"vau{i}", [128, 16, 65], BF16) for i in range(2)]
            for i in range(2):
                op("dve", lambda e: e.memset(vau[i][:], 1.0), w=[("vau", i)])
            nq = 0
            for blk in range(NB):
                for t in range(NT):
                    i = t % 2
                    tag = f"a1{i}"
                    r0 = blk * T + t * 128
                    dma("sp", xts[i][:], xbuf[r0:r0 + 128, :], w=[tag + "x"])
                    rms_tile(tag, xts[i][:], hbs[i], junk, sss[i], rss[i], [tag + "x"])
                    pb, pk = next_pb()
                    for c in range(8):
                        op("pe", lambda e: e.transpose(out=pb[:, c * 128:(c + 1) * 128], in_=hbs[i][:, c * 128:(c + 1) * 128], identity=identb[:]),
                           r=[tag + "hb", "identb"], w=[pk])
                    op("act", lambda e: e.copy(out=hT[:, :, t * 128:(t + 1) * 128], in_=pb[:, :].rearrange("p (c t) -> p c t", c=8)),
                       r=[pk], w=[("hT1", t)])
                for which in range(2):
                    for hp in range(8):
                        for tb in range(T // 512):
                            ps, pk = next_ps()
                            col = which * 1024 + hp * 128
                            for c in range(8):
                                op("pe", lambda e: e.matmul(ps[:, :], lhsT=wqkv[:, c, col:col + 128], rhs=hT[:, c, tb * 512:(tb + 1) * 512],
                                                            start=(c == 0), stop=(c == 7)),
                                   r=["wqkv"] + [("hT1", t) for t in range(tb * 4, tb * 4 + 4)], w=[pk])
                            k2 = nq % 2
                            nq += 1
                            op("act", lambda e: e.activation(out=qst[k2][:], in_=ps[:, :], func=AF.Copy, scale=(0.125 if which == 0 else 1.0)),
                               r=[pk], w=[("qst", k2)])
                            c0 = blk * T + tb * 512
                            if which == 0:
                                dma("pool", qTbuf[hp, :, c0:c0 + 512], qst[k2][:], r=[("qst", k2)])
                            else:
                                dma("pool", kTbuf[hp, :, KP + c0:KP + c0 + 512], qst[k2][:], r=[("qst", k2)])
                for t in range(NT):
                    i = t % 2
                    for hf2 in range(2):
                        ps, pk = next_ps()
                        for c in range(8):
                            op("pe", lambda e: e.matmul(ps[:, :], lhsT=hT[:, c, t * 128:(t + 1) * 128], rhs=wqkv[:, c, 2048 + hf2 * 512:2048 + (hf2 + 1) * 512],
                                                        start=(c == 0), stop=(c == 7)), r=["wqkv", ("hT1", t)], w=[pk])
                        op("act", lambda e: e.copy(out=vau[i][:, hf2 * 8:(hf2 + 1) * 8, 0:64], in_=ps[:, :].rearrange("p (h c) -> p h c", h=8)),
                           r=[pk], w=[("vau", i)])
                    r0 = PV + blk * T + t * 128
                    dma("pool", vbuf[r0:r0 + 128, :, :], vau[i][:], r=[("vau", i)])
            kb.barrier()
        if stage == 40:
            P.close()
            return
        AB = 2048
        with ExitStack() as L:
            kw = kb.sb(L, "kw", [128, 4, AB + 2 * KP], BF16)
            qw = kb.sb(L, "qw", [128, 4, AB], BF16)
            Vd = [kb.sb(L, f"Vd{i}", [128, 32, 8, 65], BF16) for i in range(2)]
            BTd = [kb.sb(L, f"BTd{i}", [128, 8, 2, 128], F32) for i in range(2)]
            BTr = kb.sb(L, "BTr", [128, 8, 2, 128], F32)
            sTs = [kb.sb(L, f"sTs{i}", [128, 512], F32) for i in range(4)]
            pTs = [kb.sb(L, f"pTs{i}", [128, 512], BF16) for i in range(4)]
            stg = [kb.sb(L, f"stg{i}", [128, 8, 65], F32) for i in range(2)]
            nv = 0
            ntile = 0
            nh = 0
            for ab in range(S // AB):
                for hh in range(2):
                    for j in range(4):
                        dma("sp", kw[:, j, :], kTbuf[hh * 4 + j, :, ab * AB:ab * AB + AB + 2 * KP], w=[("kw", j)])
                        dma("sp", qw[:, j, :], qTbuf[hh * 4 + j, :, ab * AB:(ab + 1) * AB], w=[("qw", j)])
                    for di, dl in enumerate(DILS):
                        vi = nv % 2
                        nv += 1
                        nrt = 16 // dl + 1
                        for r in range(dl):
                            st0 = PV + ab * AB + r - 64 * dl
                            src = vbuf[st0:st0 + dl * (nrt * 128 - 1) + 1:dl, hh * 8:(hh + 1) * 8, :].rearrange("(jt p) h c -> p jt h c", p=128)
                            dma("sp", Vd[vi][:, r * nrt:(r + 1) * nrt, :, :], src, w=[("Vd", vi, r)])
                        for h8 in range(8):
                            off = (di * 16 + hh * 8 + h8) * 383
                            src = bass.AP(tensor=vecbuf.tensor, offset=off, ap=[[1, 128], [128, 2], [1, 128]])
                            dma("sp", BTr[:, h8, :, :], src, r=["vecbuf"], w=[("BTr", h8)])
                            op("dve", lambda e: e.tensor_copy(out=BTd[vi][:, h8, :, :], in_=BTr[:, h8, :, ::-1]), r=[("BTr", h8)], w=[("BTd", vi)])
                        items = []
                        for r in range(dl):
                            for it in range(16 // dl):
                                si = ntile % 2
                                ntile += 1
                                for jj in range(4):
                                    items.append((r, it, si, jj, nh))
                                    nh += 1

                        def stA(item):
                            r, it, si, jj, n = item
                            psd = pdb[n % 2]
                            for hq in range(2):
                                pl = hq * 64
                                kw3 = kw[pl:pl + 64, jj, :].rearrange("p (m d) -> p m d", d=dl)
                                qw3 = qw[pl:pl + 64, jj, :].rearrange("p (m d) -> p m d", d=dl)
                                k0 = KP // dl - 64 + 128 * it
                                for c in range(2):
                                    cc0 = hq * 512 + c * 128
                                    op("pe", lambda e: e.matmul(psd[:, cc0:cc0 + 128], lhsT=kw3[:, k0 + 128 * c:k0 + 128 * (c + 1), r],
                                                                rhs=qw3[:, 128 * it:128 * (it + 1), r], start=True, stop=True),
                                       r=[("kw", jj), ("qw", jj)], w=[("pst", 2 * (n % 2) + hq)])

                        def stB(item):
                            r, it, si, jj, n = item
                            psd = pdb[n % 2]
                            k2 = n % 4
                            op("dve", lambda e: e.tensor_tensor(out=sTs[k2][:, :].rearrange("p (h x) -> p h x", h=2),
                                                                in0=psd[:, :].rearrange("p (h x) -> p h x", h=2)[:, :, 0:256],
                                                                in1=BTd[vi][:, 2 * jj:2 * jj + 2, :, :].rearrange("p h c q -> p h (c q)"), op=ALU.add),
                               r=[("pst", 2 * (n % 2)), ("pst", 2 * (n % 2) + 1), ("BTd", vi)], w=[("sTs", k2)])
                            op("act", lambda e: e.activation(out=pTs[k2][:], in_=sTs[k2][:], func=AF.Exp), r=[("sTs", k2)], w=[("pTs", k2)])

                        def stC(item):
                            r, it, si, jj, n = item
                            k2 = n % 4
                            pj_ = 4 + (n % 2)
                            ps2, pk2 = pst[pj_], ("pst", pj_)
                            for hq in range(2):
                                h8 = 2 * jj + hq
                                for c in range(2):
                                    cc0 = hq * 256 + c * 128
                                    op("pe", lambda e: e.matmul(ps2[:, hq * 128:hq * 128 + 65], lhsT=pTs[k2][:, cc0:cc0 + 128],
                                                                rhs=Vd[vi][:, r * nrt + it + c, h8, :], start=(c == 0), stop=(c == 1)),
                                       r=[("pTs", k2), ("Vd", vi, r)], w=[pk2])
                            if n % 2 == 0:
                                op("act", lambda e: e.copy(out=stg[si][:, 2 * jj:2 * jj + 2, :], in_=ps2[:, 0:256].rearrange("p (h c) -> p h c", h=2)[:, :, 0:65]),
                                   r=[pk2], w=[("stg", si, jj)])
                            else:
                                op("dve", lambda e: e.tensor_copy(out=stg[si][:, 2 * jj:2 * jj + 2, :], in_=ps2[:, 0:256].rearrange("p (h c) -> p h c", h=2)[:, :, 0:65]),
                                   r=[pk2], w=[("stg", si, jj)])
                            if jj == 3:
                                q0 = ab * AB + r + dl * 128 * it
                                dma("pool", accbuf[di, q0:q0 + dl * 127 + 1:dl, hh * 8:(hh + 1) * 8, :], stg[si][:], r=[("stg", si, jj_) for jj_ in range(4)])

                        NI = len(items)
                        stA(items[0])
                        if NI > 1:
                            stA(items[1])
                        stB(items[0])
                        for n_, item in enumerate(items):
                            if n_ + 2 < NI:
                                stA(items[n_ + 2])
                            if n_ + 1 < NI:
                                stB(items[n_ + 1])
                            stC(item)
            kb.barrier()
        if stage == 41:
            P.close()
            return
        with ExitStack() as L:
            wo = kb.sb(L, "wo1", [128, 8, D], BF16)
            dma("pool", wo[:], w_o.rearrange("(c p) n -> p c n", p=128), w=["wo1"])
            acs = [[kb.sb(L, f"ac{i}_{k}", [128, 16, 65], F32) for k in range(3)] for i in range(2)]
            rl = [kb.sb(L, f"rl{i}", [128, 16], F32) for i in range(2)]
            atb = [kb.sb(L, f"atb{i}", [128, 16, 64], BF16) for i in range(2)]
            atT = [kb.sb(L, f"atT{i}", [128, 8, 128], BF16) for i in range(2)]
            xts = [kb.sb(L, f"a3x{i}", [128, D], F32) for i in range(2)]
            for t in range(S // 128):
                i = t % 2
                tg = f"a3{i}"
                for k in range(3):
                    dma("sp", acs[i][k][:], accbuf[k, t * 128:(t + 1) * 128, :, :], w=[(tg, "ac", k)])
                dma("sp", xts[i][:], xbuf[t * 128:(t + 1) * 128, :], w=[tg + "x"])
                if stage in (143, 144, 145):
                    kk_ = stage - 143
                    op("dve", lambda e: e.tensor_copy(out=xts[i][:], in_=acs[i][kk_][:, :, :].rearrange("p h c -> p (h c)")[:, 0:1024]),
                       r=[(tg, "ac", kk_), tg + "x"], w=[tg + "x"])
                    dma("pool", xbuf[t * 128:(t + 1) * 128, :], xts[i][:], r=[tg + "x"])
                    continue
                op("dve", lambda e: e.tensor_tensor(out=acs[i][0][:], in0=acs[i][0][:], in1=acs[i][1][:], op=ALU.add),
                   r=[(tg, "ac", 0), (tg, "ac", 1)], w=[(tg, "ac", 0)])
                op("dve", lambda e: e.tensor_tensor(out=acs[i][0][:], in0=acs[i][0][:], in1=acs[i][2][:], op=ALU.add),
                   r=[(tg, "ac", 0), (tg, "ac", 2)], w=[(tg, "ac", 0)])
                op("dve", lambda e: e.reciprocal(out=rl[i][:], in_=acs[i][0][:, :, 64]), r=[(tg, "ac", 0)], w=[tg + "rl"])
                op("dve", lambda e: e.tensor_tensor(out=atb[i][:], in0=acs[i][0][:, :, 0:64],
                                                    in1=rl[i][:, :].unsqueeze(2).to_broadcast([128, 16, 64]), op=ALU.mult),
                   r=[(tg, "ac", 0), tg + "rl"], w=[tg + "atb"])
                atf = atb[i][:, :, :].rearrange("p h c -> p (h c)")
                if stage == 141:
                    op("dve", lambda e: e.tensor_copy(out=xts[i][:], in_=atf), r=[tg + "atb", tg + "x"], w=[tg + "x"])
                    dma("pool", xbuf[t * 128:(t + 1) * 128, :], xts[i][:], r=[tg + "x"])
                    continue
                pb, pk = next_pb()
                for c in range(8):
                    op("pe", lambda e: e.transpose(out=pb[:, c * 128:(c + 1) * 128], in_=atf[:, c * 128:(c + 1) * 128], identity=identb[:]),
                       r=[tg + "atb", "identb"], w=[pk])
                op("act", lambda e: e.copy(out=atT[i][:], in_=pb[:, :].rearrange("p (c t) -> p c t", c=8)), r=[pk], w=[tg + "atT"])
                for hf2 in range(2):
                    ps, pk2 = next_ps()
                    for c in range(8):
                        op("pe", lambda e: e.matmul(ps[:, :], lhsT=atT[i][:, c, :], rhs=wo[:, c, hf2 * 512:(hf2 + 1) * 512],
                                                    start=(c == 0), stop=(c == 7)), r=[tg + "atT", "wo1"], w=[pk2])
                    op("dve", lambda e: e.tensor_tensor(out=xts[i][:, hf2 * 512:(hf2 + 1) * 512], in0=ps[:, :],
                                                        in1=xts[i][:, hf2 * 512:(hf2 + 1) * 512], op=ALU.add), r=[pk2, tg + "x"], w=[tg + "x"])
                dma("pool", xbuf[t * 128:(t + 1) * 128, :], xts[i][:], r=[tg + "x"])
            kb.barrier()
        P.close()
        kb.barrier()

    do_final_norm = stage == 99
    if stage >= 100 or stage in (40, 41):
        with ExitStack() as L:
            xt = [kb.sb(L, f"cx{i}", [128, D], F32) for i in range(2)]
            for t in range(S // 128):
                i = t % 2
                dma("sp", xt[i][:], x_in[t * 128:(t + 1) * 128, :], w=[("cx", i)])
                dma("sp", xbuf[t * 128:(t + 1) * 128, :], xt[i][:], r=[("cx", i)])
            kb.barrier()
        if stage in (104, 141, 142, 143, 144, 145, 40, 41):
            phase_attn()
        if stage == 105:
            phase_moe(1)
    else:
        phase_m0()
        if stage >= 3:
            phase_moe(0)
        if stage >= 4:
            phase_attn()
        if stage >= 5:
            phase_moe(1)
    phase_final()

    G.close()
    kb.es.close()
    return nc


def _t5_bucket(rel):
    half_buckets, max_exact = 16, 8
    n = np.abs(rel)
    scaled = (np.log(np.maximum(n, 1).astype(np.float32) / np.float32(max_exact)) / np.float32(np.log(1024 / max_exact))).astype(np.float32)
    large = np.minimum(max_exact + (scaled * np.float32(half_buckets - max_exact)).astype(np.int32), half_buckets - 1)
    return np.where(rel > 0, half_buckets, 0) + np.where(n < max_exact, n, large)


def _bias_patterns():
    g = np.zeros((33, 3, 383), dtype=np.float32)
    u = np.arange(383)
    rel = u - 191
    valid = np.abs(rel) <= 64
    for di, dl in enumerate((1, 4, 16)):
        b = _t5_bucket(rel * dl)
        for uu in range(383):
            if valid[uu]:
                g[b[uu], di, uu] = 1.0
            else:
                g[32, di, uu] = -1e30
    return g


def host_inputs(inp, b):
    f = np.float32
    d = {}
    d["x"] = np.ascontiguousarray(inp["x"][b], dtype=f)
    g = np.stack([inp["mix_norm"][0], inp["ffn_norm"][0], inp["mix_norm"][1], inp["ffn_norm"][1], inp["final_norm"]])
    d["gains"] = np.ascontiguousarray(np.broadcast_to(g[:, None, :], (5, 128, D)), dtype=f)
    d["w_in"] = np.ascontiguousarray(inp["w_in_even"][0], dtype=f)
    d["w_out"] = np.ascontiguousarray(inp["w_out_even"][0], dtype=f)
    d["a_ln"] = np.ascontiguousarray(np.broadcast_to(np.stack([inp["a_ln_g"][0], inp["a_ln_b"][0]])[:, None, :], (2, 128, 512)), dtype=f)
    d["a_wsT"] = np.ascontiguousarray(np.transpose(inp["a_w_s"][0], (2, 0, 1)), dtype=f)
    d["a_bs"] = np.ascontiguousarray(np.repeat(np.transpose(inp["a_b_s"][0], (1, 0))[:, :, None], 128, axis=2).reshape(128, 512), dtype=f)
    d["lbt"] = np.ascontiguousarray(np.transpose(inp["b_lb_table"].reshape(2, 3, 4, 128), (3, 2, 0, 1)), dtype=f)
    d["b_ng"] = np.ascontiguousarray(np.broadcast_to(inp["b_norm_g"][0][None, :], (64, 512)), dtype=f)
    d["ident_f"] = np.eye(128, dtype=f)
    rm = np.ones((128, T), dtype=f)
    rm[:, ::64] = 0.0
    d["rmask"] = rm
    s_ = np.arange(64)[:, None]
    t_ = np.arange(64)[None, :]
    d["tri"] = np.ascontiguousarray(np.stack([(s_ <= t_), (s_ >= t_)], axis=1), dtype=f)
    d["ones_f"] = np.ones((128, 128), dtype=f)
    pp = np.arange(128)
    d["lstrict"] = np.ascontiguousarray((pp[:, None] < pp[None, :]), dtype=f)
    d["iota_s"] = np.ascontiguousarray(np.broadcast_to(np.arange(CAP, dtype=f)[None, :], (128, CAP)))
    r2 = np.ones((128, NE, 64), dtype=f)
    r2[:, :, 0] = 0.0
    d["rmask2"] = r2.reshape(128, NE * 64)
    td = np.zeros((128, 64, 2), dtype=f)
    td[:, :, 0] = np.arange(64)[None, :]
    td[:, :, 1] = np.arange(128)[:, None]
    d["tokdig"] = td
    d["rel_bias"] = np.ascontiguousarray(inp["rel_bias"], dtype=f)
    d["gpat"] = _bias_patterns()
    d["w_qkv"] = np.ascontiguousarray(inp["w_qkv_odd"][0], dtype=f)
    d["w_o"] = np.ascontiguousarray(inp["w_o_odd"][0], dtype=f)
    d["w_router"] = np.ascontiguousarray(inp["w_router"], dtype=f)
    d["w_gate"] = np.ascontiguousarray(inp["w_gate"], dtype=f)
    d["w_up"] = np.ascontiguousarray(inp["w_up"], dtype=f)
    d["w_down"] = np.ascontiguousarray(inp["w_down"], dtype=f)
    return d


_CACHE = {}


def kernel(**inputs):
    inp = {k: np.asarray(v) for k, v in inputs.items()}
    if "nc" not in _CACHE:
        _CACHE["nc"] = build_program()
    nc = _CACHE["nc"]
    per_b = [host_inputs(inp, b) for b in range(2)]
    if globals().get("_DBG_STAGE", 0) in (104, 141, 142, 143, 144, 145, 40, 41):
        for pb_ in per_b:
            for k_ in ("w_gate", "w_up", "w_down"):
                pb_.pop(k_)
    in_maps = [per_b[c % 2] for c in range(8)]
    res = run_bass_kernel_spmd(nc, in_maps, core_ids=list(range(8)))
    out = np.stack([res.results[0]["out"], res.results[1]["out"]], axis=0).astype(np.float32)
    return out
```

```python
import numpy as np
from contextlib import ExitStack
import concourse.bass as bass
import concourse.mybir as mybir
from concourse.bass_utils import run_bass_kernel_spmd

F32 = mybir.dt.float32
BF16 = mybir.dt.bfloat16
I32 = mybir.dt.int32
AF = mybir.ActivationFunctionType
ALU = mybir.AluOpType
AX = mybir.AxisListType

S = 8192
D = 1024
T = 1024
NB = S // T
NT = T // 128
NCH = T // 64
EPS = 1e-6
NE = 16
CAP = 1024


class KB:
    def __init__(self):
        self.nc = bass.Bass("TRN2", target_bir_lowering=False)
        nc = self.nc
        self.es = ExitStack()
        self.E = dict(pe=nc.tensor, act=nc.scalar, dve=nc.vector, pool=nc.gpsimd, sp=nc.sync)
        self.sem = {k: self.es.enter_context(nc.semaphore("sem_" + k)) for k in self.E}
        self.cnt = dict.fromkeys(self.E, 0)
        self.waited = {}
        self.lastw = {}
        self.readers = {}
        self.dq = {}
        for q in ("sp", "pool", "act"):
            sems = [self.es.enter_context(nc.semaphore(f"dq_{q}_{i}")) for i in range(12)]
            self.dq[q] = dict(sems=sems, vals=[0] * 12, i=0)
        self.nps = 0

    def sb(self, st, name, shape, dt):
        self.nps += 1
        return st.enter_context(self.nc.sbuf_tensor(f"sb{self.nps}_{name}", list(shape), dt))

    def ps(self, st, name, shape, dt=F32):
        self.nps += 1
        return st.enter_context(self.nc.psum_tensor(f"ps{self.nps}_{name}", list(shape), dt))

    def _wait(self, eng, h):
        sem, name, val, heng = h
        k = (eng, name)
        if self.waited.get(k, 0) >= val:
            return
        self.E[eng].wait_ge(sem, val)
        self.waited[k] = val

    def _deps(self, eng, r, w, is_dma=False):
        out = []
        for k in r:
            h = self.lastw.get(k)
            if h is not None:
                if is_dma or h[3] != eng or eng != "pe":
                    out.append(h)
        for k in w:
            h = self.lastw.get(k)
            if h is not None and (is_dma or h[3] != eng):
                out.append(h)
            for h in self.readers.get(k, ()):
                if is_dma or h[3] != eng:
                    out.append(h)
        return out

    def _record(self, h, r, w):
        for k in r:
            lst = self.readers.setdefault(k, [])
            if h[3] is not None:
                lst[:] = [x for x in lst if x[3] != h[3]]
            lst.append(h)
            if len(lst) > 24:
                del lst[0]
        for k in w:
            self.lastw[k] = h
            self.readers[k] = []

    def op(self, eng, fn, r=(), w=()):
        for h in self._deps(eng, r, w):
            self._wait(eng, h)
        ins = fn(self.E[eng])
        self.cnt[eng] += 1
        ins.then_inc(self.sem[eng], 1)
        h = (self.sem[eng], "sem_" + eng, self.cnt[eng], eng)
        self._record(h, r, w)
        return h

    def dma(self, q, out, in_, r=(), w=(), **kw):
        for h in self._deps(q, r, w, is_dma=True):
            self._wait(q, h)
        d = self.dq[q]
        i = d["i"]
        d["i"] = (i + 1) % len(d["sems"])
        sem = d["sems"][i]
        name = f"dq_{q}_{i}"
        if d["vals"][i] > 0:
            self._wait(q, (sem, name, d["vals"][i], None))
        ins = self.E[q].dma_start(out=out, in_=in_, **kw)
        ins.then_inc(sem, 16)
        d["vals"][i] += 16
        h = (sem, name, d["vals"][i], None)
        self._record(h, r, w)
        return h

    def idma(self, out, in_, out_offset=None, in_offset=None, r=(), w=(), **kw):
        q = "pool"
        for h in self._deps(q, r, w, is_dma=True):
            self._wait(q, h)
        d = self.dq[q]
        i = d["i"]
        d["i"] = (i + 1) % len(d["sems"])
        sem = d["sems"][i]
        name = f"dq_{q}_{i}"
        if d["vals"][i] > 0:
            self._wait(q, (sem, name, d["vals"][i], None))
        ins = self.nc.gpsimd.indirect_dma_start(out=out, out_offset=out_offset, in_=in_, in_offset=in_offset, **kw)
        ins.then_inc(sem, 16)
        d["vals"][i] += 16
        h = (sem, name, d["vals"][i], None)
        self._record(h, r, w)
        return h

    def barrier(self):
        hs = [(self.sem[e], "sem_" + e, self.cnt[e], e) for e in self.E if self.cnt[e] > 0]
        for q, d in self.dq.items():
            for i, s in enumerate(d["sems"]):
                if d["vals"][i] > 0:
                    hs.append((s, f"dq_{q}_{i}", d["vals"][i], None))
        for eng in self.E:
            for h in hs:
                if h[3] != eng:
                    self._wait(eng, h)
        self.lastw.clear()
        self.readers.clear()


def build_program(stage=99):
    kb = KB()
    nc = kb.nc
    op, dma = kb.op, kb.dma

    def dram_in(name, shape, dt=F32):
        return nc.dram_tensor(name, list(shape), dt, kind="ExternalInput").ap()

    x_in = dram_in("x", [S, D])
    gains = dram_in("gains", [5, 128, D])
    w_in = dram_in("w_in", [D, 3584])
    w_out = dram_in("w_out", [D, D])
    a_ln = dram_in("a_ln", [2, 128, 512])
    a_wsT = dram_in("a_wsT", [128, 4, 128])
    a_bs = dram_in("a_bs", [128, 512])
    lbt = dram_in("lbt", [128, 4, 2, 3])
    b_ng = dram_in("b_ng", [64, 512])
    ident_f = dram_in("ident_f", [128, 128])
    rmask_in = dram_in("rmask", [128, T])
    tri_in = dram_in("tri", [64, 2, 64])
    ones_in = dram_in("ones_f", [128, 128])
    lstrict_in = dram_in("lstrict", [128, 128])
    iota_in = dram_in("iota_s", [128, CAP])
    rmask2_in = dram_in("rmask2", [128, NE * 64])
    tokdig_in = dram_in("tokdig", [128, 64, 2])
    w_router = dram_in("w_router", [2, D, NE])
    if stage in (104, 141, 142, 143, 144, 145, 40, 41):
        w_gate = w_up = w_down = None
    else:
        w_gate = dram_in("w_gate", [2, NE, D, D])
        w_up = dram_in("w_up", [2, NE, D, D])
        w_down = dram_in("w_down", [2, NE, D, D])
    rel_bias = dram_in("rel_bias", [32, 16])
    gpat_in = dram_in("gpat", [33, 3, 383])
    w_qkv = dram_in("w_qkv", [D, 3072])
    w_o = dram_in("w_o", [D, D])
    out_d = nc.dram_tensor("out", [S, D], F32, kind="ExternalOutput").ap()
    vecbuf = nc.dram_tensor("vecbuf", [3, 16, 383], F32, kind="Internal").ap()
    dk = "ExternalOutput" if stage in (143, 144, 145) else "Internal"
    if stage == 143:
        dbg_s = nc.dram_tensor("dbg_s", [128, 256], F32, kind="ExternalOutput").ap()
        dbg_p = nc.dram_tensor("dbg_p", [128, 256], BF16, kind="ExternalOutput").ap()
        dbg_v = nc.dram_tensor("dbg_v", [128, 2, 65], BF16, kind="ExternalOutput").ap()
        dbg_k = nc.dram_tensor("dbg_k", [64, 2, 128], BF16, kind="ExternalOutput").ap()
        dbg_q = nc.dram_tensor("dbg_q", [64, 128], BF16, kind="ExternalOutput").ap()
        dbg_o = nc.dram_tensor("dbg_o", [128, 8, 65], F32, kind="ExternalOutput").ap()
        dbg_a = nc.dram_tensor("dbg_a", [128, 8, 65], F32, kind="ExternalOutput").ap()
    qTbuf = nc.dram_tensor("qTbuf", [8, 128, S], BF16, kind=dk).ap()
    kTbuf = nc.dram_tensor("kTbuf", [8, 128, S + 2048], BF16, kind=dk).ap()
    vbuf = nc.dram_tensor("vbuf", [S + 2 * 1088, 16, 65], BF16, kind=dk).ap()
    accbuf = nc.dram_tensor("accbuf", [3, S, 16, 65], F32, kind="Internal").ap()
    h2buf = nc.dram_tensor("h2buf", [S, D], BF16, kind="Internal").ap()
    xbuf = nc.dram_tensor("xbuf", [S, D], F32, kind="Internal").ap()
    hTbuf = nc.dram_tensor("hTbuf", [NB, 128, 8, T], BF16, kind="Internal").ap()

    G = ExitStack()
    identb = kb.sb(G, "identb", [128, 128], BF16)
    identf = kb.sb(G, "identf", [128, 128], F32)
    epsc = kb.sb(G, "epsc", [128, 1], F32)
    gain_sb = kb.sb(G, "gain_sb", [128, D], F32)
    pdb = [kb.ps(G, f"pdb{i}", [128, 1024], F32) for i in range(3)]
    pst = [pdb[i // 2][:, (i % 2) * 512:(i % 2 + 1) * 512] for i in range(6)]
    psb = [kb.ps(G, f"psb{i}", [128, 1024], BF16) for i in range(2)]
    psi = [0]
    pbi = [0]

    psr = [0, 6]

    def next_ps():
        i = psi[0]
        if not (psr[0] <= i < psr[1]):
            i = psr[0]
        psi[0] = i + 1 if i + 1 < psr[1] else psr[0]
        return pst[i], ("pst", i)

    hsi = [0]

    def next_hs():
        i = hsi[0]
        hsi[0] = (i + 1) % 4
        return pst[i], ("pst", i)

    pqi = [0]

    def next_pbq():
        i = pqi[0]
        pqi[0] = (i + 1) % 4
        return psb[0][:, i * 256:(i + 1) * 256], ("pbq", i)

    pbr = [0, 2]

    def next_pb():
        i = pbi[0]
        if not (pbr[0] <= i < pbr[1]):
            i = pbr[0]
        pbi[0] = i + 1 if i + 1 < pbr[1] else pbr[0]
        return psb[i], ("psb", i)

    dma("sp", identf[:], ident_f[:, :], w=["identf"])
    op("dve", lambda e: e.tensor_copy(out=identb[:], in_=identf[:]), r=["identf"], w=["identb"])
    op("dve", lambda e: e.memset(epsc[:], EPS), w=["epsc"])

    def load_gain(i):
        dma("sp", gain_sb[:], gains[i, :, :], w=["gain"])

    def rms_tile(st_tag, xt, hb, junk, ss, rs, keys_x):
        op("act", lambda e: e.activation(out=junk[:], in_=xt, func=AF.Square, accum_out=ss[:]),
           r=keys_x, w=[st_tag + "junk", st_tag + "ss"])
        op("act", lambda e: e.activation(out=rs[:], in_=ss[:], func=AF.Sqrt, bias=epsc[:], scale=1.0 / D),
           r=[st_tag + "ss", "epsc"], w=[st_tag + "rs"])
        op("dve", lambda e: e.reciprocal(out=rs[:], in_=rs[:]), r=[st_tag + "rs"], w=[st_tag + "rs"])
        op("dve", lambda e: e.scalar_tensor_tensor(out=hb[:], in0=xt, scalar=rs[:, 0:1], in1=gain_sb[:],
                                                   op0=ALU.mult, op1=ALU.mult),
           r=keys_x + [st_tag + "rs", "gain"], w=[st_tag + "hb"])

    def phase_m0():
        P = ExitStack()
        lb_sb = kb.sb(P, "lb_sb", [128, 4, 2, 3], F32)
        lb_e = kb.sb(P, "lb_e", [128, 4, 2, 3], F32)
        lb_s = kb.sb(P, "lb_s", [128, 4, 2], F32)
        lbv = kb.sb(P, "lbv", [128, 4, 2], F32)
        oml = kb.sb(P, "oml", [128, 4, 2], F32)
        rmask = kb.sb(P, "rmask", [128, T], F32)
        tri = kb.sb(P, "tri", [64, 2, 64], F32)
        wsT = kb.sb(P, "wsT", [128, 4, 128], BF16)
        lng = kb.sb(P, "lng", [128, 512], F32)
        lnb = kb.sb(P, "lnb", [128, 512], F32)
        bsf = kb.sb(P, "bsf", [128, 512], F32)
        bng = kb.sb(P, "bng", [64, 512], F32)
        Sin_b = kb.sb(P, "Sin_b", [128, NB, 4, 128], F32)
        Bb = kb.sb(P, "Bb", [128, NB, 4, 128], F32)
        Ab = kb.sb(P, "Ab", [128, NB, 4], F32)
        Sf = kb.sb(P, "Sf", [128, 4, 128], BF16)
        hT = kb.sb(P, "hT", [128, 8, T], BF16)
        load_gain(0)
        dma("sp", lb_sb[:], lbt[:, :, :, :], w=["lb_sb"])
        dma("sp", rmask[:], rmask_in[:, :], w=["rmask"])
        dma("sp", tri[:], tri_in[:, :, :], w=["tri"])
        dma("pool", wsT[:], a_wsT[:, :, :], w=["wsT"])
        dma("sp", lng[:], a_ln[0, :, :], w=["lng"])
        dma("sp", lnb[:], a_ln[1, :, :], w=["lnb"])
        dma("sp", bsf[:], a_bs[:, :], w=["bsf"])
        dma("sp", bng[:], b_ng[:, :], w=["bng"])
        op("act", lambda e: e.activation(out=lb_e[:], in_=lb_sb[:], func=AF.Exp), r=["lb_sb"], w=["lb_e"])
        op("dve", lambda e: e.tensor_reduce(out=lb_s[:], in_=lb_e[:], axis=AX.X, op=ALU.add), r=["lb_e"], w=["lb_s"])
        op("dve", lambda e: e.reciprocal(out=lb_s[:], in_=lb_s[:]), r=["lb_s"], w=["lb_s"])
        op("dve", lambda e: e.tensor_tensor(out=lbv[:], in0=lb_e[:, :, :, 0], in1=lb_s[:], op=ALU.mult),
           r=["lb_e", "lb_s"], w=["lbv"])
        op("dve", lambda e: e.tensor_scalar(out=oml[:], in0=lbv[:], scalar1=-1.0, scalar2=1.0, op0=ALU.mult, op1=ALU.add),
           r=["lbv"], w=["oml"])

        def make_hT(blk, src):
            with ExitStack() as L:
                xts = [kb.sb(L, f"m0x{i}", [128, D], F32) for i in range(2)]
                hbs = [kb.sb(L, f"m0h{i}", [128, D], BF16) for i in range(2)]
                junk = kb.sb(L, "m0junk", [128, D], BF16)
                sss = [kb.sb(L, f"m0ss{i}", [128, 1], F32) for i in range(2)]
                rss = [kb.sb(L, f"m0rs{i}", [128, 1], F32) for i in range(2)]
                for t in range(NT):
                    i = t % 2
                    tag = f"mh{i}"
                    r0 = blk * T + t * 128
                    dma("sp", xts[i][:], src[r0:r0 + 128, :], w=[tag + "x"])
                    rms_tile(tag, xts[i][:], hbs[i], junk, sss[i], rss[i], [tag + "x"])
                    pb, pk = next_pb()
                    for c in range(8):
                        op("pe", lambda e, c=c: e.transpose(out=pb[:, c * 128:(c + 1) * 128],
                                                            in_=hbs[i][:, c * 128:(c + 1) * 128], identity=identb[:]),
                           r=[tag + "hb", "identb"], w=[pk])
                    op("act", lambda e: e.copy(out=hT[:, :, t * 128:(t + 1) * 128],
                                               in_=pb[:, :].rearrange("p (c t) -> p c t", c=8)),
                       r=[pk], w=[("hT", t)])
                kb.barrier()

        def load_w(wt, col0, ncol, key):
            dma("pool", wt, w_in.rearrange("(c p) n -> p c n", p=128)[:, :, col0:col0 + ncol], w=[key])

        def proj_fm(wt, wkey, j, dst_fn):
            for tb in range(T // 512):
                ps, pk = next_ps()
                for c in range(8):
                    op("pe", lambda e, c=c: e.matmul(ps[:, :], lhsT=wt[:, c, j, :], rhs=hT[:, c, tb * 512:(tb + 1) * 512],
                                                      start=(c == 0), stop=(c == 7)),
                       r=[wkey] + [("hT", t) for t in range(tb * 4, tb * 4 + 4)], w=[pk])
                dst_fn(tb, ps, pk)

        def hgrn_prep(L, wt, wkey, h, d, qf, need_q, bufs, tag=None, Qt=None, Kt=None, eend=None, ltot=None, qkey="qf"):
            fb, lb_, eb = bufs["fb"], bufs["lb"], bufs["eb"]

            def evac_sig(tb, ps, pk):
                op("act", lambda e: e.activation(out=fb[:, tb * 512:(tb + 1) * 512], in_=ps[:, :], func=AF.Sigmoid),
                   r=[pk], w=[("fb", tb)])
            proj_fm(wt, wkey, 1 + d, evac_sig)
            fbk = [("fb", tb) for tb in range(4)]
            op("dve", lambda e: e.tensor_scalar(out=fb[:], in0=fb[:], scalar1=oml[:, h, d:d + 1], scalar2=lbv[:, h, d:d + 1],
                                                op0=ALU.mult, op1=ALU.add), r=fbk + ["oml", "lbv"], w=fbk)
            op("act", lambda e: e.activation(out=lb_[:], in_=fb[:], func=AF.Ln), r=fbk, w=["lb"])
            op("dve", lambda e: e.tensor_scalar(out=fb[:], in0=fb[:], scalar1=-1.0, scalar2=1.0, op0=ALU.mult, op1=ALU.add),
               r=fbk, w=fbk)
            op("dve", lambda e: e.tensor_tensor_scan(out=eb[:], data0=rmask[:], data1=lb_[:], initial=0.0,
                                                     op0=ALU.mult, op1=ALU.add), r=["rmask", "lb"], w=["eb"])
            eb3 = eb[:, :].rearrange("p (c j) -> p c j", j=64)
            if d == 1:
                op("dve", lambda e: e.tensor_tensor(out=lb_[:], in0=lb_[:], in1=eb[:], op=ALU.subtract), r=["lb", "eb"], w=["lb"])
                op("dve", lambda e: e.tensor_copy(out=eend[:], in_=eb3[:, :, 63]), r=["eb"], w=[tag + "eend"])
                op("dve", lambda e: e.tensor_tensor(out=eb3, in0=lb_[:, :].rearrange("p (c j) -> p c j", j=64),
                                                    in1=eend[:, :].unsqueeze(2).to_broadcast([128, NCH, 64]), op=ALU.add),
                   r=["lb", tag + "eend"], w=["eb"])
            col = 63 if d == 0 else 0
            if ltot is not None:
                op("dve", lambda e: e.tensor_reduce(out=ltot, in_=eb3[:, :, col], axis=AX.X, op=ALU.add),
                   r=["eb"], w=[tag + "ltot"])
            op("act", lambda e: e.activation(out=lb_[:], in_=eb[:], func=AF.Exp, scale=-1.0), r=["eb"], w=["lb"])
            op("dve", lambda e: e.tensor_tensor(out=Kt[:], in0=fb[:], in1=lb_[:], op=ALU.mult), r=fbk + ["lb"], w=[tag + "Kt"])
            op("act", lambda e: e.activation(out=lb_[:], in_=eb[:], func=AF.Exp), r=["eb"], w=["lb"])
            lb3 = lb_[:, :].rearrange("p (c j) -> p c j", j=64)
            op("dve", lambda e: e.tensor_copy(out=eend[:], in_=lb3[:, :, col]), r=["lb"], w=[tag + "eend"])
            if need_q:
                op("dve", lambda e: e.tensor_tensor(out=Qt[:], in0=qf[:], in1=lb_[:], op=ALU.mult), r=[qkey, "lb"], w=[tag + "Qt"])

        def proj_tok(wt, wkey, j, dst, dkey, func):
            for g4 in range(NCH // 4):
                ps, pk = next_ps()
                for cc in range(4):
                    ch = g4 * 4 + cc
                    for c in range(8):
                        op("pe", lambda e, c=c, cc=cc, ch=ch: e.matmul(ps[0:64, cc * 128:(cc + 1) * 128],
                                                                       lhsT=hT[:, c, ch * 64:(ch + 1) * 64], rhs=wt[:, c, j, :],
                                                                       start=(c == 0), stop=(c == 7)),
                           r=[wkey, ("hT", ch // 2)], w=[pk])
                op("act", lambda e: e.activation(out=dst[:, g4 * 4:(g4 + 1) * 4, :],
                                                 in_=ps[0:64, :].rearrange("p (c v) -> p c v", c=4), func=func),
                   r=[pk], w=[(dkey, g4)])

        def scan_chunk(c, ch):
            d, tag, Qt, Kt, eend, vt, St = c["d"], c["tag"], c["Qt"], c["Kt"], c["eend"], c["vt"], c["St"]
            vkey = c["vkey"]
            qi = c["n"] % 2
            c["n"] += 1
            cs = slice(ch * 64, (ch + 1) * 64)
            pbq, pbk = next_pbq()
            op("pe", lambda e: e.transpose(out=pbq[0:64, 0:128], in_=Kt[:, cs], identity=identb[:]), r=[tag + "Kt", "identb"], w=[pbk])
            ktk = c["ktk"][qi]
            op("dve", lambda e: e.tensor_copy(out=ktk[:], in_=pbq[0:64, 0:128]), r=[pbk], w=[(tag, "ktk", qi)])
            if c["emit_o"]:
                ps, pk = next_hs()
                op("pe", lambda e: e.matmul(ps[0:64, 0:64], lhsT=Kt[:, cs], rhs=Qt[:, cs], start=True, stop=True),
                   r=[tag + "Kt", tag + "Qt"], w=[pk])
                sT = c["sc"][qi]
                op("dve", lambda e: e.tensor_tensor(out=sT[:], in0=ps[0:64, 0:64], in1=tri[:, d, :], op=ALU.mult),
                   r=[pk, "tri"], w=[(tag, "sc", qi)])
                ps2, pk2 = next_hs()
                op("pe", lambda e: e.matmul(ps2[0:64, 0:128], lhsT=sT[:], rhs=vt[:, ch, :], start=True, stop=False),
                   r=[(tag, "sc", qi), (vkey, ch // 4)], w=[pk2])
                op("pe", lambda e: e.matmul(ps2[0:64, 0:128], lhsT=Qt[:, cs], rhs=St[:], start=False, stop=True),
                   r=[tag + "Qt", tag + "St"], w=[pk2])
                op("act", lambda e: e.copy(out=c["o"][:, ch, :], in_=ps2[0:64, 0:128]), r=[pk2], w=[(tag, "o", ch)])
            ps3, pk3 = next_hs()
            op("pe", lambda e: e.matmul(ps3[:, 0:128], lhsT=identb[:], rhs=St[:], start=True, stop=False),
               r=["identb", tag + "St"], w=[pk3])
            op("pe", lambda e: e.matmul(ps3[:, 0:128], lhsT=ktk[:], rhs=vt[:, ch, :], start=False, stop=True),
               r=[(tag, "ktk", qi), (vkey, ch // 4)], w=[pk3])
            op("act", lambda e: e.activation(out=St[:], in_=ps3[:, 0:128], func=AF.Copy, scale=eend[:, ch:ch + 1]),
               r=[pk3, tag + "eend"], w=[tag + "St"])

        if stage >= 1:
            for blk in range(NB):
                make_hT(blk, x_in)
                dma("pool", hTbuf[blk], hT[:], r=[("hT", t) for t in range(NT)])
                with ExitStack() as L:
                    psr[:] = [4, 6]
                    pbr[:] = [1, 2]
                    bufs = dict(fb=kb.sb(L, "fb", [128, T], F32), lb=kb.sb(L, "lbuf", [128, T], F32), eb=kb.sb(L, "eb", [128, T], F32))
                    wts = [kb.sb(L, f"whA{i}", [128, 8, 5, 128], BF16) for i in range(2)]
                    chains = []
                    for h in range(4):
                        wt = wts[h % 2]
                        wkey = ("whA", h % 2)
                        for j, c0 in ((2, 2048), (3, 2560)):
                            dma("pool", wt[:, :, j, :], w_in.rearrange("(c p) n -> p c n", p=128)[:, :, c0 + h * 128:c0 + (h + 1) * 128],
                                w=[wkey])
                        tag = f"A{h}"
                        vt = kb.sb(L, f"vtA{h}", [64, NCH, 128], BF16)
                        Kt = kb.sb(L, f"KtA{h}", [128, T], BF16)
                        eend = kb.sb(L, f"eendA{h}", [128, NCH], F32)
                        ltot = kb.sb(L, f"ltotA{h}", [128, 1], F32)
                        St = kb.sb(L, f"StA{h}", [128, 128], BF16)
                        proj_tok(wt, wkey, 3, vt, tag + "vt", AF.Copy)
                        hgrn_prep(L, wt, wkey, h, 1, None, False, bufs, tag=tag, Qt=None, Kt=Kt, eend=eend, ltot=ltot[:])
                        op("dve", lambda e: e.memset(St[:], 0.0), w=[tag + "St"])
                        chains.append(dict(d=1, tag=tag, Qt=None, Kt=Kt, eend=eend, vt=vt, St=St, vkey=tag + "vt", n=0, emit_o=False,
                                           ktk=[kb.sb(L, f"ktkA{h}_{i}", [64, 128], BF16) for i in range(2)], sc=None, o=None, ltot=ltot, h=h))
                    for k_ in range(NCH):
                        for c in chains:
                            scan_chunk(c, NCH - 1 - k_)
                    for c in chains:
                        h = c["h"]
                        op("act", lambda e: e.copy(out=Bb[:, blk, h, :], in_=c["St"][:]), r=[c["tag"] + "St"], w=[("Bb", blk, h)])
                        op("act", lambda e: e.activation(out=Ab[:, blk, h:h + 1], in_=c["ltot"][:], func=AF.Exp),
                           r=[c["tag"] + "ltot"], w=[("Ab", blk, h)])
                    kb.barrier()
                    psr[:] = [0, 6]
                    pbr[:] = [0, 2]
            op("dve", lambda e: e.memset(Sin_b[:, NB - 1, :, :], 0.0), w=[("Sin", NB - 1)])
            for blk in range(NB - 2, -1, -1):
                for h in range(4):
                    op("dve", lambda e, blk=blk, h=h: e.scalar_tensor_tensor(
                        out=Sin_b[:, blk, h, :], in0=Sin_b[:, blk + 1, h, :], scalar=Ab[:, blk + 1, h:h + 1],
                        in1=Bb[:, blk + 1, h, :], op0=ALU.mult, op1=ALU.add),
                        r=[("Sin", blk + 1), ("Ab", blk + 1, h), ("Bb", blk + 1, h)], w=[("Sin", blk)])
            kb.barrier()

        if stage >= 2:
            op("dve", lambda e: e.memset(Sf[:], 0.0), w=[("Sf", h_) for h_ in range(4)])
            for blk in range(NB):
                dma("sp", hT[:], hTbuf[blk], w=[("hT", t) for t in range(NT)])
                with ExitStack() as LB:
                    catT = kb.sb(LB, "catT", [128, 8, T], BF16)
                    with ExitStack() as L:
                        wuv = kb.sb(L, "wuv", [128, 8, 1024], BF16)
                        load_w(wuv[:, :, 0:512], 0, 512, "wu")
                        load_w(wuv[:, :, 512:1024], 512, 512, "wv")
                        gus = [kb.sb(L, f"gu{i}", [128, 512], F32) for i in range(2)]
                        gvs = [kb.sb(L, f"gv{i}", [128, 512], F32) for i in range(2)]
                        vnb = [kb.sb(L, f"vnb{i}", [128, 512], BF16) for i in range(2)]
                        aob = [kb.sb(L, f"aob{i}", [128, 512], BF16) for i in range(2)]
                        st6 = [kb.sb(L, f"st6{i}", [128, 6], F32) for i in range(2)]
                        mv = [kb.sb(L, f"mv{i}", [128, 2], F32) for i in range(2)]
                        for t in range(NT):
                            i = t % 2
                            ts = slice(t * 128, (t + 1) * 128)
                            psu, pku = next_ps()
                            psv, pkv = next_ps()
                            for c in range(8):
                                op("pe", lambda e, c=c: e.matmul(psu[:, :], lhsT=hT[:, c, ts], rhs=wuv[:, c, 0:512],
                                                                  start=(c == 0), stop=(c == 7)), r=[("hT", t), "wu"], w=[pku])
                            for c in range(8):
                                op("pe", lambda e, c=c: e.matmul(psv[:, :], lhsT=hT[:, c, ts], rhs=wuv[:, c, 512:1024],
                                                                  start=(c == 0), stop=(c == 7)), r=[("hT", t), "wv"], w=[pkv])
                            op("act", lambda e: e.activation(out=gus[i][:], in_=psu[:, :], func=AF.Gelu_apprx_tanh), r=[pku], w=[("gu", i)])
                            op("act", lambda e: e.activation(out=gvs[i][:], in_=psv[:, :], func=AF.Gelu_apprx_tanh), r=[pkv], w=[("gv", i)])
                            op("dve", lambda e: e.bn_stats(out=st6[i][:], in_=gvs[i][:]), r=[("gv", i)], w=[("st6", i)])
                            op("dve", lambda e: e.bn_aggr(out=mv[i][:], in_=st6[i][:]), r=[("st6", i)], w=[("mv", i)])
                            op("act", lambda e: e.activation(out=mv[i][:, 1:2], in_=mv[i][:, 1:2], func=AF.Sqrt, bias=epsc[:], scale=1.0),
                               r=[("mv", i), "epsc"], w=[("mv", i)])
                            op("dve", lambda e: e.reciprocal(out=mv[i][:, 1:2], in_=mv[i][:, 1:2]), r=[("mv", i)], w=[("mv", i)])
                            op("dve", lambda e: e.tensor_scalar(out=gvs[i][:], in0=gvs[i][:], scalar1=mv[i][:, 0:1], scalar2=mv[i][:, 1:2],
                                                                op0=ALU.subtract, op1=ALU.mult), r=[("gv", i), ("mv", i)], w=[("gv", i)])
                            op("dve", lambda e: e.tensor_tensor(out=gvs[i][:], in0=gvs[i][:], in1=lng[:], op=ALU.mult),
                               r=[("gv", i), "lng"], w=[("gv", i)])
                            op("dve", lambda e: e.tensor_tensor(out=vnb[i][:], in0=gvs[i][:], in1=lnb[:], op=ALU.add),
                               r=[("gv", i), "lnb"], w=[("vnb", i)])
                            psm, pkm = next_ps()
                            for g in range(4):
                                op("pe", lambda e, g=g: e.matmul(psm[:, g * 128:(g + 1) * 128], lhsT=wsT[:, g, :],
                                                                  rhs=vnb[i][:, g * 128:(g + 1) * 128], start=True, stop=True),
                                   r=["wsT", ("vnb", i)], w=[pkm])
                            op("dve", lambda e: e.tensor_tensor(out=gvs[i][:], in0=psm[:, :], in1=bsf[:], op=ALU.add),
                               r=[pkm, "bsf"], w=[("gv", i)])
                            op("dve", lambda e: e.tensor_tensor(out=aob[i][:], in0=gvs[i][:], in1=gus[i][:], op=ALU.mult),
                               r=[("gv", i), ("gu", i)], w=[("aob", i)])
                            pb, pbk = next_pb()
                            for g in range(4):
                                op("pe", lambda e, g=g: e.transpose(out=pb[:, g * 128:(g + 1) * 128], in_=aob[i][:, g * 128:(g + 1) * 128],
                                                                    identity=identb[:]), r=[("aob", i), "identb"], w=[pbk])
                            op("act", lambda e: e.copy(out=catT[:, 0:4, ts], in_=pb[:, 0:512].rearrange("p (c t) -> p c t", c=4)),
                               r=[pbk], w=[("catA", t)])
                        kb.barrier()
                    with ExitStack() as L:
                        psr[:] = [4, 6]
                        pbr[:] = [1, 2]
                        bufs = dict(fb=kb.sb(L, "fbB", [128, T], F32), lb=kb.sb(L, "lbufB", [128, T], F32), eb=kb.sb(L, "ebB", [128, T], F32))
                        wts = [kb.sb(L, f"whB{i}", [128, 8, 5, 128], BF16) for i in range(2)]
                        qf = kb.sb(L, "qf", [128, T], F32)
                        osq = kb.sb(L, "osq", [64, NCH, 128], F32)
                        osum = kb.sb(L, "osum", [64, NCH, 128], F32)
                        ssq = kb.sb(L, "ssq", [64, NCH], F32)
                        obf = kb.sb(L, "obf", [64, NCH, 128], BF16)
                        per = []
                        for hq in range(2):
                            per.append(dict(
                                vt=kb.sb(L, f"vtB{hq}", [64, NCH, 128], BF16), sg=kb.sb(L, f"sgB{hq}", [64, NCH, 128], BF16),
                                Qt=[kb.sb(L, f"QtB{hq}{d}", [128, T], BF16) for d in range(2)],
                                Kt=[kb.sb(L, f"KtB{hq}{d}", [128, T], BF16) for d in range(2)],
                                eend=[kb.sb(L, f"eendB{hq}{d}", [128, NCH], F32) for d in range(2)],
                                St=[kb.sb(L, f"StB{hq}{d}", [128, 128], BF16) for d in range(2)],
                                o=[kb.sb(L, f"oB{hq}{d}", [64, NCH, 128], BF16) for d in range(2)],
                                ktk=[[kb.sb(L, f"ktkB{hq}{d}{i}", [64, 128], BF16) for i in range(2)] for d in range(2)],
                                sc=[[kb.sb(L, f"scB{hq}{d}{i}", [64, 64], BF16) for i in range(2)] for d in range(2)]))
                        for hp2 in range(2):
                            chains = []
                            for hq in range(2):
                                h = hp2 * 2 + hq
                                pp = per[hq]
                                wt = wts[hq]
                                wkey = ("whB", hq)
                                for j in range(5):
                                    c0 = 1024 + j * 512
                                    dma("pool", wt[:, :, j, :], w_in.rearrange("(c p) n -> p c n", p=128)[:, :, c0 + h * 128:c0 + (h + 1) * 128],
                                        w=[wkey])

                                def evac_q(tb, ps, pk):
                                    op("act", lambda e: e.activation(out=qf[:, tb * 512:(tb + 1) * 512], in_=ps[:, :], func=AF.Silu),
                                       r=[pk], w=["qf"])
                                proj_fm(wt, wkey, 0, evac_q)
                                proj_tok(wt, wkey, 3, pp["vt"], f"B{hq}vt", AF.Copy)
                                proj_tok(wt, wkey, 4, pp["sg"], f"B{hq}sg", AF.Sigmoid)
                                for d in range(2):
                                    tag = f"B{hq}{d}"
                                    hgrn_prep(L, wt, wkey, h, d, qf, True, bufs, tag=tag, Qt=pp["Qt"][d], Kt=pp["Kt"][d], eend=pp["eend"][d])
                                    if d == 0:
                                        op("act", lambda e: e.copy(out=pp["St"][0][:], in_=Sf[:, h, :]), r=[("Sf", h)], w=[tag + "St"])
                                    else:
                                        op("act", lambda e: e.copy(out=pp["St"][1][:], in_=Sin_b[:, blk, h, :]), r=[("Sin", blk)], w=[tag + "St"])
                                    chains.append(dict(d=d, tag=tag, Qt=pp["Qt"][d], Kt=pp["Kt"][d], eend=pp["eend"][d], vt=pp["vt"], St=pp["St"][d],
                                                       vkey=f"B{hq}vt", n=0, emit_o=True, ktk=pp["ktk"][d], sc=pp["sc"][d], o=pp["o"][d], h=h, hq=hq))
                            for k_ in range(NCH):
                                for c in chains:
                                    scan_chunk(c, k_ if c["d"] == 0 else NCH - 1 - k_)
                            for hq in range(2):
                                h = hp2 * 2 + hq
                                pp = per[hq]
                                op("act", lambda e: e.copy(out=Sf[:, h, :], in_=pp["St"][0][:]), r=[f"B{hq}0St"], w=[("Sf", h)])
                                okeys = [(f"B{hq}{d}", "o", ch) for d in range(2) for ch in range(NCH)]
                                op("dve", lambda e: e.tensor_tensor(out=osum[:], in0=pp["o"][0][:], in1=pp["o"][1][:], op=ALU.add), r=okeys, w=["osum"])
                                op("dve", lambda e: e.tensor_tensor(out=osq[:], in0=osum[:], in1=osum[:], op=ALU.mult), r=["osum"], w=["osq"])
                                op("dve", lambda e: e.tensor_reduce(out=ssq[:], in_=osq[:], axis=AX.X, op=ALU.add), r=["osq"], w=["ssq"])
                                op("act", lambda e: e.activation(out=ssq[:], in_=ssq[:], func=AF.Sqrt, bias=epsc[0:64, :], scale=1.0 / 128),
                                   r=["ssq", "epsc"], w=["ssq"])
                                op("dve", lambda e: e.reciprocal(out=ssq[:], in_=ssq[:]), r=["ssq"], w=["ssq"])
                                op("dve", lambda e: e.tensor_tensor(out=osq[:], in0=osum[:], in1=ssq[:, :].unsqueeze(2).to_broadcast([64, NCH, 128]), op=ALU.mult),
                                   r=["osum", "ssq"], w=["osq"])
                                op("dve", lambda e: e.tensor_tensor(out=osq[:], in0=osq[:],
                                                                    in1=bng[:, h * 128:(h + 1) * 128].unsqueeze(1).to_broadcast([64, NCH, 128]),
                                                                    op=ALU.mult), r=["osq", "bng"], w=["osq"])
                                op("dve", lambda e: e.tensor_tensor(out=obf[:], in0=osq[:], in1=pp["sg"][:], op=ALU.mult),
                                   r=["osq"] + [(f"B{hq}sg", g4) for g4 in range(NCH // 4)], w=["obf"])
                                for g8 in range(NCH // 8):
                                    pb, pbk = next_pb()
                                    for cc in range(8):
                                        ch = g8 * 8 + cc
                                        op("pe", lambda e: e.transpose(out=pb[:, cc * 64:(cc + 1) * 64], in_=obf[:, ch, :],
                                                                       identity=identb[0:64, 0:64]), r=["obf", "identb"], w=[pbk])
                                    op("act", lambda e: e.copy(out=catT[:, 4 + h, g8 * 512:(g8 + 1) * 512], in_=pb[:, 0:512]),
                                       r=[pbk], w=[("catB", h, g8)])
                        kb.barrier()
                        psr[:] = [0, 6]
                        pbr[:] = [0, 2]
                    with ExitStack() as L:
                        wo = kb.sb(L, "wo", [128, 8, D], BF16)
                        dma("pool", wo[:], w_out.rearrange("(c p) n -> p c n", p=128), w=["wo"])
                        xts = [kb.sb(L, f"ox{i}", [128, D], F32) for i in range(2)]
                        for t in range(NT):
                            i = t % 2
                            ts = slice(t * 128, (t + 1) * 128)
                            r0 = blk * T + t * 128
                            dma("sp", xts[i][:], x_in[r0:r0 + 128, :], w=[("ox", i)])
                            for hf in range(2):
                                ps, pk = next_ps()
                                for c in range(8):
                                    op("pe", lambda e, c=c: e.matmul(ps[:, :], lhsT=catT[:, c, ts], rhs=wo[:, c, hf * 512:(hf + 1) * 512],
                                                                      start=(c == 0), stop=(c == 7)), r=["wo"], w=[pk])
                                op("dve", lambda e: e.tensor_tensor(out=xts[i][:, hf * 512:(hf + 1) * 512], in0=ps[:, :],
                                                                    in1=xts[i][:, hf * 512:(hf + 1) * 512], op=ALU.add),
                                   r=[pk, ("ox", i)], w=[("ox", i)])
                            dma("pool", xbuf[r0:r0 + 128, :], xts[i][:], r=[("ox", i)])
                        kb.barrier()
        P.close()
        kb.barrier()


    def phase_moe(layer):
        P = ExitStack()
        NTT = S // 128
        aff = kb.sb(P, "aff", [128, NE, NTT], F32)
        pos = kb.sb(P, "pos", [128, NE, NTT], F32)
        gsel = kb.sb(P, "gsel", [128, NE, NTT], F32)
        onesf = kb.sb(P, "onesf", [128, 128], F32)
        load_gain(1 + 2 * layer)
        dma("sp", onesf[:], ones_in[:, :], w=["onesf"])
        with ExitStack() as L:
            wr = kb.sb(L, "wr", [128, 8, NE], F32)
            dma("sp", wr[:], w_router[layer].rearrange("(c p) e -> p c e", p=128), w=["wr"])
            xts = [kb.sb(L, f"rx{i}", [128, D], F32) for i in range(2)]
            hfs = [kb.sb(L, f"rhf{i}", [128, D], F32) for i in range(2)]
            hbs = [kb.sb(L, f"rhb{i}", [128, D], BF16) for i in range(2)]
            hfT = [kb.sb(L, f"rhT{i}", [128, 8, 128], F32) for i in range(2)]
            junk = kb.sb(L, "rjunk", [128, D], BF16)
            sss = [kb.sb(L, f"rss{i}", [128, 1], F32) for i in range(2)]
            rss = [kb.sb(L, f"rrs{i}", [128, 1], F32) for i in range(2)]
            lg = [kb.sb(L, f"rlg{i}", [128, NE], F32) for i in range(2)]
            mx = [kb.sb(L, f"rmx{i}", [128, 1], F32) for i in range(2)]
            sm = [kb.sb(L, f"rsm{i}", [128, 1], F32) for i in range(2)]
            for t in range(NTT):
                i = t % 2
                tg = f"r{i}"
                dma("sp", xts[i][:], xbuf[t * 128:(t + 1) * 128, :], w=[tg + "x"])
                op("act", lambda e: e.activation(out=junk[:], in_=xts[i][:], func=AF.Square, accum_out=sss[i][:]),
                   r=[tg + "x"], w=[tg + "ss"])
                op("act", lambda e: e.activation(out=rss[i][:], in_=sss[i][:], func=AF.Sqrt, bias=epsc[:], scale=1.0 / D),
                   r=[tg + "ss", "epsc"], w=[tg + "rs"])
                op("dve", lambda e: e.reciprocal(out=rss[i][:], in_=rss[i][:]), r=[tg + "rs"], w=[tg + "rs"])
                op("dve", lambda e: e.scalar_tensor_tensor(out=hfs[i][:], in0=xts[i][:], scalar=rss[i][:, 0:1], in1=gain_sb[:],
                                                           op0=ALU.mult, op1=ALU.mult), r=[tg + "x", tg + "rs", "gain"], w=[tg + "hf"])
                op("act", lambda e: e.copy(out=hbs[i][:], in_=hfs[i][:]), r=[tg + "hf"], w=[tg + "hb"])
                dma("pool", h2buf[t * 128:(t + 1) * 128, :], hbs[i][:], r=[tg + "hb"])
                for hh in range(2):
                    ps, pk = next_ps()
                    for c4 in range(4):
                        c = hh * 4 + c4
                        op("pe", lambda e: e.transpose(out=ps[:, c4 * 128:(c4 + 1) * 128], in_=hfs[i][:, c * 128:(c + 1) * 128],
                                                       identity=identf[:]), r=[tg + "hf", "identf"], w=[pk])
                    op("act", lambda e: e.copy(out=hfT[i][:, hh * 4:(hh + 1) * 4, :], in_=ps[:, :].rearrange("p (c t) -> p c t", c=4)),
                       r=[pk], w=[tg + "hT"])
                ps, pk = next_ps()
                for c in range(8):
                    op("pe", lambda e: e.matmul(ps[:, 0:NE], lhsT=hfT[i][:, c, :], rhs=wr[:, c, :], start=(c == 0), stop=(c == 7)),
                       r=[tg + "hT", "wr"], w=[pk])
                op("dve", lambda e: e.tensor_reduce(out=mx[i][:], in_=ps[:, 0:NE], axis=AX.X, op=ALU.max), r=[pk], w=[tg + "mx"])
                op("dve", lambda e: e.tensor_scalar(out=mx[i][:], in0=mx[i][:], scalar1=-1.0, scalar2=None, op0=ALU.mult),
                   r=[tg + "mx"], w=[tg + "mx"])
                op("act", lambda e: e.activation(out=lg[i][:], in_=ps[:, 0:NE], func=AF.Exp, bias=mx[i][:], scale=1.0, accum_out=sm[i][:]),
                   r=[pk, tg + "mx"], w=[tg + "lg", tg + "sm"])
                op("dve", lambda e: e.reciprocal(out=sm[i][:], in_=sm[i][:]), r=[tg + "sm"], w=[tg + "sm"])
                op("dve", lambda e: e.tensor_scalar(out=aff[:, :, t], in0=lg[i][:], scalar1=sm[i][:, 0:1], scalar2=None, op0=ALU.mult),
                   r=[tg + "lg", tg + "sm"], w=["aff"])
            kb.barrier()
        if stage == 30:
            P.close()
            return
        with ExitStack() as L:
            lo = kb.sb(L, "bs_lo", [128, NE], F32)
            mid = kb.sb(L, "bs_mid", [128, NE], F32)
            msk = kb.sb(L, "bs_msk", [128, NE, NTT], F32)
            cnt = kb.sb(L, "bs_cnt", [128, NE], F32)
            cmp_ = kb.sb(L, "bs_cmp", [128, NE], F32)
            op("dve", lambda e: e.memset(lo[:], 0.0), w=["lo"])
            for it in range(30):
                wv = 2.0 ** (-(it + 1))
                op("dve", lambda e: e.tensor_scalar(out=mid[:], in0=lo[:], scalar1=wv, scalar2=None, op0=ALU.add), r=["lo"], w=["mid"])
                op("dve", lambda e: e.tensor_tensor(out=msk[:], in0=aff[:], in1=mid[:, :].unsqueeze(2).to_broadcast([128, NE, NTT]),
                                                    op=ALU.is_ge), r=["aff", "mid"], w=["msk"])
                op("dve", lambda e: e.tensor_reduce(out=cnt[:], in_=msk[:], axis=AX.X, op=ALU.add), r=["msk"], w=["cnt"])
                ps, pk = next_ps()
                op("pe", lambda e: e.matmul(ps[:, 0:NE], lhsT=onesf[:], rhs=cnt[:], start=True, stop=True), r=["onesf", "cnt"], w=[pk])
                op("dve", lambda e: e.tensor_scalar(out=cmp_[:], in0=ps[:, 0:NE], scalar1=CAP - 0.5, scalar2=None, op0=ALU.is_ge),
                   r=[pk], w=["cmp"])
                op("dve", lambda e: e.scalar_tensor_tensor(out=lo[:], in0=cmp_[:], scalar=wv, in1=lo[:], op0=ALU.mult, op1=ALU.add),
                   r=["cmp", "lo"], w=["lo"])
            lst = kb.sb(L, "lstrict", [128, 128], BF16)
            onesb = kb.sb(L, "onesb", [128, 128], BF16)
            rm2 = kb.sb(L, "rm2", [128, NE, NTT], F32)
            mskb = kb.sb(L, "mskb", [128, NE, NTT], BF16)
            wit = kb.sb(L, "wit", [128, NE, NTT], F32)
            tot = kb.sb(L, "tot", [128, NE, NTT], F32)
            dma("pool", lst[:], lstrict_in[:, :], w=["lst"])
            dma("sp", rm2[:], rmask2_in[:, :].rearrange("p (e t) -> p e t", e=NE), w=["rm2"])
            op("dve", lambda e: e.tensor_copy(out=onesb[:], in_=onesf[:]), r=["onesf"], w=["onesb"])
            op("dve", lambda e: e.tensor_tensor(out=msk[:], in0=aff[:], in1=lo[:, :].unsqueeze(2).to_broadcast([128, NE, NTT]),
                                                op=ALU.is_ge), r=["aff", "lo"], w=["msk"])
            op("dve", lambda e: e.tensor_tensor(out=gsel[:], in0=aff[:], in1=msk[:], op=ALU.mult), r=["aff", "msk"], w=["gsel"])
            op("dve", lambda e: e.tensor_copy(out=mskb[:], in_=msk[:]), r=["msk"], w=["mskb"])
            mflat = mskb[:, :, :].rearrange("p e t -> p (e t)")
            for hf2 in range(2):
                ps, pk = next_ps()
                op("pe", lambda e: e.matmul(ps[:, :], lhsT=lst[:], rhs=mflat[:, hf2 * 512:(hf2 + 1) * 512], start=True, stop=True),
                   r=["lst", "mskb"], w=[pk])
                op("act", lambda e: e.copy(out=wit[:, hf2 * 8:(hf2 + 1) * 8, :], in_=ps[:, :].rearrange("p (e t) -> p e t", e=8)),
                   r=[pk], w=["wit"])
                ps2, pk2 = next_ps()
                op("pe", lambda e: e.matmul(ps2[:, :], lhsT=onesb[:], rhs=mflat[:, hf2 * 512:(hf2 + 1) * 512], start=True, stop=True),
                   r=["onesb", "mskb"], w=[pk2])
                op("act", lambda e: e.copy(out=tot[:, hf2 * 8:(hf2 + 1) * 8, :], in_=ps2[:, :].rearrange("p (e t) -> p e t", e=8)),
                   r=[pk2], w=["tot"])
            op("dve", lambda e: e.tensor_tensor_scan(out=pos[:, :, :].rearrange("p e t -> p (e t)"),
                                                     data0=rm2[:, :, :].rearrange("p e t -> p (e t)"),
                                                     data1=tot[:, :, :].rearrange("p e t -> p (e t)"), initial=0.0,
                                                     op0=ALU.mult, op1=ALU.add), r=["rm2", "tot"], w=["pos"])
            op("dve", lambda e: e.tensor_tensor(out=pos[:], in0=pos[:], in1=tot[:], op=ALU.subtract), r=["pos", "tot"], w=["pos"])
            op("dve", lambda e: e.tensor_tensor(out=pos[:], in0=pos[:], in1=wit[:], op=ALU.add), r=["pos", "wit"], w=["pos"])
            op("dve", lambda e: e.scalar_tensor_tensor(out=pos[:], in0=pos[:], scalar=1.0, in1=msk[:], op0=ALU.add, op1=ALU.mult),
               r=["pos", "msk"], w=["pos"])
            op("dve", lambda e: e.tensor_scalar(out=pos[:], in0=pos[:], scalar1=-1.0, scalar2=None, op0=ALU.add), r=["pos"], w=["pos"])
            kb.barrier()
        with ExitStack() as L:
            iota = kb.sb(L, "iota", [128, CAP], F32)
            tokd = kb.sb(L, "tokd", [128, NTT, 2], F32)
            Eb = [kb.sb(L, f"Eb{i}", [128, CAP], BF16) for i in range(2)]
            rhs4 = [kb.sb(L, f"rhs4{i}", [128, NTT, 4], BF16) for i in range(2)]
            gtmp = kb.sb(L, "gtmp", [128, NTT], F32)
            sl4 = kb.sb(L, "sl4", [4, CAP], F32)
            slT = kb.sb(L, "slT", [128, 8, 4], F32)
            idxf = kb.sb(L, "idxf", [128, 8], F32)
            idxs = [kb.sb(L, f"idxi{i}", [128, 8], I32) for i in range(2)]
            gss = [kb.sb(L, f"gs{i}", [128, 8], F32) for i in range(2)]
            xe = kb.sb(L, "xe", [128, 8, D], BF16)
            xeT = kb.sb(L, "xeT", [128, 8, CAP], BF16)
            hTm = kb.sb(L, "hTm", [128, 8, CAP], BF16)
            sgt = [kb.sb(L, f"sgt{i}", [128, 512], F32) for i in range(2)]
            yts = [kb.sb(L, f"yt{i}", [128, D], F32) for i in range(2)]
            Wg = [kb.sb(L, f"Wg{i}", [128, 8, D], BF16) for i in range(2)]
            Wu = [kb.sb(L, f"Wu{i}", [128, 8, D], BF16) for i in range(2)]
            Wd = [kb.sb(L, f"Wd{i}", [128, 8, D], BF16) for i in range(2)]
            dma("sp", iota[:], iota_in[:, :], w=["iota"])
            dma("sp", tokd[:], tokdig_in[:, :, :], w=["tokd"])
            for i in range(2):
                op("dve", lambda e: e.tensor_copy(out=rhs4[i][:, :, 0:2], in_=tokd[:]), r=["tokd"], w=[("rhs4", i)])

            def load_expert(ex):
                i = ex % 2
                for nm, wt_, src in (("Wg", Wg[i], w_gate), ("Wu", Wu[i], w_up), ("Wd", Wd[i], w_down)):
                    for c2 in range(2):
                        dma("pool", wt_[:, c2 * 4:(c2 + 1) * 4, :],
                            src[layer, ex].rearrange("(c p) n -> p c n", p=128)[:, c2 * 4:(c2 + 1) * 4, :], w=[(nm, i)])

            load_expert(0)
            for ex in range(NE):
                i = ex % 2
                if ex + 1 < NE:
                    load_expert(ex + 1)
                op("dve", lambda e: e.tensor_copy(out=rhs4[i][:, :, 2], in_=gsel[:, ex, :]), r=["gsel"], w=[("rhs4", i)])
                op("dve", lambda e: e.tensor_tensor(out=gtmp[:], in0=gsel[:, ex, :], in1=rhs4[i][:, :, 2], op=ALU.subtract),
                   r=["gsel", ("rhs4", i)], w=["gtmp"])
                op("dve", lambda e: e.tensor_copy(out=rhs4[i][:, :, 3], in_=gtmp[:]), r=["gtmp"], w=[("rhs4", i)])
                psA, pkA = next_ps()
                psB, pkB = next_ps()
                for t in range(NTT):
                    j = t % 2
                    op("dve", lambda e: e.tensor_scalar(out=Eb[j][:], in0=iota[:], scalar1=pos[:, ex, t:t + 1], scalar2=None,
                                                        op0=ALU.is_equal), r=["iota", "pos"], w=[("Eb", j)])
                    op("pe", lambda e: e.matmul(psA[0:4, :], lhsT=rhs4[i][:, t, :], rhs=Eb[j][:, 0:512], start=(t == 0), stop=(t == NTT - 1)),
                       r=[("rhs4", i), ("Eb", j)], w=[pkA])
                    op("pe", lambda e: e.matmul(psB[0:4, :], lhsT=rhs4[i][:, t, :], rhs=Eb[j][:, 512:1024], start=(t == 0), stop=(t == NTT - 1)),
                       r=[("rhs4", i), ("Eb", j)], w=[pkB])
                op("act", lambda e: e.copy(out=sl4[:, 0:512], in_=psA[0:4, :]), r=[pkA], w=["sl4"])
                op("act", lambda e: e.copy(out=sl4[:, 512:1024], in_=psB[0:4, :]), r=[pkB], w=["sl4"])
                ps, pk = next_ps()
                for sbk in range(8):
                    op("pe", lambda e: e.transpose(out=ps[:, sbk * 4:(sbk + 1) * 4], in_=sl4[:, sbk * 128:(sbk + 1) * 128],
                                                   identity=identf[0:4, 0:4]), r=["sl4", "identf"], w=[pk])
                op("act", lambda e: e.copy(out=slT[:], in_=ps[:, 0:32].rearrange("p (s c) -> p s c", c=4)), r=[pk], w=["slT"])
                op("dve", lambda e: e.scalar_tensor_tensor(out=idxf[:], in0=slT[:, :, 0], scalar=128.0, in1=slT[:, :, 1],
                                                           op0=ALU.mult, op1=ALU.add), r=["slT"], w=["idxf"])
                op("dve", lambda e: e.tensor_copy(out=idxs[i][:], in_=idxf[:]), r=["idxf"], w=[("idx", i)])
                op("dve", lambda e: e.tensor_tensor(out=gss[i][:], in0=slT[:, :, 2], in1=slT[:, :, 3], op=ALU.add), r=["slT"], w=[("gs", i)])
                for sbk in range(8):
                    kb.idma(out=xe[:, sbk, :], in_=h2buf[:, :], in_offset=bass.IndirectOffsetOnAxis(ap=idxs[i][:, sbk:sbk + 1], axis=0),
                            r=[("idx", i)], w=[("xe", sbk)])
                for sbk in range(8):
                    pb, pbk = next_pb()
                    for c in range(8):
                        op("pe", lambda e: e.transpose(out=pb[:, c * 128:(c + 1) * 128], in_=xe[:, sbk, c * 128:(c + 1) * 128],
                                                       identity=identb[:]), r=[("xe", sbk), "identb"], w=[pbk])
                    op("act", lambda e: e.copy(out=xeT[:, :, sbk * 128:(sbk + 1) * 128], in_=pb[:, :].rearrange("p (c t) -> p c t", c=8)),
                       r=[pbk], w=[("xeT", sbk // 4)])
                for fb in range(8):
                    for sh in range(2):
                        psg, pkg = next_ps()
                        psu, pku = next_ps()
                        for c in range(8):
                            op("pe", lambda e: e.matmul(psg[:, :], lhsT=Wg[i][:, c, fb * 128:(fb + 1) * 128], rhs=xeT[:, c, sh * 512:(sh + 1) * 512],
                                                        start=(c == 0), stop=(c == 7)), r=[("Wg", i), ("xeT", sh)], w=[pkg])
                        for c in range(8):
                            op("pe", lambda e: e.matmul(psu[:, :], lhsT=Wu[i][:, c, fb * 128:(fb + 1) * 128], rhs=xeT[:, c, sh * 512:(sh + 1) * 512],
                                                        start=(c == 0), stop=(c == 7)), r=[("Wu", i), ("xeT", sh)], w=[pku])
                        k2 = (fb * 2 + sh) % 2
                        op("act", lambda e: e.activation(out=sgt[k2][:], in_=psg[:, :], func=AF.Silu), r=[pkg], w=[("sgt", k2)])
                        op("dve", lambda e: e.tensor_tensor(out=hTm[:, fb, sh * 512:(sh + 1) * 512], in0=psu[:, :], in1=sgt[k2][:], op=ALU.mult),
                           r=[pku, ("sgt", k2)], w=[("hTm", fb, sh)])
                for sbk in range(8):
                    k2 = sbk % 2
                    for dh in range(2):
                        ps, pk = next_ps()
                        for fb in range(8):
                            op("pe", lambda e: e.matmul(ps[:, :], lhsT=hTm[:, fb, sbk * 128:(sbk + 1) * 128], rhs=Wd[i][:, fb, dh * 512:(dh + 1) * 512],
                                                        start=(fb == 0), stop=(fb == 7)), r=[("Wd", i), ("hTm", fb, sbk // 4)], w=[pk])
                        op("act", lambda e: e.activation(out=yts[k2][:, dh * 512:(dh + 1) * 512], in_=ps[:, :], func=AF.Copy,
                                                         scale=gss[i][:, sbk:sbk + 1]), r=[pk, ("gs", i)], w=[("yt", k2)])
                    kb.idma(out=xbuf[:, :], out_offset=bass.IndirectOffsetOnAxis(ap=idxs[i][:, sbk:sbk + 1], axis=0), in_=yts[k2][:],
                            compute_op=ALU.add, r=[("yt", k2), ("idx", i)] + [("xs", ex - 1, jj) for jj in range(8)], w=[("xs", ex, sbk)])
            kb.barrier()
        P.close()
        kb.barrier()

    def phase_final():
        load_gain(4)
        with ExitStack() as L:
            xts = [kb.sb(L, f"fx{i}", [128, D], F32) for i in range(2)]
            ots = [kb.sb(L, f"fo{i}", [128, D], F32) for i in range(2)]
            junk = kb.sb(L, "fjunk", [128, D], BF16)
            sss = [kb.sb(L, f"fss{i}", [128, 1], F32) for i in range(2)]
            rss = [kb.sb(L, f"frs{i}", [128, 1], F32) for i in range(2)]
            for t in range(S // 128):
                i = t % 2
                tg = f"f{i}"
                dma("sp", xts[i][:], xbuf[t * 128:(t + 1) * 128, :], w=[tg + "x"])
                if do_final_norm:
                    op("act", lambda e: e.activation(out=junk[:], in_=xts[i][:], func=AF.Square, accum_out=sss[i][:]),
                       r=[tg + "x"], w=[tg + "ss"])
                    op("act", lambda e: e.activation(out=rss[i][:], in_=sss[i][:], func=AF.Sqrt, bias=epsc[:], scale=1.0 / D),
                       r=[tg + "ss", "epsc"], w=[tg + "rs"])
                    op("dve", lambda e: e.reciprocal(out=rss[i][:], in_=rss[i][:]), r=[tg + "rs"], w=[tg + "rs"])
                    op("dve", lambda e: e.scalar_tensor_tensor(out=ots[i][:], in0=xts[i][:], scalar=rss[i][:, 0:1], in1=gain_sb[:],
                                                               op0=ALU.mult, op1=ALU.mult), r=[tg + "x", tg + "rs", "gain"], w=[tg + "o"])
                    dma("pool", out_d[t * 128:(t + 1) * 128, :], ots[i][:], r=[tg + "o"], w=[("outd", t)])
                else:
                    dma("pool", out_d[t * 128:(t + 1) * 128, :], xts[i][:], r=[tg + "x"], w=[("outd", t)])
            kb.barrier()

    PV = 1088
    KP = 1024
    DILS = (1, 4, 16)

    def phase_attn():
        P = ExitStack()
        load_gain(2)
        with ExitStack() as L:
            rbl = kb.sb(L, "rbl", [33, 16], F32)
            gq = kb.sb(L, "gq", [33, 3, 383], F32)
            vecs = kb.sb(L, "vecs", [16, 3, 383], F32)
            zt = kb.sb(L, "zt", [128, 1040], BF16)
            op("dve", lambda e: e.memset(rbl[:], 1.0), w=["rbl"])
            dma("sp", rbl[0:32, :], rel_bias[:, :], w=["rbl"])
            dma("sp", gq[:], gpat_in[:, :, :], w=["gq"])
            for di in range(3):
                ps, pk = next_ps()
                op("pe", lambda e: e.matmul(ps[0:16, 0:383], lhsT=rbl[:], rhs=gq[:, di, :], start=True, stop=True), r=["rbl", "gq"], w=[pk])
                op("act", lambda e: e.copy(out=vecs[:, di, :], in_=ps[0:16, 0:383]), r=[pk], w=["vecs"])
            dma("sp", vecbuf.rearrange("d h u -> h d u"), vecs[:], r=["vecs"], w=["vecbuf"])
            op("dve", lambda e: e.memset(zt[:], 0.0), w=["zt"])
            for hp in range(8):
                dma("sp", kTbuf[hp, :, 0:KP], zt[:, 0:KP], r=["zt"])
                dma("sp", kTbuf[hp, :, KP + S:KP + S + KP], zt[:, 0:KP], r=["zt"])
            for base in (0, PV + S):
                for a in range(8):
                    dma("sp", vbuf[base + a * 128:base + (a + 1) * 128, :, :], zt[:, :].rearrange("p (h c) -> p h c", h=16), r=["zt"])
                dma("sp", vbuf[base + 1024:base + 1088, :, :], zt[0:64, :].rearrange("p (h c) -> p h c", h=16), r=["zt"])
            kb.barrier()
        with ExitStack() as L:
            wqkv = kb.sb(L, "wqkv", [128, 8, 3072], BF16)
            for j in range(6):
                dma("pool", wqkv[:, :, j * 512:(j + 1) * 512], w_qkv.rearrange("(c p) n -> p c n", p=128)[:, :, j * 512:(j + 1) * 512], w=["wqkv"])
            hT = kb.sb(L, "hT1", [128, 8, T], BF16)
            xts = [kb.sb(L, f"a1x{i}", [128, D], F32) for i in range(2)]
            hbs = [kb.sb(L, f"a1h{i}", [128, D], BF16) for i in range(2)]
            junk = kb.sb(L, "a1junk", [128, D], BF16)
            sss = [kb.sb(L, f"a1ss{i}", [128, 1], F32) for i in range(2)]
            rss = [kb.sb(L, f"a1rs{i}", [128, 1], F32) for i in range(2)]
            qst = [kb.sb(L, f"qst{i}", [128, 512], BF16) for i in range(2)]
            vau = [kb.sb(L, f"vau{i}", [128, 16, 65], BF16) for i in range(2)]
            for i in range(2):
                op("dve", lambda e: e.memset(vau[i][:], 1.0), w=[("vau", i)])
            nq = 0
            for blk in range(NB):
                for t in range(NT):
                    i = t % 2
                    tag = f"a1{i}"
                    r0 = blk * T + t * 128
                    dma("sp", xts[i][:], xbuf[r0:r0 + 128, :], w=[tag + "x"])
                    rms_tile(tag, xts[i][:], hbs[i], junk, sss[i], rss[i], [tag + "x"])
                    pb, pk = next_pb()
                    for c in range(8):
                        op("pe", lambda e: e.transpose(out=pb[:, c * 128:(c + 1) * 128], in_=hbs[i][:, c * 128:(c + 1) * 128], identity=identb[:]),
                           r=[tag + "hb", "identb"], w=[pk])
                    op("act", lambda e: e.copy(out=hT[:, :, t * 128:(t + 1) * 128], in_=pb[:, :].rearrange("p (c t) -> p c t", c=8)),
                       r=[pk], w=[("hT1", t)])
                for which in range(2):
                    for hp in range(8):
                        for tb in range(T // 512):
                            ps, pk = next_ps()
                            col = which * 1024 + hp * 128
                            for c in range(8):
                                op("pe", lambda e: e.matmul(ps[:, :], lhsT=wqkv[:, c, col:col + 128], rhs=hT[:, c, tb * 512:(tb + 1) * 512],
                                                            start=(c == 0), stop=(c == 7)),
                                   r=["wqkv"] + [("hT1", t) for t in range(tb * 4, tb * 4 + 4)], w=[pk])
                            k2 = nq % 2
                            nq += 1
                            op("act", lambda e: e.activation(out=qst[k2][:], in_=ps[:, :], func=AF.Copy, scale=(0.125 if which == 0 else 1.0)),
                               r=[pk], w=[("qst", k2)])
                            c0 = blk * T + tb * 512
                            if which == 0:
                                dma("pool", qTbuf[hp, :, c0:c0 + 512], qst[k2][:], r=[("qst", k2)])
                            else:
                                dma("pool", kTbuf[hp, :, KP + c0:KP + c0 + 512], qst[k2][:], r=[("qst", k2)])
                for t in range(NT):
                    i = t % 2
                    for hf2 in range(2):
                        ps, pk = next_ps()
                        for c in range(8):
                            op("pe", lambda e: e.matmul(ps[:, :], lhsT=hT[:, c, t * 128:(t + 1) * 128], rhs=wqkv[:, c, 2048 + hf2 * 512:2048 + (hf2 + 1) * 512],
                                                        start=(c == 0), stop=(c == 7)), r=["wqkv", ("hT1", t)], w=[pk])
                        op("act", lambda e: e.copy(out=vau[i][:, hf2 * 8:(hf2 + 1) * 8, 0:64], in_=ps[:, :].rearrange("p (h c) -> p h c", h=8)),
                           r=[pk], w=[("vau", i)])
                    r0 = PV + blk * T + t * 128
                    dma("pool", vbuf[r0:r0 + 128, :, :], vau[i][:], r=[("vau", i)])
            kb.barrier()
        if stage == 40:
            P.close()
            return
        AB = 2048
        with ExitStack() as L:
            kw = kb.sb(L, "kw", [128, 4, AB + 2 * KP], BF16)
            qw = kb.sb(L, "qw", [128, 4, AB], BF16)
            Vd = [kb.sb(L, f"Vd{i}", [128, 32, 8, 65], BF16) for i in range(2)]
            BTd = [kb.sb(L, f"BTd{i}", [128, 8, 2, 128], F32) for i in range(2)]
            BTr = kb.sb(L, "BTr", [128, 8, 2, 128], F32)
            sTs = [kb.sb(L, f"sTs{i}", [128, 512], F32) for i in range(4)]
            pTs = [kb.sb(L, f"pTs{i}", [128, 512], BF16) for i in range(4)]
            stg = [kb.sb(L, f"stg{i}", [128, 8, 65], F32) for i in range(2)]
            nv = 0
            ntile = 0
            nh = 0
            for ab in range(S // AB):
                for hh in range(2):
                    for j in range(4):
                        dma("sp", kw[:, j, :], kTbuf[hh * 4 + j, :, ab * AB:ab * AB + AB + 2 * KP], w=[("kw", j)])
                        dma("sp", qw[:, j, :], qTbuf[hh * 4 + j, :, ab * AB:(ab + 1) * AB], w=[("qw", j)])
                    for di, dl in enumerate(DILS):
                        vi = nv % 2
                        nv += 1
                        nrt = 16 // dl + 1
                        for r in range(dl):
                            st0 = PV + ab * AB + r - 64 * dl
                            src = vbuf[st0:st0 + dl * (nrt * 128 - 1) + 1:dl, hh * 8:(hh + 1) * 8, :].rearrange("(jt p) h c -> p jt h c", p=128)
                            dma("sp", Vd[vi][:, r * nrt:(r + 1) * nrt, :, :], src, w=[("Vd", vi, r)])
                        for h8 in range(8):
                            off = (di * 16 + hh * 8 + h8) * 383
                            src = bass.AP(tensor=vecbuf.tensor, offset=off, ap=[[1, 128], [128, 2], [1, 128]])
                            dma("sp", BTr[:, h8, :, :], src, r=["vecbuf"], w=[("BTr", h8)])
                            op("dve", lambda e: e.tensor_copy(out=BTd[vi][:, h8, :, :], in_=BTr[:, h8, :, ::-1]), r=[("BTr", h8)], w=[("BTd", vi)])
                        items = []
                        for r in range(dl):
                            for it in range(16 // dl):
                                si = ntile % 2
                                ntile += 1
                                for jj in range(4):
                                    items.append((r, it, si, jj, nh))
                                    nh += 1

                        def stA(item):
                            r, it, si, jj, n = item
                            psd = pdb[n % 2]
                            for hq in range(2):
                                pl = hq * 64
                                kw3 = kw[pl:pl + 64, jj, :].rearrange("p (m d) -> p m d", d=dl)
                                qw3 = qw[pl:pl + 64, jj, :].rearrange("p (m d) -> p m d", d=dl)
                                k0 = KP // dl - 64 + 128 * it
                                for c in range(2):
                                    cc0 = hq * 512 + c * 128
                                    op("pe", lambda e: e.matmul(psd[:, cc0:cc0 + 128], lhsT=kw3[:, k0 + 128 * c:k0 + 128 * (c + 1), r],
                                                                rhs=qw3[:, 128 * it:128 * (it + 1), r], start=True, stop=True),
                                       r=[("kw", jj), ("qw", jj)], w=[("pst", 2 * (n % 2) + hq)])

                        def stB(item):
                            r, it, si, jj, n = item
                            psd = pdb[n % 2]
                            k2 = n % 4
                            op("dve", lambda e: e.tensor_tensor(out=sTs[k2][:, :].rearrange("p (h x) -> p h x", h=2),
                                                                in0=psd[:, :].rearrange("p (h x) -> p h x", h=2)[:, :, 0:256],
                                                                in1=BTd[vi][:, 2 * jj:2 * jj + 2, :, :].rearrange("p h c q -> p h (c q)"), op=ALU.add),
                               r=[("pst", 2 * (n % 2)), ("pst", 2 * (n % 2) + 1), ("BTd", vi)], w=[("sTs", k2)])
                            op("act", lambda e: e.activation(out=pTs[k2][:], in_=sTs[k2][:], func=AF.Exp), r=[("sTs", k2)], w=[("pTs", k2)])

                        def stC(item):
                            r, it, si, jj, n = item
                            k2 = n % 4
                            pj_ = 4 + (n % 2)
                            ps2, pk2 = pst[pj_], ("pst", pj_)
                            for hq in range(2):
                                h8 = 2 * jj + hq
                                for c in range(2):
                                    cc0 = hq * 256 + c * 128
                                    op("pe", lambda e: e.matmul(ps2[:, hq * 128:hq * 128 + 65], lhsT=pTs[k2][:, cc0:cc0 + 128],
                                                                rhs=Vd[vi][:, r * nrt + it + c, h8, :], start=(c == 0), stop=(c == 1)),
                                       r=[("pTs", k2), ("Vd", vi, r)], w=[pk2])
                            if n % 2 == 0:
                                op("act", lambda e: e.copy(out=stg[si][:, 2 * jj:2 * jj + 2, :], in_=ps2[:, 0:256].rearrange("p (h c) -> p h c", h=2)[:, :, 0:65]),
                                   r=[pk2], w=[("stg", si, jj)])
                            else:
                                op("dve", lambda e: e.tensor_copy(out=stg[si][:, 2 * jj:2 * jj + 2, :], in_=ps2[:, 0:256].rearrange("p (h c) -> p h c", h=2)[:, :, 0:65]),
                                   r=[pk2], w=[("stg", si, jj)])
                            if jj == 3:
                                q0 = ab * AB + r + dl * 128 * it
                                dma("pool", accbuf[di, q0:q0 + dl * 127 + 1:dl, hh * 8:(hh + 1) * 8, :], stg[si][:], r=[("stg", si, jj_) for jj_ in range(4)])

                        NI = len(items)
                        stA(items[0])
                        if NI > 1:
                            stA(items[1])
                        stB(items[0])
                        for n_, item in enumerate(items):
                            if n_ + 2 < NI:
                                stA(items[n_ + 2])
                            if n_ + 1 < NI:
                                stB(items[n_ + 1])
                            stC(item)
            kb.barrier()
        if stage == 41:
            P.close()
            return
        with ExitStack() as L:
            wo = kb.sb(L, "wo1", [128, 8, D], BF16)
            dma("pool", wo[:], w_o.rearrange("(c p) n -> p c n", p=128), w=["wo1"])
            acs = [[kb.sb(L, f"ac{i}_{k}", [128, 16, 65], F32) for k in range(3)] for i in range(2)]
            rl = [kb.sb(L, f"rl{i}", [128, 16], F32) for i in range(2)]
            atb = [kb.sb(L, f"atb{i}", [128, 16, 64], BF16) for i in range(2)]
            atT = [kb.sb(L, f"atT{i}", [128, 8, 128], BF16) for i in range(2)]
            xts = [kb.sb(L, f"a3x{i}", [128, D], F32) for i in range(2)]
            for t in range(S // 128):
                i = t % 2
                tg = f"a3{i}"
                for k in range(3):
                    dma("sp", acs[i][k][:], accbuf[k, t * 128:(t + 1) * 128, :, :], w=[(tg, "ac", k)])
                dma("sp", xts[i][:], xbuf[t * 128:(t + 1) * 128, :], w=[tg + "x"])
                if stage in (143, 144, 145):
                    kk_ = stage - 143
                    op("dve", lambda e: e.tensor_copy(out=xts[i][:], in_=acs[i][kk_][:, :, :].rearrange("p h c -> p (h c)")[:, 0:1024]),
                       r=[(tg, "ac", kk_), tg + "x"], w=[tg + "x"])
                    dma("pool", xbuf[t * 128:(t + 1) * 128, :], xts[i][:], r=[tg + "x"])
                    continue
                op("dve", lambda e: e.tensor_tensor(out=acs[i][0][:], in0=acs[i][0][:], in1=acs[i][1][:], op=ALU.add),
                   r=[(tg, "ac", 0), (tg, "ac", 1)], w=[(tg, "ac", 0)])
                op("dve", lambda e: e.tensor_tensor(out=acs[i][0][:], in0=acs[i][0][:], in1=acs[i][2][:], op=ALU.add),
                   r=[(tg, "ac", 0), (tg, "ac", 2)], w=[(tg, "ac", 0)])
                op("dve", lambda e: e.reciprocal(out=rl[i][:], in_=acs[i][0][:, :, 64]), r=[(tg, "ac", 0)], w=[tg + "rl"])
                op("dve", lambda e: e.tensor_tensor(out=atb[i][:], in0=acs[i][0][:, :, 0:64],
                                                    in1=rl[i][:, :].unsqueeze(2).to_broadcast([128, 16, 64]), op=ALU.mult),
                   r=[(tg, "ac", 0), tg + "rl"], w=[tg + "atb"])
                atf = atb[i][:, :, :].rearrange("p h c -> p (h c)")
                if stage == 141:
                    op("dve", lambda e: e.tensor_copy(out=xts[i][:], in_=atf), r=[tg + "atb", tg + "x"], w=[tg + "x"])
                    dma("pool", xbuf[t * 128:(t + 1) * 128, :], xts[i][:], r=[tg + "x"])
                    continue
                pb, pk = next_pb()
                for c in range(8):
                    op("pe", lambda e: e.transpose(out=pb[:, c * 128:(c + 1) * 128], in_=atf[:, c * 128:(c + 1) * 128], identity=identb[:]),
                       r=[tg + "atb", "identb"], w=[pk])
                op("act", lambda e: e.copy(out=atT[i][:], in_=pb[:, :].rearrange("p (c t) -> p c t", c=8)), r=[pk], w=[tg + "atT"])
                for hf2 in range(2):
                    ps, pk2 = next_ps()
                    for c in range(8):
                        op("pe", lambda e: e.matmul(ps[:, :], lhsT=atT[i][:, c, :], rhs=wo[:, c, hf2 * 512:(hf2 + 1) * 512],
                                                    start=(c == 0), stop=(c == 7)), r=[tg + "atT", "wo1"], w=[pk2])
                    op("dve", lambda e: e.tensor_tensor(out=xts[i][:, hf2 * 512:(hf2 + 1) * 512], in0=ps[:, :],
                                                        in1=xts[i][:, hf2 * 512:(hf2 + 1) * 512], op=ALU.add), r=[pk2, tg + "x"], w=[tg + "x"])
                dma("pool", xbuf[t * 128:(t + 1) * 128, :], xts[i][:], r=[tg + "x"])
            kb.barrier()
        P.close()
        kb.barrier()

    do_final_norm = stage == 99
    if stage >= 100 or stage in (40, 41):
        with ExitStack() as L:
            xt = [kb.sb(L, f"cx{i}", [128, D], F32) for i in range(2)]
            for t in range(S // 128):
                i = t % 2
                dma("sp", xt[i][:], x_in[t * 128:(t + 1) * 128, :], w=[("cx", i)])
                dma("sp", xbuf[t * 128:(t + 1) * 128, :], xt[i][:], r=[("cx", i)])
            kb.barrier()
        if stage in (104, 141, 142, 143, 144, 145, 40, 41):
            phase_attn()
        if stage == 105:
            phase_moe(1)
    else:
        phase_m0()
        if stage >= 3:
            phase_moe(0)
        if stage >= 4:
            phase_attn()
        if stage >= 5:
            phase_moe(1)
    phase_final()

    G.close()
    kb.es.close()
    return nc


def _t5_bucket(rel):
    half_buckets, max_exact = 16, 8
    n = np.abs(rel)
    scaled = (np.log(np.maximum(n, 1).astype(np.float32) / np.float32(max_exact)) / np.float32(np.log(1024 / max_exact))).astype(np.float32)
    large = np.minimum(max_exact + (scaled * np.float32(half_buckets - max_exact)).astype(np.int32), half_buckets - 1)
    return np.where(rel > 0, half_buckets, 0) + np.where(n < max_exact, n, large)


def _bias_patterns():
    g = np.zeros((33, 3, 383), dtype=np.float32)
    u = np.arange(383)
    rel = u - 191
    valid = np.abs(rel) <= 64
    for di, dl in enumerate((1, 4, 16)):
        b = _t5_bucket(rel * dl)
        for uu in range(383):
            if valid[uu]:
                g[b[uu], di, uu] = 1.0
            else:
                g[32, di, uu] = -1e30
    return g


def host_inputs(inp, b):
    f = np.float32
    d = {}
    d["x"] = np.ascontiguousarray(inp["x"][b], dtype=f)
    g = np.stack([inp["mix_norm"][0], inp["ffn_norm"][0], inp["mix_norm"][1], inp["ffn_norm"][1], inp["final_norm"]])
    d["gains"] = np.ascontiguousarray(np.broadcast_to(g[:, None, :], (5, 128, D)), dtype=f)
    d["w_in"] = np.ascontiguousarray(inp["w_in_even"][0], dtype=f)
    d["w_out"] = np.ascontiguousarray(inp["w_out_even"][0], dtype=f)
    d["a_ln"] = np.ascontiguousarray(np.broadcast_to(np.stack([inp["a_ln_g"][0], inp["a_ln_b"][0]])[:, None, :], (2, 128, 512)), dtype=f)
    d["a_wsT"] = np.ascontiguousarray(np.transpose(inp["a_w_s"][0], (2, 0, 1)), dtype=f)
    d["a_bs"] = np.ascontiguousarray(np.repeat(np.transpose(inp["a_b_s"][0], (1, 0))[:, :, None], 128, axis=2).reshape(128, 512), dtype=f)
    d["lbt"] = np.ascontiguousarray(np.transpose(inp["b_lb_table"].reshape(2, 3, 4, 128), (3, 2, 0, 1)), dtype=f)
    d["b_ng"] = np.ascontiguousarray(np.broadcast_to(inp["b_norm_g"][0][None, :], (64, 512)), dtype=f)
    d["ident_f"] = np.eye(128, dtype=f)
    rm = np.ones((128, T), dtype=f)
    rm[:, ::64] = 0.0
    d["rmask"] = rm
    s_ = np.arange(64)[:, None]
    t_ = np.arange(64)[None, :]
    d["tri"] = np.ascontiguousarray(np.stack([(s_ <= t_), (s_ >= t_)], axis=1), dtype=f)
    d["ones_f"] = np.ones((128, 128), dtype=f)
    pp = np.arange(128)
    d["lstrict"] = np.ascontiguousarray((pp[:, None] < pp[None, :]), dtype=f)
    d["iota_s"] = np.ascontiguousarray(np.broadcast_to(np.arange(CAP, dtype=f)[None, :], (128, CAP)))
    r2 = np.ones((128, NE, 64), dtype=f)
    r2[:, :, 0] = 0.0
    d["rmask2"] = r2.reshape(128, NE * 64)
    td = np.zeros((128, 64, 2), dtype=f)
    td[:, :, 0] = np.arange(64)[None, :]
    td[:, :, 1] = np.arange(128)[:, None]
    d["tokdig"] = td
    d["rel_bias"] = np.ascontiguousarray(inp["rel_bias"], dtype=f)
    d["gpat"] = _bias_patterns()
    d["w_qkv"] = np.ascontiguousarray(inp["w_qkv_odd"][0], dtype=f)
    d["w_o"] = np.ascontiguousarray(inp["w_o_odd"][0], dtype=f)
    d["w_router"] = np.ascontiguousarray(inp["w_router"], dtype=f)
    d["w_gate"] = np.ascontiguousarray(inp["w_gate"], dtype=f)
    d["w_up"] = np.ascontiguousarray(inp["w_up"], dtype=f)
    d["w_down"] = np.ascontiguousarray(inp["w_down"], dtype=f)
    return d


_CACHE = {}


def kernel(**inputs):
    inp = {k: np.asarray(v) for k, v in inputs.items()}
    if "nc" not in _CACHE:
        _CACHE["nc"] = build_program()
    nc = _CACHE["nc"]
    per_b = [host_inputs(inp, b) for b in range(2)]
    if globals().get("_DBG_STAGE", 0) in (104, 141, 142, 143, 144, 145, 40, 41):
        for pb_ in per_b:
            for k_ in ("w_gate", "w_up", "w_down"):
                pb_.pop(k_)
    in_maps = [per_b[c % 2] for c in range(8)]
    res = run_bass_kernel_spmd(nc, in_maps, core_ids=list(range(8)))
    out = np.stack([res.results[0]["out"], res.results[1]["out"]], axis=0).astype(np.float32)
    return out
```

```python
import numpy as np
from contextlib import ExitStack
import concourse.bass as bass
import concourse.mybir as mybir
from concourse.bass_utils import run_bass_kernel_spmd

F32 = mybir.dt.float32
BF16 = mybir.dt.bfloat16
I32 = mybir.dt.int32
AF = mybir.ActivationFunctionType
ALU = mybir.AluOpType
AX = mybir.AxisListType

S = 8192
D = 1024
T = 1024
NB = S // T
NT = T // 128
NCH = T // 64
EPS = 1e-6
NE = 16
CAP = 1024


class KB:
    def __init__(self):
        self.nc = bass.Bass("TRN2", target_bir_lowering=False)
        nc = self.nc
        self.es = ExitStack()
        self.E = dict(pe=nc.tensor, act=nc.scalar, dve=nc.vector, pool=nc.gpsimd, sp=nc.sync)
        self.sem = {k: self.es.enter_context(nc.semaphore("sem_" + k)) for k in self.E}
        self.cnt = dict.fromkeys(self.E, 0)
        self.waited = {}
        self.lastw = {}
        self.readers = {}
        self.dq = {}
        for q in ("sp", "pool", "act"):
            sems = [self.es.enter_context(nc.semaphore(f"dq_{q}_{i}")) for i in range(12)]
            self.dq[q] = dict(sems=sems, vals=[0] * 12, i=0)
        self.nps = 0

    def sb(self, st, name, shape, dt):
        self.nps += 1
        return st.enter_context(self.nc.sbuf_tensor(f"sb{self.nps}_{name}", list(shape), dt))

    def ps(self, st, name, shape, dt=F32):
        self.nps += 1
        return st.enter_context(self.nc.psum_tensor(f"ps{self.nps}_{name}", list(shape), dt))

    def _wait(self, eng, h):
        sem, name, val, heng = h
        k = (eng, name)
        if self.waited.get(k, 0) >= val:
            return
        self.E[eng].wait_ge(sem, val)
        self.waited[k] = val

    def _deps(self, eng, r, w, is_dma=False):
        out = []
        for k in r:
            h = self.lastw.get(k)
            if h is not None:
                if is_dma or h[3] != eng or eng != "pe":
                    out.append(h)
        for k in w:
            h = self.lastw.get(k)
            if h is not None and (is_dma or h[3] != eng):
                out.append(h)
            for h in self.readers.get(k, ()):
                if is_dma or h[3] != eng:
                    out.append(h)
        return out

    def _record(self, h, r, w):
        for k in r:
            lst = self.readers.setdefault(k, [])
            if h[3] is not None:
                lst[:] = [x for x in lst if x[3] != h[3]]
            lst.append(h)
            if len(lst) > 24:
                del lst[0]
        for k in w:
            self.lastw[k] = h
            self.readers[k] = []

    def op(self, eng, fn, r=(), w=()):
        for h in self._deps(eng, r, w):
            self._wait(eng, h)
        ins = fn(self.E[eng])
        self.cnt[eng] += 1
        ins.then_inc(self.sem[eng], 1)
        h = (self.sem[eng], "sem_" + eng, self.cnt[eng], eng)
        self._record(h, r, w)
        return h

    def dma(self, q, out, in_, r=(), w=(), **kw):
        for h in self._deps(q, r, w, is_dma=True):
            self._wait(q, h)
        d = self.dq[q]
        i = d["i"]
        d["i"] = (i + 1) % len(d["sems"])
        sem = d["sems"][i]
        name = f"dq_{q}_{i}"
        if d["vals"][i] > 0:
            self._wait(q, (sem, name, d["vals"][i], None))
        ins = self.E[q].dma_start(out=out, in_=in_, **kw)
        ins.then_inc(sem, 16)
        d["vals"][i] += 16
        h = (sem, name, d["vals"][i], None)
        self._record(h, r, w)
        return h

    def idma(self, out, in_, out_offset=None, in_offset=None, r=(), w=(), **kw):
        q = "pool"
        for h in self._deps(q, r, w, is_dma=True):
            self._wait(q, h)
        d = self.dq[q]
        i = d["i"]
        d["i"] = (i + 1) % len(d["sems"])
        sem = d["sems"][i]
        name = f"dq_{q}_{i}"
        if d["vals"][i] > 0:
            self._wait(q, (sem, name, d["vals"][i], None))
        ins = self.nc.gpsimd.indirect_dma_start(out=out, out_offset=out_offset, in_=in_, in_offset=in_offset, **kw)
        ins.then_inc(sem, 16)
        d["vals"][i] += 16
        h = (sem, name, d["vals"][i], None)
        self._record(h, r, w)
        return h

    def barrier(self):
        hs = [(self.sem[e], "sem_" + e, self.cnt[e], e) for e in self.E if self.cnt[e] > 0]
        for q, d in self.dq.items():
            for i, s in enumerate(d["sems"]):
                if d["vals"][i] > 0:
                    hs.append((s, f"dq_{q}_{i}", d["vals"][i], None))
        for eng in self.E:
            for h in hs:
                if h[3] != eng:
                    self._wait(eng, h)
        self.lastw.clear()
        self.readers.clear()


def build_program(stage=99):
    kb = KB()
    nc = kb.nc
    op, dma = kb.op, kb.dma

    def dram_in(name, shape, dt=F32):
        return nc.dram_tensor(name, list(shape), dt, kind="ExternalInput").ap()

    x_in = dram_in("x", [S, D])
    gains = dram_in("gains", [5, 128, D])
    w_in = dram_in("w_in", [D, 3584])
    w_out = dram_in("w_out", [D, D])
    a_ln = dram_in("a_ln", [2, 128, 512])
    a_wsT = dram_in("a_wsT", [128, 4, 128])
    a_bs = dram_in("a_bs", [128, 512])
    lbt = dram_in("lbt", [128, 4, 2, 3])
    b_ng = dram_in("b_ng", [64, 512])
    ident_f = dram_in("ident_f", [128, 128])
    rmask_in = dram_in("rmask", [128, T])
    tri_in = dram_in("tri", [64, 2, 64])
    ones_in = dram_in("ones_f", [128, 128])
    lstrict_in = dram_in("lstrict", [128, 128])
    iota_in = dram_in("iota_s", [128, CAP])
    rmask2_in = dram_in("rmask2", [128, NE * 64])
    tokdig_in = dram_in("tokdig", [128, 64, 2])
    w_router = dram_in("w_router", [2, D, NE])
    if stage in (104, 141, 142, 143, 144, 145, 40, 41):
        w_gate = w_up = w_down = None
    else:
        w_gate = dram_in("w_gate", [2, NE, D, D])
        w_up = dram_in("w_up", [2, NE, D, D])
        w_down = dram_in("w_down", [2, NE, D, D])
    rel_bias = dram_in("rel_bias", [32, 16])
    gpat_in = dram_in("gpat", [33, 3, 383])
    w_qkv = dram_in("w_qkv", [D, 3072])
    w_o = dram_in("w_o", [D, D])
    out_d = nc.dram_tensor("out", [S, D], F32, kind="ExternalOutput").ap()
    vecbuf = nc.dram_tensor("vecbuf", [3, 16, 383], F32, kind="Internal").ap()
    dk = "ExternalOutput" if stage in (143, 144, 145) else "Internal"
    if stage == 143:
        dbg_s = nc.dram_tensor("dbg_s", [128, 256], F32, kind="ExternalOutput").ap()
        dbg_p = nc.dram_tensor("dbg_p", [128, 256], BF16, kind="ExternalOutput").ap()
        dbg_v = nc.dram_tensor("dbg_v", [128, 2, 65], BF16, kind="ExternalOutput").ap()
        dbg_k = nc.dram_tensor("dbg_k", [64, 2, 128], BF16, kind="ExternalOutput").ap()
        dbg_q = nc.dram_tensor("dbg_q", [64, 128], BF16, kind="ExternalOutput").ap()
        dbg_o = nc.dram_tensor("dbg_o", [128, 8, 65], F32, kind="ExternalOutput").ap()
        dbg_a = nc.dram_tensor("dbg_a", [128, 8, 65], F32, kind="ExternalOutput").ap()
    qTbuf = nc.dram_tensor("qTbuf", [8, 128, S], BF16, kind=dk).ap()
    kTbuf = nc.dram_tensor("kTbuf", [8, 128, S + 2048], BF16, kind=dk).ap()
    vbuf = nc.dram_tensor("vbuf", [S + 2 * 1088, 16, 65], BF16, kind=dk).ap()
    accbuf = nc.dram_tensor("accbuf", [3, S, 16, 65], F32, kind="Internal").ap()
    h2buf = nc.dram_tensor("h2buf", [S, D], BF16, kind="Internal").ap()
    xbuf = nc.dram_tensor("xbuf", [S, D], F32, kind="Internal").ap()
    hTbuf = nc.dram_tensor("hTbuf", [NB, 128, 8, T], BF16, kind="Internal").ap()

    G = ExitStack()
    identb = kb.sb(G, "identb", [128, 128], BF16)
    identf = kb.sb(G, "identf", [128, 128], F32)
    epsc = kb.sb(G, "epsc", [128, 1], F32)
    gain_sb = kb.sb(G, "gain_sb", [128, D], F32)
    pdb = [kb.ps(G, f"pdb{i}", [128, 1024], F32) for i in range(3)]
    pst = [pdb[i // 2][:, (i % 2) * 512:(i % 2 + 1) * 512] for i in range(6)]
    psb = [kb.ps(G, f"psb{i}", [128, 1024], BF16) for i in range(2)]
    psi = [0]
    pbi = [0]

    psr = [0, 6]

    def next_ps():
        i = psi[0]
        if not (psr[0] <= i < psr[1]):
            i = psr[0]
        psi[0] = i + 1 if i + 1 < psr[1] else psr[0]
        return pst[i], ("pst", i)

    hsi = [0]

    def next_hs():
        i = hsi[0]
        hsi[0] = (i + 1) % 4
        return pst[i], ("pst", i)

    pqi = [0]

    def next_pbq():
        i = pqi[0]
        pqi[0] = (i + 1) % 4
        return psb[0][:, i * 256:(i + 1) * 256], ("pbq", i)

    pbr = [0, 2]

    def next_pb():
        i = pbi[0]
        if not (pbr[0] <= i < pbr[1]):
            i = pbr[0]
        pbi[0] = i + 1 if i + 1 < pbr[1] else pbr[0]
        return psb[i], ("psb", i)

    dma("sp", identf[:], ident_f[:, :], w=["identf"])
    op("dve", lambda e: e.tensor_copy(out=identb[:], in_=identf[:]), r=["identf"], w=["identb"])
    op("dve", lambda e: e.memset(epsc[:], EPS), w=["epsc"])

    def load_gain(i):
        dma("sp", gain_sb[:], gains[i, :, :], w=["gain"])

    def rms_tile(st_tag, xt, hb, junk, ss, rs, keys_x):
        op("act", lambda e: e.activation(out=junk[:], in_=xt, func=AF.Square, accum_out=ss[:]),
           r=keys_x, w=[st_tag + "junk", st_tag + "ss"])
        op("act", lambda e: e.activation(out=rs[:], in_=ss[:], func=AF.Sqrt, bias=epsc[:], scale=1.0 / D),
           r=[st_tag + "ss", "epsc"], w=[st_tag + "rs"])
        op("dve", lambda e: e.reciprocal(out=rs[:], in_=rs[:]), r=[st_tag + "rs"], w=[st_tag + "rs"])
        op("dve", lambda e: e.scalar_tensor_tensor(out=hb[:], in0=xt, scalar=rs[:, 0:1], in1=gain_sb[:],
                                                   op0=ALU.mult, op1=ALU.mult),
           r=keys_x + [st_tag + "rs", "gain"], w=[st_tag + "hb"])

    def phase_m0():
        P = ExitStack()
        lb_sb = kb.sb(P, "lb_sb", [128, 4, 2, 3], F32)
        lb_e = kb.sb(P, "lb_e", [128, 4, 2, 3], F32)
        lb_s = kb.sb(P, "lb_s", [128, 4, 2], F32)
        lbv = kb.sb(P, "lbv", [128, 4, 2], F32)
        oml = kb.sb(P, "oml", [128, 4, 2], F32)
        rmask = kb.sb(P, "rmask", [128, T], F32)
        tri = kb.sb(P, "tri", [64, 2, 64], F32)
        wsT = kb.sb(P, "wsT", [128, 4, 128], BF16)
        lng = kb.sb(P, "lng", [128, 512], F32)
        lnb = kb.sb(P, "lnb", [128, 512], F32)
        bsf = kb.sb(P, "bsf", [128, 512], F32)
        bng = kb.sb(P, "bng", [64, 512], F32)
        Sin_b = kb.sb(P, "Sin_b", [128, NB, 4, 128], F32)
        Bb = kb.sb(P, "Bb", [128, NB, 4, 128], F32)
        Ab = kb.sb(P, "Ab", [128, NB, 4], F32)
        Sf = kb.sb(P, "Sf", [128, 4, 128], BF16)
        hT = kb.sb(P, "hT", [128, 8, T], BF16)
        load_gain(0)
        dma("sp", lb_sb[:], lbt[:, :, :, :], w=["lb_sb"])
        dma("sp", rmask[:], rmask_in[:, :], w=["rmask"])
        dma("sp", tri[:], tri_in[:, :, :], w=["tri"])
        dma("pool", wsT[:], a_wsT[:, :, :], w=["wsT"])
        dma("sp", lng[:], a_ln[0, :, :], w=["lng"])
        dma("sp", lnb[:], a_ln[1, :, :], w=["lnb"])
        dma("sp", bsf[:], a_bs[:, :], w=["bsf"])
        dma("sp", bng[:], b_ng[:, :], w=["bng"])
        op("act", lambda e: e.activation(out=lb_e[:], in_=lb_sb[:], func=AF.Exp), r=["lb_sb"], w=["lb_e"])
        op("dve", lambda e: e.tensor_reduce(out=lb_s[:], in_=lb_e[:], axis=AX.X, op=ALU.add), r=["lb_e"], w=["lb_s"])
        op("dve", lambda e: e.reciprocal(out=lb_s[:], in_=lb_s[:]), r=["lb_s"], w=["lb_s"])
        op("dve", lambda e: e.tensor_tensor(out=lbv[:], in0=lb_e[:, :, :, 0], in1=lb_s[:], op=ALU.mult),
           r=["lb_e", "lb_s"], w=["lbv"])
        op("dve", lambda e: e.tensor_scalar(out=oml[:], in0=lbv[:], scalar1=-1.0, scalar2=1.0, op0=ALU.mult, op1=ALU.add),
           r=["lbv"], w=["oml"])

        def make_hT(blk, src):
            with ExitStack() as L:
                xts = [kb.sb(L, f"m0x{i}", [128, D], F32) for i in range(2)]
                hbs = [kb.sb(L, f"m0h{i}", [128, D], BF16) for i in range(2)]
                junk = kb.sb(L, "m0junk", [128, D], BF16)
                sss = [kb.sb(L, f"m0ss{i}", [128, 1], F32) for i in range(2)]
                rss = [kb.sb(L, f"m0rs{i}", [128, 1], F32) for i in range(2)]
                for t in range(NT):
                    i = t % 2
                    tag = f"mh{i}"
                    r0 = blk * T + t * 128
                    dma("sp", xts[i][:], src[r0:r0 + 128, :], w=[tag + "x"])
                    rms_tile(tag, xts[i][:], hbs[i], junk, sss[i], rss[i], [tag + "x"])
                    pb, pk = next_pb()
                    for c in range(8):
                        op("pe", lambda e, c=c: e.transpose(out=pb[:, c * 128:(c + 1) * 128],
                                                            in_=hbs[i][:, c * 128:(c + 1) * 128], identity=identb[:]),
                           r=[tag + "hb", "identb"], w=[pk])
                    op("act", lambda e: e.copy(out=hT[:, :, t * 128:(t + 1) * 128],
                                               in_=pb[:, :].rearrange("p (c t) -> p c t", c=8)),
                       r=[pk], w=[("hT", t)])
                kb.barrier()

        def load_w(wt, col0, ncol, key):
            dma("pool", wt, w_in.rearrange("(c p) n -> p c n", p=128)[:, :, col0:col0 + ncol], w=[key])

        def proj_fm(wt, wkey, j, dst_fn):
            for tb in range(T // 512):
                ps, pk = next_ps()
                for c in range(8):
                    op("pe", lambda e, c=c: e.matmul(ps[:, :], lhsT=wt[:, c, j, :], rhs=hT[:, c, tb * 512:(tb + 1) * 512],
                                                      start=(c == 0), stop=(c == 7)),
                       r=[wkey] + [("hT", t) for t in range(tb * 4, tb * 4 + 4)], w=[pk])
                dst_fn(tb, ps, pk)

        def hgrn_prep(L, wt, wkey, h, d, qf, need_q, bufs, tag=None, Qt=None, Kt=None, eend=None, ltot=None, qkey="qf"):
            fb, lb_, eb = bufs["fb"], bufs["lb"], bufs["eb"]

            def evac_sig(tb, ps, pk):
                op("act", lambda e: e.activation(out=fb[:, tb * 512:(tb + 1) * 512], in_=ps[:, :], func=AF.Sigmoid),
                   r=[pk], w=[("fb", tb)])
            proj_fm(wt, wkey, 1 + d, evac_sig)
            fbk = [("fb", tb) for tb in range(4)]
            op("dve", lambda e: e.tensor_scalar(out=fb[:], in0=fb[:], scalar1=oml[:, h, d:d + 1], scalar2=lbv[:, h, d:d + 1],
                                                op0=ALU.mult, op1=ALU.add), r=fbk + ["oml", "lbv"], w=fbk)
            op("act", lambda e: e.activation(out=lb_[:], in_=fb[:], func=AF.Ln), r=fbk, w=["lb"])
            op("dve", lambda e: e.tensor_scalar(out=fb[:], in0=fb[:], scalar1=-1.0, scalar2=1.0, op0=ALU.mult, op1=ALU.add),
               r=fbk, w=fbk)
            op("dve", lambda e: e.tensor_tensor_scan(out=eb[:], data0=rmask[:], data1=lb_[:], initial=0.0,
                                                     op0=ALU.mult, op1=ALU.add), r=["rmask", "lb"], w=["eb"])
            eb3 = eb[:, :].rearrange("p (c j) -> p c j", j=64)
            if d == 1:
                op("dve", lambda e: e.tensor_tensor(out=lb_[:], in0=lb_[:], in1=eb[:], op=ALU.subtract), r=["lb", "eb"], w=["lb"])
                op("dve", lambda e: e.tensor_copy(out=eend[:], in_=eb3[:, :, 63]), r=["eb"], w=[tag + "eend"])
                op("dve", lambda e: e.tensor_tensor(out=eb3, in0=lb_[:, :].rearrange("p (c j) -> p c j", j=64),
                                                    in1=eend[:, :].unsqueeze(2).to_broadcast([128, NCH, 64]), op=ALU.add),
                   r=["lb", tag + "eend"], w=["eb"])
            col = 63 if d == 0 else 0
            if ltot is not None:
                op("dve", lambda e: e.tensor_reduce(out=ltot, in_=eb3[:, :, col], axis=AX.X, op=ALU.add),
                   r=["eb"], w=[tag + "ltot"])
            op("act", lambda e: e.activation(out=lb_[:], in_=eb[:], func=AF.Exp, scale=-1.0), r=["eb"], w=["lb"])
            op("dve", lambda e: e.tensor_tensor(out=Kt[:], in0=fb[:], in1=lb_[:], op=ALU.mult), r=fbk + ["lb"], w=[tag + "Kt"])
            op("act", lambda e: e.activation(out=lb_[:], in_=eb[:], func=AF.Exp), r=["eb"], w=["lb"])
            lb3 = lb_[:, :].rearrange("p (c j) -> p c j", j=64)
            op("dve", lambda e: e.tensor_copy(out=eend[:], in_=lb3[:, :, col]), r=["lb"], w=[tag + "eend"])
            if need_q:
                op("dve", lambda e: e.tensor_tensor(out=Qt[:], in0=qf[:], in1=lb_[:], op=ALU.mult), r=[qkey, "lb"], w=[tag + "Qt"])

        def proj_tok_wide(w_ap, wkey, ncol, evac):
            for ch in range(NCH):
                ps, pk = next_ps()
                for c in range(8):
                    op("pe", lambda e: e.matmul(ps[0:64, 0:ncol], lhsT=hT[:, c, ch * 64:(ch + 1) * 64], rhs=w_ap[:, c, :],
                                                start=(c == 0), stop=(c == 7)), r=[wkey, ("hT", ch // 2)], w=[pk])
                evac(ch, ps, pk)

        def proj_tok(wt, wkey, j, dst, dkey, func):
            for g4 in range(NCH // 4):
                ps, pk = next_ps()
                for cc in range(4):
                    ch = g4 * 4 + cc
                    for c in range(8):
                        op("pe", lambda e, c=c, cc=cc, ch=ch: e.matmul(ps[0:64, cc * 128:(cc + 1) * 128],
                                                                       lhsT=hT[:, c, ch * 64:(ch + 1) * 64], rhs=wt[:, c, j, :],
                                                                       start=(c == 0), stop=(c == 7)),
                           r=[wkey, ("hT", ch // 2)], w=[pk])
                op("act", lambda e: e.activation(out=dst[:, g4 * 4:(g4 + 1) * 4, :],
                                                 in_=ps[0:64, :].rearrange("p (c v) -> p c v", c=4), func=func),
                   r=[pk], w=[(dkey, g4)])

        def scan_chunk(c, ch):
            d, tag, Qt, Kt, eend, vt, St = c["d"], c["tag"], c["Qt"], c["Kt"], c["eend"], c["vt"], c["St"]
            vkey = c["vkey"]
            qi = c["n"] % 2
            c["n"] += 1
            cs = slice(ch * 64, (ch + 1) * 64)
            pbq, pbk = next_pbq()
            op("pe", lambda e: e.transpose(out=pbq[0:64, 0:128], in_=Kt[:, cs], identity=identb[:]), r=[tag + "Kt", "identb"], w=[pbk])
            ktk = c["ktk"][qi]
            op("dve", lambda e: e.tensor_copy(out=ktk[:], in_=pbq[0:64, 0:128]), r=[pbk], w=[(tag, "ktk", qi)])
            if c["emit_o"]:
                ps, pk = next_hs()
                op("pe", lambda e: e.matmul(ps[0:64, 0:64], lhsT=Kt[:, cs], rhs=Qt[:, cs], start=True, stop=True),
                   r=[tag + "Kt", tag + "Qt"], w=[pk])
                sT = c["sc"][qi]
                op("dve", lambda e: e.tensor_tensor(out=sT[:], in0=ps[0:64, 0:64], in1=tri[:, d, :], op=ALU.mult),
                   r=[pk, "tri"], w=[(tag, "sc", qi)])
                ps2, pk2 = next_hs()
                op("pe", lambda e: e.matmul(ps2[0:64, 0:128], lhsT=sT[:], rhs=vt[:, ch, :], start=True, stop=False),
                   r=[(tag, "sc", qi), (vkey, ch // 4)], w=[pk2])
                op("pe", lambda e: e.matmul(ps2[0:64, 0:128], lhsT=Qt[:, cs], rhs=St[:], start=False, stop=True),
                   r=[tag + "Qt", tag + "St"], w=[pk2])
                op("act", lambda e: e.copy(out=c["o"][:, ch, :], in_=ps2[0:64, 0:128]), r=[pk2], w=[(tag, "o", ch)])
            ps3, pk3 = next_hs()
            op("pe", lambda e: e.matmul(ps3[:, 0:128], lhsT=identb[:], rhs=St[:], start=True, stop=False),
               r=["identb", tag + "St"], w=[pk3])
            op("pe", lambda e: e.matmul(ps3[:, 0:128], lhsT=ktk[:], rhs=vt[:, ch, :], start=False, stop=True),
               r=[(tag, "ktk", qi), (vkey, ch // 4)], w=[pk3])
            op("act", lambda e: e.activation(out=St[:], in_=ps3[:, 0:128], func=AF.Copy, scale=eend[:, ch:ch + 1]),
               r=[pk3, tag + "eend"], w=[tag + "St"])

        if stage >= 1:
            for blk in range(NB):
                make_hT(blk, x_in)
                dma("pool", hTbuf[blk], hT[:], r=[("hT", t) for t in range(NT)])
                with ExitStack() as L:
                    psr[:] = [4, 6]
                    pbr[:] = [1, 2]
                    bufs = dict(fb=kb.sb(L, "fb", [128, T], F32), lb=kb.sb(L, "lbuf", [128, T], F32), eb=kb.sb(L, "eb", [128, T], F32))
                    wts = [kb.sb(L, f"whA{i}", [128, 8, 5, 128], BF16) for i in range(2)]
                    wv4 = kb.sb(L, "wv4A", [128, 8, 512], BF16)
                    vt4 = kb.sb(L, "vt4A", [64, NCH, 4, 128], BF16)
                    dma("pool", wv4[:], w_in.rearrange("(c p) n -> p c n", p=128)[:, :, 2560:3072], w=["wv4A"])

                    def evac_v4(ch, ps, pk):
                        op("act", lambda e: e.copy(out=vt4[:, ch, :, :], in_=ps[0:64, 0:512].rearrange("p (h v) -> p h v", h=4)),
                           r=[pk], w=[("Avt", ch // 4)])
                    proj_tok_wide(wv4, "wv4A", 512, evac_v4)
                    chains = []
                    for h in range(4):
                        wt = wts[h % 2]
                        wkey = ("whA", h % 2)
                        for j, c0 in ((2, 2048),):
                            dma("pool", wt[:, :, j, :], w_in.rearrange("(c p) n -> p c n", p=128)[:, :, c0 + h * 128:c0 + (h + 1) * 128],
                                w=[wkey])
                        tag = f"A{h}"
                        vt = vt4[:, :, h, :]
                        Kt = kb.sb(L, f"KtA{h}", [128, T], BF16)
                        eend = kb.sb(L, f"eendA{h}", [128, NCH], F32)
                        ltot = kb.sb(L, f"ltotA{h}", [128, 1], F32)
                        St = kb.sb(L, f"StA{h}", [128, 128], BF16)
                        hgrn_prep(L, wt, wkey, h, 1, None, False, bufs, tag=tag, Qt=None, Kt=Kt, eend=eend, ltot=ltot[:])
                        op("dve", lambda e: e.memset(St[:], 0.0), w=[tag + "St"])
                        chains.append(dict(d=1, tag=tag, Qt=None, Kt=Kt, eend=eend, vt=vt, St=St, vkey="Avt", n=0, emit_o=False,
                                           ktk=[kb.sb(L, f"ktkA{h}_{i}", [64, 128], BF16) for i in range(2)], sc=None, o=None, ltot=ltot, h=h))
                    for k_ in range(NCH):
                        for c in chains:
                            scan_chunk(c, NCH - 1 - k_)
                    for c in chains:
                        h = c["h"]
                        op("act", lambda e: e.copy(out=Bb[:, blk, h, :], in_=c["St"][:]), r=[c["tag"] + "St"], w=[("Bb", blk, h)])
                        op("act", lambda e: e.activation(out=Ab[:, blk, h:h + 1], in_=c["ltot"][:], func=AF.Exp),
                           r=[c["tag"] + "ltot"], w=[("Ab", blk, h)])
                    kb.barrier()
                    psr[:] = [0, 6]
                    pbr[:] = [0, 2]
            op("dve", lambda e: e.memset(Sin_b[:, NB - 1, :, :], 0.0), w=[("Sin", NB - 1)])
            for blk in range(NB - 2, -1, -1):
                for h in range(4):
                    op("dve", lambda e, blk=blk, h=h: e.scalar_tensor_tensor(
                        out=Sin_b[:, blk, h, :], in0=Sin_b[:, blk + 1, h, :], scalar=Ab[:, blk + 1, h:h + 1],
                        in1=Bb[:, blk + 1, h, :], op0=ALU.mult, op1=ALU.add),
                        r=[("Sin", blk + 1), ("Ab", blk + 1, h), ("Bb", blk + 1, h)], w=[("Sin", blk)])
            kb.barrier()

        if stage >= 2:
            op("dve", lambda e: e.memset(Sf[:], 0.0), w=[("Sf", h_) for h_ in range(4)])
            for blk in range(NB):
                dma("sp", hT[:], hTbuf[blk], w=[("hT", t) for t in range(NT)])
                with ExitStack() as LB:
                    catT = kb.sb(LB, "catT", [128, 8, T], BF16)
                    with ExitStack() as L:
                        wuv = kb.sb(L, "wuv", [128, 8, 1024], BF16)
                        load_w(wuv[:, :, 0:512], 0, 512, "wu")
                        load_w(wuv[:, :, 512:1024], 512, 512, "wv")
                        gus = [kb.sb(L, f"gu{i}", [128, 512], F32) for i in range(2)]
                        gvs = [kb.sb(L, f"gv{i}", [128, 512], F32) for i in range(2)]
                        vnb = [kb.sb(L, f"vnb{i}", [128, 512], BF16) for i in range(2)]
                        aob = [kb.sb(L, f"aob{i}", [128, 512], BF16) for i in range(2)]
                        st6 = [kb.sb(L, f"st6{i}", [128, 6], F32) for i in range(2)]
                        mv = [kb.sb(L, f"mv{i}", [128, 2], F32) for i in range(2)]
                        for t in range(NT):
                            i = t % 2
                            ts = slice(t * 128, (t + 1) * 128)
                            psu, pku = next_ps()
                            psv, pkv = next_ps()
                            for c in range(8):
                                op("pe", lambda e, c=c: e.matmul(psu[:, :], lhsT=hT[:, c, ts], rhs=wuv[:, c, 0:512],
                                                                  start=(c == 0), stop=(c == 7)), r=[("hT", t), "wu"], w=[pku])
                            for c in range(8):
                                op("pe", lambda e, c=c: e.matmul(psv[:, :], lhsT=hT[:, c, ts], rhs=wuv[:, c, 512:1024],
                                                                  start=(c == 0), stop=(c == 7)), r=[("hT", t), "wv"], w=[pkv])
                            op("act", lambda e: e.activation(out=gus[i][:], in_=psu[:, :], func=AF.Gelu_apprx_tanh), r=[pku], w=[("gu", i)])
                            op("act", lambda e: e.activation(out=gvs[i][:], in_=psv[:, :], func=AF.Gelu_apprx_tanh), r=[pkv], w=[("gv", i)])
                            op("dve", lambda e: e.bn_stats(out=st6[i][:], in_=gvs[i][:]), r=[("gv", i)], w=[("st6", i)])
                            op("dve", lambda e: e.bn_aggr(out=mv[i][:], in_=st6[i][:]), r=[("st6", i)], w=[("mv", i)])
                            op("act", lambda e: e.activation(out=mv[i][:, 1:2], in_=mv[i][:, 1:2], func=AF.Sqrt, bias=epsc[:], scale=1.0),
                               r=[("mv", i), "epsc"], w=[("mv", i)])
                            op("dve", lambda e: e.reciprocal(out=mv[i][:, 1:2], in_=mv[i][:, 1:2]), r=[("mv", i)], w=[("mv", i)])
                            op("dve", lambda e: e.tensor_scalar(out=gvs[i][:], in0=gvs[i][:], scalar1=mv[i][:, 0:1], scalar2=mv[i][:, 1:2],
                                                                op0=ALU.subtract, op1=ALU.mult), r=[("gv", i), ("mv", i)], w=[("gv", i)])
                            op("dve", lambda e: e.tensor_tensor(out=gvs[i][:], in0=gvs[i][:], in1=lng[:], op=ALU.mult),
                               r=[("gv", i), "lng"], w=[("gv", i)])
                            op("dve", lambda e: e.tensor_tensor(out=vnb[i][:], in0=gvs[i][:], in1=lnb[:], op=ALU.add),
                               r=[("gv", i), "lnb"], w=[("vnb", i)])
                            psm, pkm = next_ps()
                            for g in range(4):
                                op("pe", lambda e, g=g: e.matmul(psm[:, g * 128:(g + 1) * 128], lhsT=wsT[:, g, :],
                                                                  rhs=vnb[i][:, g * 128:(g + 1) * 128], start=True, stop=True),
                                   r=["wsT", ("vnb", i)], w=[pkm])
                            op("dve", lambda e: e.tensor_tensor(out=gvs[i][:], in0=psm[:, :], in1=bsf[:], op=ALU.add),
                               r=[pkm, "bsf"], w=[("gv", i)])
                            op("dve", lambda e: e.tensor_tensor(out=aob[i][:], in0=gvs[i][:], in1=gus[i][:], op=ALU.mult),
                               r=[("gv", i), ("gu", i)], w=[("aob", i)])
                            pb, pbk = next_pb()
                            for g in range(4):
                                op("pe", lambda e, g=g: e.transpose(out=pb[:, g * 128:(g + 1) * 128], in_=aob[i][:, g * 128:(g + 1) * 128],
                                                                    identity=identb[:]), r=[("aob", i), "identb"], w=[pbk])
                            op("act", lambda e: e.copy(out=catT[:, 0:4, ts], in_=pb[:, 0:512].rearrange("p (c t) -> p c t", c=4)),
                               r=[pbk], w=[("catA", t)])
                        kb.barrier()
                    with ExitStack() as L:
                        psr[:] = [4, 6]
                        pbr[:] = [1, 2]
                        bufs = dict(fb=kb.sb(L, "fbB", [128, T], F32), lb=kb.sb(L, "lbufB", [128, T], F32), eb=kb.sb(L, "ebB", [128, T], F32))
                        wts = [kb.sb(L, f"whB{i}", [128, 8, 5, 128], BF16) for i in range(2)]
                        qf = kb.sb(L, "qf", [128, T], F32)
                        osq = kb.sb(L, "osq", [64, NCH, 128], F32)
                        osum = kb.sb(L, "osum", [64, NCH, 128], F32)
                        ssq = kb.sb(L, "ssq", [64, NCH], F32)
                        obf = kb.sb(L, "obf", [64, NCH, 128], BF16)
                        per = []
                        for hq in range(2):
                            per.append(dict(
                                vt=None, sg=None,
                                Qt=[kb.sb(L, f"QtB{hq}{d}", [128, T], BF16) for d in range(2)],
                                Kt=[kb.sb(L, f"KtB{hq}{d}", [128, T], BF16) for d in range(2)],
                                eend=[kb.sb(L, f"eendB{hq}{d}", [128, NCH], F32) for d in range(2)],
                                St=[kb.sb(L, f"StB{hq}{d}", [128, 128], BF16) for d in range(2)],
                                o=[kb.sb(L, f"oB{hq}{d}", [64, NCH, 128], BF16) for d in range(2)],
                                ktk=[[kb.sb(L, f"ktkB{hq}{d}{i}", [64, 128], BF16) for i in range(2)] for d in range(2)],
                                sc=[[kb.sb(L, f"scB{hq}{d}{i}", [64, 64], BF16) for i in range(2)] for d in range(2)]))
                        wvg = kb.sb(L, "wvgB", [128, 8, 512], BF16)
                        vt2 = kb.sb(L, "vt2B", [64, NCH, 2, 128], BF16)
                        sg2 = kb.sb(L, "sg2B", [64, NCH, 2, 128], BF16)
                        for hp2 in range(2):
                            chains = []
                            dma("pool", wvg[:, :, 0:256], w_in.rearrange("(c p) n -> p c n", p=128)[:, :, 2560 + hp2 * 256:2560 + (hp2 + 1) * 256], w=["wvgB"])
                            dma("pool", wvg[:, :, 256:512], w_in.rearrange("(c p) n -> p c n", p=128)[:, :, 3072 + hp2 * 256:3072 + (hp2 + 1) * 256], w=["wvgB"])

                            def evac_vg(ch, ps, pk):
                                op("act", lambda e: e.copy(out=vt2[:, ch, :, :], in_=ps[0:64, 0:256].rearrange("p (h v) -> p h v", h=2)),
                                   r=[pk], w=[("Bvt", ch // 4)])
                                op("act", lambda e: e.activation(out=sg2[:, ch, :, :], in_=ps[0:64, 256:512].rearrange("p (h v) -> p h v", h=2),
                                                                 func=AF.Sigmoid), r=[pk], w=[("Bsg", ch // 4)])
                            proj_tok_wide(wvg, "wvgB", 512, evac_vg)
                            for hq in range(2):
                                h = hp2 * 2 + hq
                                pp = per[hq]
                                pp["vt"] = vt2[:, :, hq, :]
                                pp["sg"] = sg2[:, :, hq, :]
                                wt = wts[hq]
                                wkey = ("whB", hq)
                                for j in range(3):
                                    c0 = 1024 + j * 512
                                    dma("pool", wt[:, :, j, :], w_in.rearrange("(c p) n -> p c n", p=128)[:, :, c0 + h * 128:c0 + (h + 1) * 128],
                                        w=[wkey])

                                def evac_q(tb, ps, pk):
                                    op("act", lambda e: e.activation(out=qf[:, tb * 512:(tb + 1) * 512], in_=ps[:, :], func=AF.Silu),
                                       r=[pk], w=["qf"])
                                proj_fm(wt, wkey, 0, evac_q)
                                for d in range(2):
                                    tag = f"B{hq}{d}"
                                    hgrn_prep(L, wt, wkey, h, d, qf, True, bufs, tag=tag, Qt=pp["Qt"][d], Kt=pp["Kt"][d], eend=pp["eend"][d])
                                    if d == 0:
                                        op("act", lambda e: e.copy(out=pp["St"][0][:], in_=Sf[:, h, :]), r=[("Sf", h)], w=[tag + "St"])
                                    else:
                                        op("act", lambda e: e.copy(out=pp["St"][1][:], in_=Sin_b[:, blk, h, :]), r=[("Sin", blk)], w=[tag + "St"])
                                    chains.append(dict(d=d, tag=tag, Qt=pp["Qt"][d], Kt=pp["Kt"][d], eend=pp["eend"][d], vt=pp["vt"], St=pp["St"][d],
                                                       vkey="Bvt", n=0, emit_o=True, ktk=pp["ktk"][d], sc=pp["sc"][d], o=pp["o"][d], h=h, hq=hq))
                            for k_ in range(NCH):
                                for c in chains:
                                    scan_chunk(c, k_ if c["d"] == 0 else NCH - 1 - k_)
                            for hq in range(2):
                                h = hp2 * 2 + hq
                                pp = per[hq]
                                op("act", lambda e: e.copy(out=Sf[:, h, :], in_=pp["St"][0][:]), r=[f"B{hq}0St"], w=[("Sf", h)])
                                okeys = [(f"B{hq}{d}", "o", ch) for d in range(2) for ch in range(NCH)]
                                op("dve", lambda e: e.tensor_tensor(out=osum[:], in0=pp["o"][0][:], in1=pp["o"][1][:], op=ALU.add), r=okeys, w=["osum"])
                                op("dve", lambda e: e.tensor_tensor(out=osq[:], in0=osum[:], in1=osum[:], op=ALU.mult), r=["osum"], w=["osq"])
                                op("dve", lambda e: e.tensor_reduce(out=ssq[:], in_=osq[:], axis=AX.X, op=ALU.add), r=["osq"], w=["ssq"])
                                op("act", lambda e: e.activation(out=ssq[:], in_=ssq[:], func=AF.Sqrt, bias=epsc[0:64, :], scale=1.0 / 128),
                                   r=["ssq", "epsc"], w=["ssq"])
                                op("dve", lambda e: e.reciprocal(out=ssq[:], in_=ssq[:]), r=["ssq"], w=["ssq"])
                                op("dve", lambda e: e.tensor_tensor(out=osq[:], in0=osum[:], in1=ssq[:, :].unsqueeze(2).to_broadcast([64, NCH, 128]), op=ALU.mult),
                                   r=["osum", "ssq"], w=["osq"])
                                op("dve", lambda e: e.tensor_tensor(out=osq[:], in0=osq[:],
                                                                    in1=bng[:, h * 128:(h + 1) * 128].unsqueeze(1).to_broadcast([64, NCH, 128]),
                                                                    op=ALU.mult), r=["osq", "bng"], w=["osq"])
                                op("dve", lambda e: e.tensor_tensor(out=obf[:], in0=osq[:], in1=pp["sg"], op=ALU.mult),
                                   r=["osq"] + [("Bsg", g4) for g4 in range(NCH // 4)], w=["obf"])
                                for g8 in range(NCH // 8):
                                    pb, pbk = next_pb()
                                    for cc in range(8):
                                        ch = g8 * 8 + cc
                                        op("pe", lambda e: e.transpose(out=pb[:, cc * 64:(cc + 1) * 64], in_=obf[:, ch, :],
                                                                       identity=identb[0:64, 0:64]), r=["obf", "identb"], w=[pbk])
                                    op("act", lambda e: e.copy(out=catT[:, 4 + h, g8 * 512:(g8 + 1) * 512], in_=pb[:, 0:512]),
                                       r=[pbk], w=[("catB", h, g8)])
                        kb.barrier()
                        psr[:] = [0, 6]
                        pbr[:] = [0, 2]
                    with ExitStack() as L:
                        wo = kb.sb(L, "wo", [128, 8, D], BF16)
                        dma("pool", wo[:], w_out.rearrange("(c p) n -> p c n", p=128), w=["wo"])
                        xts = [kb.sb(L, f"ox{i}", [128, D], F32) for i in range(2)]
                        for t in range(NT):
                            i = t % 2
                            ts = slice(t * 128, (t + 1) * 128)
                            r0 = blk * T + t * 128
                            dma("sp", xts[i][:], x_in[r0:r0 + 128, :], w=[("ox", i)])
                            for hf in range(2):
                                ps, pk = next_ps()
                                for c in range(8):
                                    op("pe", lambda e, c=c: e.matmul(ps[:, :], lhsT=catT[:, c, ts], rhs=wo[:, c, hf * 512:(hf + 1) * 512],
                                                                      start=(c == 0), stop=(c == 7)), r=["wo"], w=[pk])
                                op("dve", lambda e: e.tensor_tensor(out=xts[i][:, hf * 512:(hf + 1) * 512], in0=ps[:, :],
                                                                    in1=xts[i][:, hf * 512:(hf + 1) * 512], op=ALU.add),
                                   r=[pk, ("ox", i)], w=[("ox", i)])
                            dma("pool", xbuf[r0:r0 + 128, :], xts[i][:], r=[("ox", i)])
                        kb.barrier()
        P.close()
        kb.barrier()


    def phase_moe(layer):
        P = ExitStack()
        NTT = S // 128
        aff = kb.sb(P, "aff", [128, NE, NTT], F32)
        pos = kb.sb(P, "pos", [128, NE, NTT], F32)
        gsel = kb.sb(P, "gsel", [128, NE, NTT], F32)
        onesf = kb.sb(P, "onesf", [128, 128], F32)
        load_gain(1 + 2 * layer)
        dma("sp", onesf[:], ones_in[:, :], w=["onesf"])
        with ExitStack() as L:
            wr = kb.sb(L, "wr", [128, 8, NE], F32)
            dma("sp", wr[:], w_router[layer].rearrange("(c p) e -> p c e", p=128), w=["wr"])
            xts = [kb.sb(L, f"rx{i}", [128, D], F32) for i in range(2)]
            hfs = [kb.sb(L, f"rhf{i}", [128, D], F32) for i in range(2)]
            hbs = [kb.sb(L, f"rhb{i}", [128, D], BF16) for i in range(2)]
            hfT = [kb.sb(L, f"rhT{i}", [128, 8, 128], F32) for i in range(2)]
            junk = kb.sb(L, "rjunk", [128, D], BF16)
            sss = [kb.sb(L, f"rss{i}", [128, 1], F32) for i in range(2)]
            rss = [kb.sb(L, f"rrs{i}", [128, 1], F32) for i in range(2)]
            lg = [kb.sb(L, f"rlg{i}", [128, NE], F32) for i in range(2)]
            mx = [kb.sb(L, f"rmx{i}", [128, 1], F32) for i in range(2)]
            sm = [kb.sb(L, f"rsm{i}", [128, 1], F32) for i in range(2)]
            for t in range(NTT):
                i = t % 2
                tg = f"r{i}"
                dma("sp", xts[i][:], xbuf[t * 128:(t + 1) * 128, :], w=[tg + "x"])
                op("act", lambda e: e.activation(out=junk[:], in_=xts[i][:], func=AF.Square, accum_out=sss[i][:]),
                   r=[tg + "x"], w=[tg + "ss"])
                op("act", lambda e: e.activation(out=rss[i][:], in_=sss[i][:], func=AF.Sqrt, bias=epsc[:], scale=1.0 / D),
                   r=[tg + "ss", "epsc"], w=[tg + "rs"])
                op("dve", lambda e: e.reciprocal(out=rss[i][:], in_=rss[i][:]), r=[tg + "rs"], w=[tg + "rs"])
                op("dve", lambda e: e.scalar_tensor_tensor(out=hfs[i][:], in0=xts[i][:], scalar=rss[i][:, 0:1], in1=gain_sb[:],
                                                           op0=ALU.mult, op1=ALU.mult), r=[tg + "x", tg + "rs", "gain"], w=[tg + "hf"])
                op("act", lambda e: e.copy(out=hbs[i][:], in_=hfs[i][:]), r=[tg + "hf"], w=[tg + "hb"])
                dma("pool", h2buf[t * 128:(t + 1) * 128, :], hbs[i][:], r=[tg + "hb"])
                for hh in range(2):
                    ps, pk = next_ps()
                    for c4 in range(4):
                        c = hh * 4 + c4
                        op("pe", lambda e: e.transpose(out=ps[:, c4 * 128:(c4 + 1) * 128], in_=hfs[i][:, c * 128:(c + 1) * 128],
                                                       identity=identf[:]), r=[tg + "hf", "identf"], w=[pk])
                    op("act", lambda e: e.copy(out=hfT[i][:, hh * 4:(hh + 1) * 4, :], in_=ps[:, :].rearrange("p (c t) -> p c t", c=4)),
                       r=[pk], w=[tg + "hT"])
                ps, pk = next_ps()
                for c in range(8):
                    op("pe", lambda e: e.matmul(ps[:, 0:NE], lhsT=hfT[i][:, c, :], rhs=wr[:, c, :], start=(c == 0), stop=(c == 7)),
                       r=[tg + "hT", "wr"], w=[pk])
                op("dve", lambda e: e.tensor_reduce(out=mx[i][:], in_=ps[:, 0:NE], axis=AX.X, op=ALU.max), r=[pk], w=[tg + "mx"])
                op("dve", lambda e: e.tensor_scalar(out=mx[i][:], in0=mx[i][:], scalar1=-1.0, scalar2=None, op0=ALU.mult),
                   r=[tg + "mx"], w=[tg + "mx"])
                op("act", lambda e: e.activation(out=lg[i][:], in_=ps[:, 0:NE], func=AF.Exp, bias=mx[i][:], scale=1.0, accum_out=sm[i][:]),
                   r=[pk, tg + "mx"], w=[tg + "lg", tg + "sm"])
                op("dve", lambda e: e.reciprocal(out=sm[i][:], in_=sm[i][:]), r=[tg + "sm"], w=[tg + "sm"])
                op("dve", lambda e: e.tensor_scalar(out=aff[:, :, t], in0=lg[i][:], scalar1=sm[i][:, 0:1], scalar2=None, op0=ALU.mult),
                   r=[tg + "lg", tg + "sm"], w=["aff"])
            kb.barrier()
        if stage == 30:
            P.close()
            return
        with ExitStack() as L:
            lo = kb.sb(L, "bs_lo", [128, NE], F32)
            mid = kb.sb(L, "bs_mid", [128, NE], F32)
            msk = kb.sb(L, "bs_msk", [128, NE, NTT], F32)
            cnt = kb.sb(L, "bs_cnt", [128, NE], F32)
            cmp_ = kb.sb(L, "bs_cmp", [128, NE], F32)
            op("dve", lambda e: e.memset(lo[:], 0.0), w=["lo"])
            for it in range(30):
                wv = 2.0 ** (-(it + 1))
                op("dve", lambda e: e.tensor_scalar(out=mid[:], in0=lo[:], scalar1=wv, scalar2=None, op0=ALU.add), r=["lo"], w=["mid"])
                op("dve", lambda e: e.tensor_tensor(out=msk[:], in0=aff[:], in1=mid[:, :].unsqueeze(2).to_broadcast([128, NE, NTT]),
                                                    op=ALU.is_ge), r=["aff", "mid"], w=["msk"])
                op("dve", lambda e: e.tensor_reduce(out=cnt[:], in_=msk[:], axis=AX.X, op=ALU.add), r=["msk"], w=["cnt"])
                ps, pk = next_ps()
                op("pe", lambda e: e.matmul(ps[:, 0:NE], lhsT=onesf[:], rhs=cnt[:], start=True, stop=True), r=["onesf", "cnt"], w=[pk])
                op("dve", lambda e: e.tensor_scalar(out=cmp_[:], in0=ps[:, 0:NE], scalar1=CAP - 0.5, scalar2=None, op0=ALU.is_ge),
                   r=[pk], w=["cmp"])
                op("dve", lambda e: e.scalar_tensor_tensor(out=lo[:], in0=cmp_[:], scalar=wv, in1=lo[:], op0=ALU.mult, op1=ALU.add),
                   r=["cmp", "lo"], w=["lo"])
            lst = kb.sb(L, "lstrict", [128, 128], BF16)
            onesb = kb.sb(L, "onesb", [128, 128], BF16)
            rm2 = kb.sb(L, "rm2", [128, NE, NTT], F32)
            mskb = kb.sb(L, "mskb", [128, NE, NTT], BF16)
            wit = kb.sb(L, "wit", [128, NE, NTT], F32)
            tot = kb.sb(L, "tot", [128, NE, NTT], F32)
            dma("pool", lst[:], lstrict_in[:, :], w=["lst"])
            dma("sp", rm2[:], rmask2_in[:, :].rearrange("p (e t) -> p e t", e=NE), w=["rm2"])
            op("dve", lambda e: e.tensor_copy(out=onesb[:], in_=onesf[:]), r=["onesf"], w=["onesb"])
            op("dve", lambda e: e.tensor_tensor(out=msk[:], in0=aff[:], in1=lo[:, :].unsqueeze(2).to_broadcast([128, NE, NTT]),
                                                op=ALU.is_ge), r=["aff", "lo"], w=["msk"])
            op("dve", lambda e: e.tensor_tensor(out=gsel[:], in0=aff[:], in1=msk[:], op=ALU.mult), r=["aff", "msk"], w=["gsel"])
            op("dve", lambda e: e.tensor_copy(out=mskb[:], in_=msk[:]), r=["msk"], w=["mskb"])
            mflat = mskb[:, :, :].rearrange("p e t -> p (e t)")
            for hf2 in range(2):
                ps, pk = next_ps()
                op("pe", lambda e: e.matmul(ps[:, :], lhsT=lst[:], rhs=mflat[:, hf2 * 512:(hf2 + 1) * 512], start=True, stop=True),
                   r=["lst", "mskb"], w=[pk])
                op("act", lambda e: e.copy(out=wit[:, hf2 * 8:(hf2 + 1) * 8, :], in_=ps[:, :].rearrange("p (e t) -> p e t", e=8)),
                   r=[pk], w=["wit"])
                ps2, pk2 = next_ps()
                op("pe", lambda e: e.matmul(ps2[:, :], lhsT=onesb[:], rhs=mflat[:, hf2 * 512:(hf2 + 1) * 512], start=True, stop=True),
                   r=["onesb", "mskb"], w=[pk2])
                op("act", lambda e: e.copy(out=tot[:, hf2 * 8:(hf2 + 1) * 8, :], in_=ps2[:, :].rearrange("p (e t) -> p e t", e=8)),
                   r=[pk2], w=["tot"])
            op("dve", lambda e: e.tensor_tensor_scan(out=pos[:, :, :].rearrange("p e t -> p (e t)"),
                                                     data0=rm2[:, :, :].rearrange("p e t -> p (e t)"),
                                                     data1=tot[:, :, :].rearrange("p e t -> p (e t)"), initial=0.0,
                                                     op0=ALU.mult, op1=ALU.add), r=["rm2", "tot"], w=["pos"])
            op("dve", lambda e: e.tensor_tensor(out=pos[:], in0=pos[:], in1=tot[:], op=ALU.subtract), r=["pos", "tot"], w=["pos"])
            op("dve", lambda e: e.tensor_tensor(out=pos[:], in0=pos[:], in1=wit[:], op=ALU.add), r=["pos", "wit"], w=["pos"])
            op("dve", lambda e: e.scalar_tensor_tensor(out=pos[:], in0=pos[:], scalar=1.0, in1=msk[:], op0=ALU.add, op1=ALU.mult),
               r=["pos", "msk"], w=["pos"])
            op("dve", lambda e: e.tensor_scalar(out=pos[:], in0=pos[:], scalar1=-1.0, scalar2=None, op0=ALU.add), r=["pos"], w=["pos"])
            kb.barrier()
        with ExitStack() as L:
            iota = kb.sb(L, "iota", [128, CAP], F32)
            tokd = kb.sb(L, "tokd", [128, NTT, 2], F32)
            Eb = [kb.sb(L, f"Eb{i}", [128, CAP], BF16) for i in range(2)]
            rhs4 = [kb.sb(L, f"rhs4{i}", [128, NTT, 4], BF16) for i in range(2)]
            gtmp = kb.sb(L, "gtmp", [128, NTT], F32)
            sl4 = kb.sb(L, "sl4", [4, CAP], F32)
            slT = kb.sb(L, "slT", [128, 8, 4], F32)
            idxf = kb.sb(L, "idxf", [128, 8], F32)
            idxs = [kb.sb(L, f"idxi{i}", [128, 8], I32) for i in range(2)]
            gss = [kb.sb(L, f"gs{i}", [128, 8], F32) for i in range(2)]
            xe = kb.sb(L, "xe", [128, 8, D], BF16)
            xeT = kb.sb(L, "xeT", [128, 8, CAP], BF16)
            hTm = kb.sb(L, "hTm", [128, 8, CAP], BF16)
            sgt = [kb.sb(L, f"sgt{i}", [128, 512], F32) for i in range(2)]
            yts = [kb.sb(L, f"yt{i}", [128, D], F32) for i in range(2)]
            Wg = [kb.sb(L, f"Wg{i}", [128, 8, D], BF16) for i in range(2)]
            Wu = [kb.sb(L, f"Wu{i}", [128, 8, D], BF16) for i in range(2)]
            Wd = [kb.sb(L, f"Wd{i}", [128, 8, D], BF16) for i in range(2)]
            dma("sp", iota[:], iota_in[:, :], w=["iota"])
            dma("sp", tokd[:], tokdig_in[:, :, :], w=["tokd"])
            for i in range(2):
                op("dve", lambda e: e.tensor_copy(out=rhs4[i][:, :, 0:2], in_=tokd[:]), r=["tokd"], w=[("rhs4", i)])

            def load_expert(ex):
                i = ex % 2
                for nm, wt_, src in (("Wg", Wg[i], w_gate), ("Wu", Wu[i], w_up), ("Wd", Wd[i], w_down)):
                    for c2 in range(2):
                        dma("pool", wt_[:, c2 * 4:(c2 + 1) * 4, :],
                            src[layer, ex].rearrange("(c p) n -> p c n", p=128)[:, c2 * 4:(c2 + 1) * 4, :], w=[(nm, i)])

            load_expert(0)
            for ex in range(NE):
                i = ex % 2
                if ex + 1 < NE:
                    load_expert(ex + 1)
                op("dve", lambda e: e.tensor_copy(out=rhs4[i][:, :, 2], in_=gsel[:, ex, :]), r=["gsel"], w=[("rhs4", i)])
                op("dve", lambda e: e.tensor_tensor(out=gtmp[:], in0=gsel[:, ex, :], in1=rhs4[i][:, :, 2], op=ALU.subtract),
                   r=["gsel", ("rhs4", i)], w=["gtmp"])
                op("dve", lambda e: e.tensor_copy(out=rhs4[i][:, :, 3], in_=gtmp[:]), r=["gtmp"], w=[("rhs4", i)])
                psA, pkA = next_ps()
                psB, pkB = next_ps()
                for t in range(NTT):
                    j = t % 2
                    op("dve", lambda e: e.tensor_scalar(out=Eb[j][:], in0=iota[:], scalar1=pos[:, ex, t:t + 1], scalar2=None,
                                                        op0=ALU.is_equal), r=["iota", "pos"], w=[("Eb", j)])
                    op("pe", lambda e: e.matmul(psA[0:4, :], lhsT=rhs4[i][:, t, :], rhs=Eb[j][:, 0:512], start=(t == 0), stop=(t == NTT - 1)),
                       r=[("rhs4", i), ("Eb", j)], w=[pkA])
                    op("pe", lambda e: e.matmul(psB[0:4, :], lhsT=rhs4[i][:, t, :], rhs=Eb[j][:, 512:1024], start=(t == 0), stop=(t == NTT - 1)),
                       r=[("rhs4", i), ("Eb", j)], w=[pkB])
                op("act", lambda e: e.copy(out=sl4[:, 0:512], in_=psA[0:4, :]), r=[pkA], w=["sl4"])
                op("act", lambda e: e.copy(out=sl4[:, 512:1024], in_=psB[0:4, :]), r=[pkB], w=["sl4"])
                ps, pk = next_ps()
                for sbk in range(8):
                    op("pe", lambda e: e.transpose(out=ps[:, sbk * 4:(sbk + 1) * 4], in_=sl4[:, sbk * 128:(sbk + 1) * 128],
                                                   identity=identf[0:4, 0:4]), r=["sl4", "identf"], w=[pk])
                op("act", lambda e: e.copy(out=slT[:], in_=ps[:, 0:32].rearrange("p (s c) -> p s c", c=4)), r=[pk], w=["slT"])
                op("dve", lambda e: e.scalar_tensor_tensor(out=idxf[:], in0=slT[:, :, 0], scalar=128.0, in1=slT[:, :, 1],
                                                           op0=ALU.mult, op1=ALU.add), r=["slT"], w=["idxf"])
                op("dve", lambda e: e.tensor_copy(out=idxs[i][:], in_=idxf[:]), r=["idxf"], w=[("idx", i)])
                op("dve", lambda e: e.tensor_tensor(out=gss[i][:], in0=slT[:, :, 2], in1=slT[:, :, 3], op=ALU.add), r=["slT"], w=[("gs", i)])
                for sbk in range(8):
                    kb.idma(out=xe[:, sbk, :], in_=h2buf[:, :], in_offset=bass.IndirectOffsetOnAxis(ap=idxs[i][:, sbk:sbk + 1], axis=0),
                            r=[("idx", i)], w=[("xe", sbk)])
                for sbk in range(8):
                    pb, pbk = next_pb()
                    for c in range(8):
                        op("pe", lambda e: e.transpose(out=pb[:, c * 128:(c + 1) * 128], in_=xe[:, sbk, c * 128:(c + 1) * 128],
                                                       identity=identb[:]), r=[("xe", sbk), "identb"], w=[pbk])
                    op("act", lambda e: e.copy(out=xeT[:, :, sbk * 128:(sbk + 1) * 128], in_=pb[:, :].rearrange("p (c t) -> p c t", c=8)),
                       r=[pbk], w=[("xeT", sbk // 4)])
                for fb in range(8):
                    for sh in range(2):
                        psg, pkg = next_ps()
                        psu, pku = next_ps()
                        for c in range(8):
                            op("pe", lambda e: e.matmul(psg[:, :], lhsT=Wg[i][:, c, fb * 128:(fb + 1) * 128], rhs=xeT[:, c, sh * 512:(sh + 1) * 512],
                                                        start=(c == 0), stop=(c == 7)), r=[("Wg", i), ("xeT", sh)], w=[pkg])
                        for c in range(8):
                            op("pe", lambda e: e.matmul(psu[:, :], lhsT=Wu[i][:, c, fb * 128:(fb + 1) * 128], rhs=xeT[:, c, sh * 512:(sh + 1) * 512],
                                                        start=(c == 0), stop=(c == 7)), r=[("Wu", i), ("xeT", sh)], w=[pku])
                        k2 = (fb * 2 + sh) % 2
                        op("act", lambda e: e.activation(out=sgt[k2][:], in_=psg[:, :], func=AF.Silu), r=[pkg], w=[("sgt", k2)])
                        op("dve", lambda e: e.tensor_tensor(out=hTm[:, fb, sh * 512:(sh + 1) * 512], in0=psu[:, :], in1=sgt[k2][:], op=ALU.mult),
                           r=[pku, ("sgt", k2)], w=[("hTm", fb, sh)])
                for sbk in range(8):
                    k2 = sbk % 2
                    for dh in range(2):
                        ps, pk = next_ps()
                        for fb in range(8):
                            op("pe", lambda e: e.matmul(ps[:, :], lhsT=hTm[:, fb, sbk * 128:(sbk + 1) * 128], rhs=Wd[i][:, fb, dh * 512:(dh + 1) * 512],
                                                        start=(fb == 0), stop=(fb == 7)), r=[("Wd", i), ("hTm", fb, sbk // 4)], w=[pk])
                        op("act", lambda e: e.activation(out=yts[k2][:, dh * 512:(dh + 1) * 512], in_=ps[:, :], func=AF.Copy,
                                                         scale=gss[i][:, sbk:sbk + 1]), r=[pk, ("gs", i)], w=[("yt", k2)])
                    kb.idma(out=xbuf[:, :], out_offset=bass.IndirectOffsetOnAxis(ap=idxs[i][:, sbk:sbk + 1], axis=0), in_=yts[k2][:],
                            compute_op=ALU.add, r=[("yt", k2), ("idx", i)] + [("xs", ex - 1, jj) for jj in range(8)], w=[("xs", ex, sbk)])
            kb.barrier()
        P.close()
        kb.barrier()

    def phase_final():
        load_gain(4)
        with ExitStack() as L:
            xts = [kb.sb(L, f"fx{i}", [128, D], F32) for i in range(2)]
            ots = [kb.sb(L, f"fo{i}", [128, D], F32) for i in range(2)]
            junk = kb.sb(L, "fjunk", [128, D], BF16)
            sss = [kb.sb(L, f"fss{i}", [128, 1], F32) for i in range(2)]
            rss = [kb.sb(L, f"frs{i}", [128, 1], F32) for i in range(2)]
            for t in range(S // 128):
                i = t % 2
                tg = f"f{i}"
                dma("sp", xts[i][:], xbuf[t * 128:(t + 1) * 128, :], w=[tg + "x"])
                if do_final_norm:
                    op("act", lambda e: e.activation(out=junk[:], in_=xts[i][:], func=AF.Square, accum_out=sss[i][:]),
                       r=[tg + "x"], w=[tg + "ss"])
                    op("act", lambda e: e.activation(out=rss[i][:], in_=sss[i][:], func=AF.Sqrt, bias=epsc[:], scale=1.0 / D),
                       r=[tg + "ss", "epsc"], w=[tg + "rs"])
                    op("dve", lambda e: e.reciprocal(out=rss[i][:], in_=rss[i][:]), r=[tg + "rs"], w=[tg + "rs"])
                    op("dve", lambda e: e.scalar_tensor_tensor(out=ots[i][:], in0=xts[i][:], scalar=rss[i][:, 0:1], in1=gain_sb[:],
                                                               op0=ALU.mult, op1=ALU.mult), r=[tg + "x", tg + "rs", "gain"], w=[tg + "o"])
                    dma("pool", out_d[t * 128:(t + 1) * 128, :], ots[i][:], r=[tg + "o"], w=[("outd", t)])
                else:
                    dma("pool", out_d[t * 128:(t + 1) * 128, :], xts[i][:], r=[tg + "x"], w=[("outd", t)])
            kb.barrier()

    PV = 1088
    KP = 1024
    DILS = (1, 4, 16)

    def phase_attn():
        P = ExitStack()
        load_gain(2)
        with ExitStack() as L:
            rbl = kb.sb(L, "rbl", [33, 16], F32)
            gq = kb.sb(L, "gq", [33, 3, 383], F32)
            vecs = kb.sb(L, "vecs", [16, 3, 383], F32)
            zt = kb.sb(L, "zt", [128, 1040], BF16)
            op("dve", lambda e: e.memset(rbl[:], 1.0), w=["rbl"])
            dma("sp", rbl[0:32, :], rel_bias[:, :], w=["rbl"])
            dma("sp", gq[:], gpat_in[:, :, :], w=["gq"])
            for di in range(3):
                ps, pk = next_ps()
                op("pe", lambda e: e.matmul(ps[0:16, 0:383], lhsT=rbl[:], rhs=gq[:, di, :], start=True, stop=True), r=["rbl", "gq"], w=[pk])
                op("act", lambda e: e.copy(out=vecs[:, di, :], in_=ps[0:16, 0:383]), r=[pk], w=["vecs"])
            dma("sp", vecbuf.rearrange("d h u -> h d u"), vecs[:], r=["vecs"], w=["vecbuf"])
            op("dve", lambda e: e.memset(zt[:], 0.0), w=["zt"])
            for hp in range(8):
                dma("sp", kTbuf[hp, :, 0:KP], zt[:, 0:KP], r=["zt"])
                dma("sp", kTbuf[hp, :, KP + S:KP + S + KP], zt[:, 0:KP], r=["zt"])
            for base in (0, PV + S):
                for a in range(8):
                    dma("sp", vbuf[base + a * 128:base + (a + 1) * 128, :, :], zt[:, :].rearrange("p (h c) -> p h c", h=16), r=["zt"])
                dma("sp", vbuf[base + 1024:base + 1088, :, :], zt[0:64, :].rearrange("p (h c) -> p h c", h=16), r=["zt"])
            kb.barrier()
        with ExitStack() as L:
            wqkv = kb.sb(L, "wqkv", [128, 8, 3072], BF16)
            for j in range(6):
                dma("pool", wqkv[:, :, j * 512:(j + 1) * 512], w_qkv.rearrange("(c p) n -> p c n", p=128)[:, :, j * 512:(j + 1) * 512], w=["wqkv"])
            hT = kb.sb(L, "hT1", [128, 8, T], BF16)
            xts = [kb.sb(L, f"a1x{i}", [128, D], F32) for i in range(2)]
            hbs = [kb.sb(L, f"a1h{i}", [128, D], BF16) for i in range(2)]
            junk = kb.sb(L, "a1junk", [128, D], BF16)
            sss = [kb.sb(L, f"a1ss{i}", [128, 1], F32) for i in range(2)]
            rss = [kb.sb(L, f"a1rs{i}", [128, 1], F32) for i in range(2)]
            qst = [kb.sb(L, f"qst{i}", [128, 512], BF16) for i in range(2)]
            vau = [kb.sb(L, f"vau{i}", [128, 16, 65], BF16) for i in range(2)]
            for i in range(2):
                op("dve", lambda e: e.memset(vau[i][:], 1.0), w=[("vau", i)])
            nq = 0
            for blk in range(NB):
                for t in range(NT):
                    i = t % 2
                    tag = f"a1{i}"
                    r0 = blk * T + t * 128
                    dma("sp", xts[i][:], xbuf[r0:r0 + 128, :], w=[tag + "x"])
                    rms_tile(tag, xts[i][:], hbs[i], junk, sss[i], rss[i], [tag + "x"])
                    pb, pk = next_pb()
                    for c in range(8):
                        op("pe", lambda e: e.transpose(out=pb[:, c * 128:(c + 1) * 128], in_=hbs[i][:, c * 128:(c + 1) * 128], identity=identb[:]),
                           r=[tag + "hb", "identb"], w=[pk])
                    op("act", lambda e: e.copy(out=hT[:, :, t * 128:(t + 1) * 128], in_=pb[:, :].rearrange("p (c t) -> p c t", c=8)),
                       r=[pk], w=[("hT1", t)])
                for which in range(2):
                    for hp in range(8):
                        for tb in range(T // 512):
                            ps, pk = next_ps()
                            col = which * 1024 + hp * 128
                            for c in range(8):
                                op("pe", lambda e: e.matmul(ps[:, :], lhsT=wqkv[:, c, col:col + 128], rhs=hT[:, c, tb * 512:(tb + 1) * 512],
                                                            start=(c == 0), stop=(c == 7)),
                                   r=["wqkv"] + [("hT1", t) for t in range(tb * 4, tb * 4 + 4)], w=[pk])
                            k2 = nq % 2
                            nq += 1
                            op("act", lambda e: e.activation(out=qst[k2][:], in_=ps[:, :], func=AF.Copy, scale=(0.125 if which == 0 else 1.0)),
                               r=[pk], w=[("qst", k2)])
                            c0 = blk * T + tb * 512
                            if which == 0:
                                dma("pool", qTbuf[hp, :, c0:c0 + 512], qst[k2][:], r=[("qst", k2)])
                            else:
                                dma("pool", kTbuf[hp, :, KP + c0:KP + c0 + 512], qst[k2][:], r=[("qst", k2)])
                for t in range(NT):
                    i = t % 2
                    for hf2 in range(2):
                        ps, pk = next_ps()
                        for c in range(8):
                            op("pe", lambda e: e.matmul(ps[:, :], lhsT=hT[:, c, t * 128:(t + 1) * 128], rhs=wqkv[:, c, 2048 + hf2 * 512:2048 + (hf2 + 1) * 512],
                                                        start=(c == 0), stop=(c == 7)), r=["wqkv", ("hT1", t)], w=[pk])
                        op("act", lambda e: e.copy(out=vau[i][:, hf2 * 8:(hf2 + 1) * 8, 0:64], in_=ps[:, :].rearrange("p (h c) -> p h c", h=8)),
                           r=[pk], w=[("vau", i)])
                    r0 = PV + blk * T + t * 128
                    dma("pool", vbuf[r0:r0 + 128, :, :], vau[i][:], r=[("vau", i)])
            kb.barrier()
        if stage == 40:
            P.close()
            return
        AB = 2048
        with ExitStack() as L:
            kw = kb.sb(L, "kw", [128, 4, AB + 2 * KP], BF16)
            qw = kb.sb(L, "qw", [128, 4, AB], BF16)
            Vd = [kb.sb(L, f"Vd{i}", [128, 32, 8, 65], BF16) for i in range(2)]
            BTd = [kb.sb(L, f"BTd{i}", [128, 8, 2, 128], F32) for i in range(2)]
            BTr = kb.sb(L, "BTr", [128, 8, 2, 128], F32)
            sTs = [kb.sb(L, f"sTs{i}", [128, 512], F32) for i in range(4)]
            pTs = [kb.sb(L, f"pTs{i}", [128, 512], BF16) for i in range(4)]
            stg = [kb.sb(L, f"stg{i}", [128, 8, 65], F32) for i in range(2)]
            nv = 0
            ntile = 0
            nh = 0
            for ab in range(S // AB):
                for hh in range(2):
                    for j in range(4):
                        dma("sp", kw[:, j, :], kTbuf[hh * 4 + j, :, ab * AB:ab * AB + AB + 2 * KP], w=[("kw", j)])
                        dma("sp", qw[:, j, :], qTbuf[hh * 4 + j, :, ab * AB:(ab + 1) * AB], w=[("qw", j)])
                    for di, dl in enumerate(DILS):
                        vi = nv % 2
                        nv += 1
                        nrt = 16 // dl + 1
                        for r in range(dl):
                            st0 = PV + ab * AB + r - 64 * dl
                            src = vbuf[st0:st0 + dl * (nrt * 128 - 1) + 1:dl, hh * 8:(hh + 1) * 8, :].rearrange("(jt p) h c -> p jt h c", p=128)
                            dma("sp", Vd[vi][:, r * nrt:(r + 1) * nrt, :, :], src, w=[("Vd", vi, r)])
                        for h8 in range(8):
                            off = (di * 16 + hh * 8 + h8) * 383
                            src = bass.AP(tensor=vecbuf.tensor, offset=off, ap=[[1, 128], [128, 2], [1, 128]])
                            dma("sp", BTr[:, h8, :, :], src, r=["vecbuf"], w=[("BTr", h8)])
                            op("dve", lambda e: e.tensor_copy(out=BTd[vi][:, h8, :, :], in_=BTr[:, h8, :, ::-1]), r=[("BTr", h8)], w=[("BTd", vi)])
                        items = []
                        for r in range(dl):
                            for it in range(16 // dl):
                                si = ntile % 2
                                ntile += 1
                                for jj in range(4):
                                    items.append((r, it, si, jj, nh))
                                    nh += 1

                        def stA(item):
                            r, it, si, jj, n = item
                            psd = pdb[n % 2]
                            for hq in range(2):
                                pl = hq * 64
                                kw3 = kw[pl:pl + 64, jj, :].rearrange("p (m d) -> p m d", d=dl)
                                qw3 = qw[pl:pl + 64, jj, :].rearrange("p (m d) -> p m d", d=dl)
                                k0 = KP // dl - 64 + 128 * it
                                for c in range(2):
                                    cc0 = hq * 512 + c * 128
                                    op("pe", lambda e: e.matmul(psd[:, cc0:cc0 + 128], lhsT=kw3[:, k0 + 128 * c:k0 + 128 * (c + 1), r],
                                                                rhs=qw3[:, 128 * it:128 * (it + 1), r], start=True, stop=True),
                                       r=[("kw", jj), ("qw", jj)], w=[("pst", 2 * (n % 2) + hq)])

                        def stB(item):
                            r, it, si, jj, n = item
                            psd = pdb[n % 2]
                            k2 = n % 4
                            op("dve", lambda e: e.tensor_tensor(out=sTs[k2][:, :].rearrange("p (h x) -> p h x", h=2),
                                                                in0=psd[:, :].rearrange("p (h x) -> p h x", h=2)[:, :, 0:256],
                                                                in1=BTd[vi][:, 2 * jj:2 * jj + 2, :, :].rearrange("p h c q -> p h (c q)"), op=ALU.add),
                               r=[("pst", 2 * (n % 2)), ("pst", 2 * (n % 2) + 1), ("BTd", vi)], w=[("sTs", k2)])
                            op("act", lambda e: e.activation(out=pTs[k2][:], in_=sTs[k2][:], func=AF.Exp), r=[("sTs", k2)], w=[("pTs", k2)])

                        def stC(item):
                            r, it, si, jj, n = item
                            k2 = n % 4
                            pj_ = 4 + (n % 2)
                            ps2, pk2 = pst[pj_], ("pst", pj_)
                            for hq in range(2):
                                h8 = 2 * jj + hq
                                for c in range(2):
                                    cc0 = hq * 256 + c * 128
                                    op("pe", lambda e: e.matmul(ps2[:, hq * 128:hq * 128 + 65], lhsT=pTs[k2][:, cc0:cc0 + 128],
                                                                rhs=Vd[vi][:, r * nrt + it + c, h8, :], start=(c == 0), stop=(c == 1)),
                                       r=[("pTs", k2), ("Vd", vi, r)], w=[pk2])
                            if n % 2 == 0:
                                op("act", lambda e: e.copy(out=stg[si][:, 2 * jj:2 * jj + 2, :], in_=ps2[:, 0:256].rearrange("p (h c) -> p h c", h=2)[:, :, 0:65]),
                                   r=[pk2], w=[("stg", si, jj)])
                            else:
                                op("dve", lambda e: e.tensor_copy(out=stg[si][:, 2 * jj:2 * jj + 2, :], in_=ps2[:, 0:256].rearrange("p (h c) -> p h c", h=2)[:, :, 0:65]),
                                   r=[pk2], w=[("stg", si, jj)])
                            if jj == 3:
                                q0 = ab * AB + r + dl * 128 * it
                                dma("pool", accbuf[di, q0:q0 + dl * 127 + 1:dl, hh * 8:(hh + 1) * 8, :], stg[si][:], r=[("stg", si, jj_) for jj_ in range(4)])

                        NI = len(items)
                        stA(items[0])
                        if NI > 1:
                            stA(items[1])
                        stB(items[0])
                        for n_, item in enumerate(items):
                            if n_ + 2 < NI:
                                stA(items[n_ + 2])
                            if n_ + 1 < NI:
                                stB(items[n_ + 1])
                            stC(item)
            kb.barrier()
        if stage == 41:
            P.close()
            return
        with ExitStack() as L:
            wo = kb.sb(L, "wo1", [128, 8, D], BF16)
            dma("pool", wo[:], w_o.rearrange("(c p) n -> p c n", p=128), w=["wo1"])
            acs = [[kb.sb(L, f"ac{i}_{k}", [128, 16, 65], F32) for k in range(3)] for i in range(2)]
            rl = [kb.sb(L, f"rl{i}", [128, 16], F32) for i in range(2)]
            atb = [kb.sb(L, f"atb{i}", [128, 16, 64], BF16) for i in range(2)]
            atT = [kb.sb(L, f"atT{i}", [128, 8, 128], BF16) for i in range(2)]
            xts = [kb.sb(L, f"a3x{i}", [128, D], F32) for i in range(2)]
            for t in range(S // 128):
                i = t % 2
                tg = f"a3{i}"
                for k in range(3):
                    dma("sp", acs[i][k][:], accbuf[k, t * 128:(t + 1) * 128, :, :], w=[(tg, "ac", k)])
                dma("sp", xts[i][:], xbuf[t * 128:(t + 1) * 128, :], w=[tg + "x"])
                if stage in (143, 144, 145):
                    kk_ = stage - 143
                    op("dve", lambda e: e.tensor_copy(out=xts[i][:], in_=acs[i][kk_][:, :, :].rearrange("p h c -> p (h c)")[:, 0:1024]),
                       r=[(tg, "ac", kk_), tg + "x"], w=[tg + "x"])
                    dma("pool", xbuf[t * 128:(t + 1) * 128, :], xts[i][:], r=[tg + "x"])
                    continue
                op("dve", lambda e: e.tensor_tensor(out=acs[i][0][:], in0=acs[i][0][:], in1=acs[i][1][:], op=ALU.add),
                   r=[(tg, "ac", 0), (tg, "ac", 1)], w=[(tg, "ac", 0)])
                op("dve", lambda e: e.tensor_tensor(out=acs[i][0][:], in0=acs[i][0][:], in1=acs[i][2][:], op=ALU.add),
                   r=[(tg, "ac", 0), (tg, "ac", 2)], w=[(tg, "ac", 0)])
                op("dve", lambda e: e.reciprocal(out=rl[i][:], in_=acs[i][0][:, :, 64]), r=[(tg, "ac", 0)], w=[tg + "rl"])
                op("dve", lambda e: e.tensor_tensor(out=atb[i][:], in0=acs[i][0][:, :, 0:64],
                                                    in1=rl[i][:, :].unsqueeze(2).to_broadcast([128, 16, 64]), op=ALU.mult),
                   r=[(tg, "ac", 0), tg + "rl"], w=[tg + "atb"])
                atf = atb[i][:, :, :].rearrange("p h c -> p (h c)")
                if stage == 141:
                    op("dve", lambda e: e.tensor_copy(out=xts[i][:], in_=atf), r=[tg + "atb", tg + "x"], w=[tg + "x"])
                    dma("pool", xbuf[t * 128:(t + 1) * 128, :], xts[i][:], r=[tg + "x"])
                    continue
                pb, pk = next_pb()
                for c in range(8):
                    op("pe", lambda e: e.transpose(out=pb[:, c * 128:(c + 1) * 128], in_=atf[:, c * 128:(c + 1) * 128], identity=identb[:]),
                       r=[tg + "atb", "identb"], w=[pk])
                op("act", lambda e: e.copy(out=atT[i][:], in_=pb[:, :].rearrange("p (c t) -> p c t", c=8)), r=[pk], w=[tg + "atT"])
                for hf2 in range(2):
                    ps, pk2 = next_ps()
                    for c in range(8):
                        op("pe", lambda e: e.matmul(ps[:, :], lhsT=atT[i][:, c, :], rhs=wo[:, c, hf2 * 512:(hf2 + 1) * 512],
                                                    start=(c == 0), stop=(c == 7)), r=[tg + "atT", "wo1"], w=[pk2])
                    op("dve", lambda e: e.tensor_tensor(out=xts[i][:, hf2 * 512:(hf2 + 1) * 512], in0=ps[:, :],
                                                        in1=xts[i][:, hf2 * 512:(hf2 + 1) * 512], op=ALU.add), r=[pk2, tg + "x"], w=[tg + "x"])
                dma("pool", xbuf[t * 128:(t + 1) * 128, :], xts[i][:], r=[tg + "x"])
            kb.barrier()
        P.close()
        kb.barrier()

    do_final_norm = stage == 99
    if stage >= 100 or stage in (40, 41):
        with ExitStack() as L:
            xt = [kb.sb(L, f"cx{i}", [128, D], F32) for i in range(2)]
            for t in range(S // 128):
                i = t % 2
                dma("sp", xt[i][:], x_in[t * 128:(t + 1) * 128, :], w=[("cx", i)])
                dma("sp", xbuf[t * 128:(t + 1) * 128, :], xt[i][:], r=[("cx", i)])
            kb.barrier()
        if stage in (104, 141, 142, 143, 144, 145, 40, 41):
            phase_attn()
        if stage == 105:
            phase_moe(1)
    else:
        phase_m0()
        if stage >= 3:
            phase_moe(0)
        if stage >= 4:
            phase_attn()
        if stage >= 5:
            phase_moe(1)
    phase_final()

    G.close()
    kb.es.close()
    return nc


def _t5_bucket(rel):
    half_buckets, max_exact = 16, 8
    n = np.abs(rel)
    scaled = (np.log(np.maximum(n, 1).astype(np.float32) / np.float32(max_exact)) / np.float32(np.log(1024 / max_exact))).astype(np.float32)
    large = np.minimum(max_exact + (scaled * np.float32(half_buckets - max_exact)).astype(np.int32), half_buckets - 1)
    return np.where(rel > 0, half_buckets, 0) + np.where(n < max_exact, n, large)


def _bias_patterns():
    g = np.zeros((33, 3, 383), dtype=np.float32)
    u = np.arange(383)
    rel = u - 191
    valid = np.abs(rel) <= 64
    for di, dl in enumerate((1, 4, 16)):
        b = _t5_bucket(rel * dl)
        for uu in range(383):
            if valid[uu]:
                g[b[uu], di, uu] = 1.0
            else:
                g[32, di, uu] = -1e30
    return g


def host_inputs(inp, b):
    f = np.float32
    d = {}
    d["x"] = np.ascontiguousarray(inp["x"][b], dtype=f)
    g = np.stack([inp["mix_norm"][0], inp["ffn_norm"][0], inp["mix_norm"][1], inp["ffn_norm"][1], inp["final_norm"]])
    d["gains"] = np.ascontiguousarray(np.broadcast_to(g[:, None, :], (5, 128, D)), dtype=f)
    d["w_in"] = np.ascontiguousarray(inp["w_in_even"][0], dtype=f)
    d["w_out"] = np.ascontiguousarray(inp["w_out_even"][0], dtype=f)
    d["a_ln"] = np.ascontiguousarray(np.broadcast_to(np.stack([inp["a_ln_g"][0], inp["a_ln_b"][0]])[:, None, :], (2, 128, 512)), dtype=f)
    d["a_wsT"] = np.ascontiguousarray(np.transpose(inp["a_w_s"][0], (2, 0, 1)), dtype=f)
    d["a_bs"] = np.ascontiguousarray(np.repeat(np.transpose(inp["a_b_s"][0], (1, 0))[:, :, None], 128, axis=2).reshape(128, 512), dtype=f)
    d["lbt"] = np.ascontiguousarray(np.transpose(inp["b_lb_table"].reshape(2, 3, 4, 128), (3, 2, 0, 1)), dtype=f)
    d["b_ng"] = np.ascontiguousarray(np.broadcast_to(inp["b_norm_g"][0][None, :], (64, 512)), dtype=f)
    d["ident_f"] = np.eye(128, dtype=f)
    rm = np.ones((128, T), dtype=f)
    rm[:, ::64] = 0.0
    d["rmask"] = rm
    s_ = np.arange(64)[:, None]
    t_ = np.arange(64)[None, :]
    d["tri"] = np.ascontiguousarray(np.stack([(s_ <= t_), (s_ >= t_)], axis=1), dtype=f)
    d["ones_f"] = np.ones((128, 128), dtype=f)
    pp = np.arange(128)
    d["lstrict"] = np.ascontiguousarray((pp[:, None] < pp[None, :]), dtype=f)
    d["iota_s"] = np.ascontiguousarray(np.broadcast_to(np.arange(CAP, dtype=f)[None, :], (128, CAP)))
    r2 = np.ones((128, NE, 64), dtype=f)
    r2[:, :, 0] = 0.0
    d["rmask2"] = r2.reshape(128, NE * 64)
    td = np.zeros((128, 64, 2), dtype=f)
    td[:, :, 0] = np.arange(64)[None, :]
    td[:, :, 1] = np.arange(128)[:, None]
    d["tokdig"] = td
    d["rel_bias"] = np.ascontiguousarray(inp["rel_bias"], dtype=f)
    d["gpat"] = _bias_patterns()
    d["w_qkv"] = np.ascontiguousarray(inp["w_qkv_odd"][0], dtype=f)
    d["w_o"] = np.ascontiguousarray(inp["w_o_odd"][0], dtype=f)
    d["w_router"] = np.ascontiguousarray(inp["w_router"], dtype=f)
    d["w_gate"] = np.ascontiguousarray(inp["w_gate"], dtype=f)
    d["w_up"] = np.ascontiguousarray(inp["w_up"], dtype=f)
    d["w_down"] = np.ascontiguousarray(inp["w_down"], dtype=f)
    return d


_CACHE = {}


def kernel(**inputs):
    inp = {k: np.asarray(v) for k, v in inputs.items()}
    if "nc" not in _CACHE:
        _CACHE["nc"] = build_program()
    nc = _CACHE["nc"]
    per_b = [host_inputs(inp, b) for b in range(2)]
    if globals().get("_DBG_STAGE", 0) in (104, 141, 142, 143, 144, 145, 40, 41):
        for pb_ in per_b:
            for k_ in ("w_gate", "w_up", "w_down"):
                pb_.pop(k_)
    in_maps = [per_b[c % 2] for c in range(8)]
    res = run_bass_kernel_spmd(nc, in_maps, core_ids=list(range(8)))
    out = np.stack([res.results[0]["out"], res.results[1]["out"]], axis=0).astype(np.float32)
    return out
```
